# Optimizing a Trainium2 kernel written in Bass

```python
import math
import jax
import jax.numpy as jnp
from jax import lax
import numpy as np

D_MODEL = 1024
BATCH = 32
SEQ = 2048
DEPTH = 2

GRID_W = 64
CTX_LEN = 256
EPS = 1e-6
CONV_W = 4
CONV_PAD = (CONV_W // 2, CONV_W - 1 - CONV_W // 2)

LRU_WIDTH = D_MODEL // 2
LRU_BLOCKS = 8
LRU_BLOCK = LRU_WIDTH // LRU_BLOCKS
LRU_C = 8.0

DN_HEAD_DIM = 128
DN_HEADS = D_MODEL // DN_HEAD_DIM
DN_WIDTH = DN_HEADS * DN_HEAD_DIM
DN_CHUNK = 64

S5_WIDTH = D_MODEL // 2
S5_GROUP = 16
S5_GROUPS = S5_WIDTH // S5_GROUP
S5_STATE = 64

N_BRANCH = 3

N_EXPERTS = 32
TOP_K = 4
D_EXPERT = D_MODEL
SWIGLU_LIMIT = 7.0
SWIGLU_ALPHA = 1.702
MOE_BLOCK = 512

IN_SIZES = (LRU_WIDTH, LRU_WIDTH, DN_WIDTH, DN_WIDTH, DN_WIDTH, DN_WIDTH,
            2 * DN_HEADS, 2 * DN_HEADS, S5_WIDTH, N_BRANCH * D_MODEL)
D_IN = sum(IN_SIZES)

kernel_name = "hybrid_lru_deltanet_s5_moe_dit"


def rmsnorm(x, g):
    xf = x.astype(jnp.float32)
    y = xf * lax.rsqrt(jnp.mean(xf * xf, axis=-1, keepdims=True) + EPS)
    return (y * g.astype(jnp.float32)).astype(x.dtype)


def l2norm(x):
    return x * lax.rsqrt(jnp.sum(x * x, axis=-1, keepdims=True) + EPS)


def modulate(h, shift, scale):
    return h * (1.0 + scale) + shift


def dwconv(x, w, rows):
    bsz, t, ch = x.shape
    xr = x if rows is None else x.reshape(bsz * rows, GRID_W, ch)
    y = lax.conv_general_dilated(xr, w[:, None, :].astype(x.dtype), window_strides=(1,), padding=(CONV_PAD,),
                                 dimension_numbers=("NWC", "WIO", "NWC"), feature_group_count=ch)
    return y.reshape(bsz, t, ch)


def _affine_combine(e1, e2):
    a1, b1 = e1
    a2, b2 = e2
    return a1 * a2, a2 * b1 + b2


def linear_scan(a, b, h0):
    b = b.at[:, 0].add(a[:, 0] * h0)
    return lax.associative_scan(_affine_combine, (a, b), axis=1)[1]


def _complex_affine_combine(e1, e2):
    a1r, a1i, b1r, b1i = e1
    a2r, a2i, b2r, b2i = e2
    return (a1r * a2r - a1i * a2i, a1r * a2i + a1i * a2r,
            a2r * b1r - a2i * b1i + b2r, a2r * b1i + a2i * b1r + b2i)


def rglru_scan(xc, w_gate, b_gate, lam, h0):
    bsz, t, w = xc.shape
    xb = xc.reshape(bsz, t, LRU_BLOCKS, LRU_BLOCK)
    gates = jnp.einsum("btni,gnij->gbtnj", xb, w_gate).reshape(2, bsz, t, w) + b_gate[:, None, None, :]
    r = jax.nn.sigmoid(gates[0])
    i = jax.nn.sigmoid(gates[1])
    log_a = -LRU_C * r * jax.nn.softplus(-lam)
    a = jnp.exp(log_a)
    b = jnp.sqrt(-jnp.expm1(2.0 * log_a)) * (i * xc)
    return linear_scan(a, b, h0)


def rglru_mixer(a_x, a_y, conv_w, conv_b, w_gate, b_gate, lam, h0, rows):
    f32 = jnp.float32
    xc = (dwconv(a_x, conv_w, rows) + conv_b).astype(f32)
    w_gate, b_gate, lam = w_gate.astype(f32), b_gate.astype(f32), lam.astype(f32)
    h_f = rglru_scan(xc, w_gate[0], b_gate[0], lam[0], h0[0])
    h_b = rglru_scan(jnp.flip(xc, 1), w_gate[1], b_gate[1], lam[1], h0[1])
    y = (h_f + jnp.flip(h_b, 1)).astype(a_x.dtype) * jax.nn.gelu(a_y)
    return y, (h_f[:, -1], h_b[:, -1])


def delta_chunk(q, k, v, g, beta, s0):
    bsz, t, nh, _ = q.shape
    n = t // DN_CHUNK

    def to_chunks(u):
        u = u.reshape((bsz, n, DN_CHUNK, nh) + u.shape[3:])
        return jnp.moveaxis(u, 3, 1)

    q, k, v, g, beta = (to_chunks(u) for u in (q, k, v, g, beta))
    dv = v.shape[-1]
    g = jnp.cumsum(g, axis=-1)
    idx = jnp.arange(DN_CHUNK)
    incl = idx[:, None] >= idx[None, :]
    strict = idx[:, None] > idx[None, :]
    decay = jnp.exp(jnp.where(incl, g[..., :, None] - g[..., None, :], -jnp.inf))
    k_beta = k * beta[..., None]
    a_low = jnp.where(strict, jnp.einsum("bhnid,bhnjd->bhnij", k_beta, k) * decay, 0.0)
    eye = jnp.eye(DN_CHUNK, dtype=q.dtype)
    t_inv = lax.linalg.triangular_solve(eye + a_low, jnp.broadcast_to(eye, a_low.shape),
                                        left_side=True, lower=True, unit_diagonal=True)
    u = jnp.einsum("bhnij,bhnjd->bhnid", t_inv, v * beta[..., None])
    w = jnp.einsum("bhnij,bhnjd->bhnid", t_inv, k_beta * jnp.exp(g)[..., None])
    attn = jnp.where(incl, jnp.einsum("bhnid,bhnjd->bhnij", q, k) * decay, 0.0)
    q_dec = q * jnp.exp(g)[..., None]
    k_dec = k * jnp.exp(g[..., -1:] - g)[..., None]
    g_tot = jnp.exp(g[..., -1])

    def step(s, inp):
        u_i, w_i, qd_i, kd_i, at_i, gt_i = inp
        v_new = u_i - jnp.einsum("bhck,bhkv->bhcv", w_i, s)
        o_i = jnp.einsum("bhck,bhkv->bhcv", qd_i, s) + jnp.einsum("bhij,bhjv->bhiv", at_i, v_new)
        s = s * gt_i[..., None, None] + jnp.einsum("bhck,bhcv->bhkv", kd_i, v_new)
        return s, o_i

    xs = tuple(jnp.moveaxis(a, 2, 0) for a in (u, w, q_dec, k_dec, attn, g_tot))
    s_fin, o = lax.scan(step, s0, xs)
    o = jnp.transpose(o, (1, 0, 3, 2, 4)).reshape(bsz, t, nh, dv)
    return o, s_fin


def deltanet_mixer(q, k, v, z, beta_raw, alpha_raw, conv_w, a_log, dt_bias, norm_g, s0, rows):
    f32 = jnp.float32
    bsz, t, _ = q.shape
    qkv = jax.nn.silu(dwconv(jnp.concatenate([q, k, v], axis=-1), conv_w, rows)).astype(f32)

    def heads(u):
        return u.reshape(bsz, t, DN_HEADS, DN_HEAD_DIM)

    q, k, v = (heads(u) for u in jnp.split(qkv, 3, axis=-1))
    q = l2norm(q) * DN_HEAD_DIM ** -0.5
    k = l2norm(k)
    beta = jax.nn.sigmoid(beta_raw.astype(f32)).reshape(bsz, t, 2, DN_HEADS)
    g = -jnp.exp(a_log.astype(f32)) * jax.nn.softplus(
        alpha_raw.astype(f32).reshape(bsz, t, 2, DN_HEADS) + dt_bias.astype(f32))
    o_f, s_f = delta_chunk(q, k, v, g[:, :, 0], beta[:, :, 0], s0[0])
    fl = lambda u: jnp.flip(u, 1)
    o_b, s_b = delta_chunk(fl(q), fl(k), fl(v), fl(g[:, :, 1]), fl(beta[:, :, 1]), s0[1])
    o = o_f + fl(o_b)
    o = rmsnorm(o, norm_g) * jax.nn.silu(heads(z.astype(f32)))
    return o.reshape(bsz, t, DN_WIDTH).astype(z.dtype), (s_f, s_b)


def s5_scan(u, lam_re, lam_im, log_dt, b_re, b_im, c_re, c_im, h0):
    t = u.shape[1]
    dt = jnp.exp(log_dt)[:, None]
    mag = jnp.exp(lam_re * dt)
    ar, ai = mag * jnp.cos(lam_im * dt), mag * jnp.sin(lam_im * dt)
    den = lam_re * lam_re + lam_im * lam_im
    fr = ((ar - 1.0) * lam_re + ai * lam_im) / den
    fi = (ai * lam_re - (ar - 1.0) * lam_im) / den
    bb_re = fr[..., None] * b_re - fi[..., None] * b_im
    bb_im = fr[..., None] * b_im + fi[..., None] * b_re
    xr = jnp.einsum("btgh,gph->btgp", u, bb_re)
    xi = jnp.einsum("btgh,gph->btgp", u, bb_im)
    h0r, h0i = h0
    xr = xr.at[:, 0].add(ar * h0r - ai * h0i)
    xi = xi.at[:, 0].add(ar * h0i + ai * h0r)
    shape = (1, t) + ar.shape
    _, _, hr, hi = lax.associative_scan(
        _complex_affine_combine,
        (jnp.broadcast_to(ar, shape), jnp.broadcast_to(ai, shape), xr, xi), axis=1)
    y = jnp.einsum("btgp,ghp->btgh", hr, c_re) - jnp.einsum("btgp,ghp->btgh", hi, c_im)
    return y, (hr[:, -1], hi[:, -1])


def s5_mixer(u, lam_re, lam_im, log_dt, b_re, b_im, c_re, c_im, d_skip, w_glu, b_glu, h0):
    f32 = jnp.float32
    bsz, t, _ = u.shape
    uf = u.astype(f32)
    ug = uf.reshape(bsz, t, S5_GROUPS, S5_GROUP)
    prm = [a.astype(f32) for a in (lam_re, lam_im, log_dt, b_re, b_im, c_re, c_im)]
    y_f, s_f = s5_scan(ug, *(a[0] for a in prm), h0[0])
    y_b, s_b = s5_scan(jnp.flip(ug, 1), *(a[1] for a in prm), h0[1])
    y = (y_f + jnp.flip(y_b, 1)).reshape(bsz, t, S5_WIDTH) + d_skip.astype(f32) * uf
    y = jax.nn.gelu(y)
    y = y * jax.nn.sigmoid(y @ w_glu.astype(f32) + b_glu.astype(f32))
    return y.astype(u.dtype), (s_f, s_b)


def mix_stream(h, p, init, rows, emit):
    bsz, t, _ = h.shape
    if init is None:
        zeros = lambda *s: jnp.zeros(s, jnp.float32)
        init = ((zeros(bsz, LRU_WIDTH),) * 2,
                (zeros(bsz, DN_HEADS, DN_HEAD_DIM, DN_HEAD_DIM),) * 2,
                ((zeros(bsz, S5_GROUPS, S5_STATE),) * 2,) * 2)
    proj = h @ p["w_in"]
    a_x, a_y, q, k, v, z, beta_raw, alpha_raw, s5_u, gate_raw = jnp.split(
        proj, np.cumsum(IN_SIZES)[:-1].tolist(), axis=-1)
    y_a, st_a = rglru_mixer(a_x, a_y, p["lru_conv_w"], p["lru_conv_b"], p["lru_w_gate"], p["lru_b_gate"],
                            p["lru_lam"], init[0], rows)
    y_b, st_b = deltanet_mixer(q, k, v, z, beta_raw, alpha_raw, p["dn_conv_w"], p["dn_a_log"],
                               p["dn_dt_bias"], p["dn_norm_g"], init[1], rows)
    y_c, st_c = s5_mixer(s5_u, p["s5_lam_re"], p["s5_lam_im"], p["s5_log_dt"], p["s5_b_re"], p["s5_b_im"],
                         p["s5_c_re"], p["s5_c_im"], p["s5_d"], p["s5_w_glu"], p["s5_b_glu"], init[2])
    states = (st_a, st_b, st_c)
    if not emit:
        return None, states
    gates = jax.nn.sigmoid(gate_raw).reshape(bsz, t, N_BRANCH, D_MODEL)
    merged = (gates[..., 0, :] * (y_a @ p["w_br_a"])
              + gates[..., 1, :] * (y_b @ p["w_br_b"])
              + gates[..., 2, :] * (y_c @ p["w_br_c"]))
    return merged @ p["w_out"], states


def moe(h, w_router, b_router, w_e1, b_e1, w_e2, b_e2):
    n, d = h.shape
    nk = n * TOP_K
    logits = (h @ w_router + b_router).astype(jnp.float32)
    top_val, top_idx = lax.top_k(logits, TOP_K)
    weights = jax.nn.softmax(top_val, axis=-1).reshape(-1)
    flat_e = top_idx.reshape(-1)
    order = jnp.argsort(flat_e)
    e_sorted = flat_e[order]
    tok = order // TOP_K
    sizes = jnp.bincount(flat_e, length=N_EXPERTS).astype(jnp.int32)
    padded = ((sizes + MOE_BLOCK - 1) // MOE_BLOCK) * MOE_BLOCK
    ends_pad = jnp.cumsum(padded)
    starts_pad = ends_pad - padded
    starts = jnp.cumsum(sizes) - sizes
    dest = starts_pad[e_sorted] + (jnp.arange(nk, dtype=jnp.int32) - starts[e_sorted])
    n_blocks = -(-nk // MOE_BLOCK) + N_EXPERTS
    block_e = jnp.minimum(jnp.searchsorted(ends_pad, jnp.arange(n_blocks) * MOE_BLOCK, side="right"),
                          N_EXPERTS - 1).astype(jnp.int32)
    xs = jnp.zeros((n_blocks * MOE_BLOCK, d), h.dtype).at[dest].set(h[tok])

    def expert_block(args):
        xb, e = args
        gu = xb @ w_e1[e] + b_e1[e]
        glu, lin = jnp.split(gu, 2, axis=-1)
        glu = jnp.minimum(glu, SWIGLU_LIMIT)
        lin = jnp.clip(lin, -SWIGLU_LIMIT, SWIGLU_LIMIT)
        act = glu * jax.nn.sigmoid(SWIGLU_ALPHA * glu) * (lin + 1.0)
        return act @ w_e2[e] + b_e2[e]

    yb = lax.map(expert_block, (xs.reshape(n_blocks, MOE_BLOCK, d), block_e))
    y = yb.reshape(n_blocks * MOE_BLOCK, d)[dest] * weights[order][:, None].astype(h.dtype)
    return jax.ops.segment_sum(y, tok, num_segments=n)


def setup_inputs(seed: int = 0) -> dict:
    key = jax.random.key(seed)
    keys = list(jax.random.split(key, 48))

    def nrm(shape, std):
        return std * jax.random.normal(keys.pop(), shape, jnp.float32)

    def unif(shape, lo, hi):
        return jax.random.uniform(keys.pop(), shape, jnp.float32, lo, hi)

    L, D = DEPTH, D_MODEL
    G, P, H = S5_GROUPS, S5_STATE, S5_GROUP
    lru_base = unif((L, 2, LRU_WIDTH), 0.9, 0.999) ** (1.0 / LRU_C)
    dn_dt = jnp.exp(unif((L, 2, DN_HEADS), math.log(1e-3), math.log(1e-1)))
    return {
        "x": nrm((BATCH, SEQ, D), 1.0),
        "c": nrm((BATCH, D), 1.0),
        "ctx": nrm((BATCH, CTX_LEN, D), 1.0),
        "c_ctx": nrm((D,), 1.0),
        "w_ada": nrm((L, D, 6 * D), 0.5 * D ** -0.5),
        "b_ada": nrm((L, 6 * D), 0.02),
        "g_mix": 1.0 + nrm((L, D), 0.02),
        "g_ffn": 1.0 + nrm((L, D), 0.02),
        "w_in": nrm((L, D, D_IN), D ** -0.5),
        "lru_conv_w": nrm((L, CONV_W, LRU_WIDTH), CONV_W ** -0.5),
        "lru_conv_b": nrm((L, LRU_WIDTH), 0.02),
        "lru_w_gate": nrm((L, 2, 2, LRU_BLOCKS, LRU_BLOCK, LRU_BLOCK), LRU_BLOCK ** -0.5),
        "lru_b_gate": nrm((L, 2, 2, LRU_WIDTH), 0.02),
        "lru_lam": jnp.log(lru_base) - jnp.log1p(-lru_base),
        "dn_conv_w": nrm((L, CONV_W, 3 * DN_WIDTH), CONV_W ** -0.5),
        "dn_a_log": jnp.log(unif((L, 2, DN_HEADS), 1.0, 16.0)),
        "dn_dt_bias": dn_dt + jnp.log(-jnp.expm1(-dn_dt)),
        "dn_norm_g": 1.0 + nrm((L, DN_HEAD_DIM), 0.02),
        "s5_lam_re": -0.5 + nrm((L, 2, G, P), 0.01),
        "s5_lam_im": math.pi * jnp.arange(P, dtype=jnp.float32) + nrm((L, 2, G, P), 0.01),
        "s5_log_dt": unif((L, 2, G), math.log(1e-3), math.log(1e-1)),
        "s5_b_re": nrm((L, 2, G, P, H), (2 * H) ** -0.5),
        "s5_b_im": nrm((L, 2, G, P, H), (2 * H) ** -0.5),
        "s5_c_re": nrm((L, 2, G, H, P), P ** -0.5),
        "s5_c_im": nrm((L, 2, G, H, P), P ** -0.5),
        "s5_d": nrm((L, S5_WIDTH), 1.0),
        "s5_w_glu": nrm((L, S5_WIDTH, S5_WIDTH), S5_WIDTH ** -0.5),
        "s5_b_glu": nrm((L, S5_WIDTH), 0.02),
        "w_br_a": nrm((L, LRU_WIDTH, D), LRU_WIDTH ** -0.5),
        "w_br_b": nrm((L, DN_WIDTH, D), DN_WIDTH ** -0.5),
        "w_br_c": nrm((L, S5_WIDTH, D), S5_WIDTH ** -0.5),
        "w_out": nrm((L, D, D), D ** -0.5),
        "w_router": nrm((L, D, N_EXPERTS), D ** -0.5),
        "b_router": nrm((L, N_EXPERTS), 0.01),
        "w_e1": nrm((L, N_EXPERTS, D, 2 * D_EXPERT), D ** -0.5),
        "b_e1": nrm((L, N_EXPERTS, 2 * D_EXPERT), 0.01),
        "w_e2": nrm((L, N_EXPERTS, D_EXPERT, D), D_EXPERT ** -0.5),
        "b_e2": nrm((L, N_EXPERTS, D), 0.01),
        "g_final": 1.0 + nrm((D,), 0.02),
    }


def reference(x, c, ctx, c_ctx, w_ada, b_ada, g_mix, g_ffn, w_in, lru_conv_w, lru_conv_b, lru_w_gate,
              lru_b_gate, lru_lam, dn_conv_w, dn_a_log, dn_dt_bias, dn_norm_g, s5_lam_re, s5_lam_im,
              s5_log_dt, s5_b_re, s5_b_im, s5_c_re, s5_c_im, s5_d, s5_w_glu, s5_b_glu, w_br_a, w_br_b,
              w_br_c, w_out, w_router, b_router, w_e1, b_e1, w_e2, b_e2, g_final):
    rows = x.shape[1] // GRID_W
    s_c = jax.nn.silu(c)
    s_ctx = jax.nn.silu(c_ctx)
    x_lat, x_ctx = x, ctx
    for l in range(DEPTH):
        last = l == DEPTH - 1
        p = {
            "w_in": w_in[l], "lru_conv_w": lru_conv_w[l], "lru_conv_b": lru_conv_b[l],
            "lru_w_gate": lru_w_gate[l], "lru_b_gate": lru_b_gate[l], "lru_lam": lru_lam[l],
            "dn_conv_w": dn_conv_w[l], "dn_a_log": dn_a_log[l], "dn_dt_bias": dn_dt_bias[l],
            "dn_norm_g": dn_norm_g[l], "s5_lam_re": s5_lam_re[l], "s5_lam_im": s5_lam_im[l],
            "s5_log_dt": s5_log_dt[l], "s5_b_re": s5_b_re[l], "s5_b_im": s5_b_im[l],
            "s5_c_re": s5_c_re[l], "s5_c_im": s5_c_im[l], "s5_d": s5_d[l], "s5_w_glu": s5_w_glu[l],
            "s5_b_glu": s5_b_glu[l], "w_br_a": w_br_a[l], "w_br_b": w_br_b[l], "w_br_c": w_br_c[l],
            "w_out": w_out[l],
        }
        m_lat = jnp.split((s_c @ w_ada[l] + b_ada[l])[:, None, :], 6, axis=-1)
        m_ctx = jnp.split(s_ctx @ w_ada[l] + b_ada[l], 6, axis=-1)

        h_ctx = modulate(rmsnorm(x_ctx, g_mix[l]), m_ctx[0], m_ctx[1])
        o_ctx, ctx_states = mix_stream(h_ctx, p, None, None, not last)
        h_lat = modulate(rmsnorm(x_lat, g_mix[l]), m_lat[0], m_lat[1])
        o_lat, _ = mix_stream(h_lat, p, ctx_states, rows, True)
        x_lat = x_lat + m_lat[2] * o_lat

        h_lat = modulate(rmsnorm(x_lat, g_ffn[l]), m_lat[3], m_lat[4])
        if last:
            y_lat = moe(h_lat.reshape(-1, D_MODEL), w_router[l], b_router[l], w_e1[l], b_e1[l], w_e2[l], b_e2[l])
            x_lat = x_lat + m_lat[5] * y_lat.reshape(h_lat.shape)
        else:
            x_ctx = x_ctx + m_ctx[2] * o_ctx
            h_ctx = modulate(rmsnorm(x_ctx, g_ffn[l]), m_ctx[3], m_ctx[4])
            n_ctx = h_ctx.shape[0] * h_ctx.shape[1]
            y = moe(jnp.concatenate([h_ctx.reshape(-1, D_MODEL), h_lat.reshape(-1, D_MODEL)], axis=0),
                    w_router[l], b_router[l], w_e1[l], b_e1[l], w_e2[l], b_e2[l])
            x_ctx = x_ctx + m_ctx[5] * y[:n_ctx].reshape(h_ctx.shape)
            x_lat = x_lat + m_lat[5] * y[n_ctx:].reshape(h_lat.shape)
    return rmsnorm(x_lat, g_final)
```

```python
import contextlib
import math
import numpy as np
import concourse.bass as bass
import concourse.mybir as mybir
from concourse.bass_utils import run_bass_kernel_spmd

F32 = mybir.dt.float32
BF16 = mybir.dt.bfloat16
ALU = mybir.AluOpType
AF = mybir.ActivationFunctionType
AX = mybir.AxisListType

D = 1024
SEQ = 2048
CTX = 256
DEPTH = 2
D_IN = 8736
NE = 32
EPS = 1e-6
OFF_AX, OFF_AY, OFF_Q, OFF_K, OFF_V, OFF_Z, OFF_BETA, OFF_ALPHA, OFF_U, OFF_GATE = (
    0, 512, 1024, 2048, 3072, 4096, 5120, 5136, 5152, 5664)

SAME_ENGINE_SYNC = True
NDSEM = 12


class Buf:
    __slots__ = ("w", "r")

    def __init__(self):
        self.w = {}
        self.r = {}


class Prog:
    ENG = ("pe", "act", "dve", "pool", "sp")

    def __init__(self, nc):
        self.nc = nc
        self.ops = {e: [] for e in self.ENG}
        self.cnt = {e: 0 for e in self.ENG}
        self.known = {e: {} for e in self.ENG}
        self.dq = {e: 0 for e in self.ENG}
        self.dval = {}
        self.bufs = {}
        self.st = contextlib.ExitStack()
        self.uid = 0

    def sb(self, name, shape, dtype=F32):
        self.uid += 1
        t = self.st.enter_context(self.nc.sbuf_tensor("%s_%d" % (name, self.uid), list(shape), dtype))
        return t

    def psum(self, name, shape, dtype=F32):
        return self.st.enter_context(self.nc.psum_tensor(name, list(shape), dtype))

    def dram(self, name, shape, dtype=F32, kind="Internal"):
        return self.nc.dram_tensor(name, list(shape), dtype, kind=kind)

    def _buf(self, ap):
        n = ap.tensor.name
        b = self.bufs.get(n)
        if b is None:
            b = self.bufs[n] = Buf()
        return b

    def _sched(self, e, reads, writes, tok_fn):
        d = {}
        rb = [self._buf(a) for a in reads if a is not None and not isinstance(a, (int, float))]
        wb = [self._buf(a) for a in writes]
        for a, b in zip([a for a in reads if a is not None and not isinstance(a, (int, float))], rb):
            for k, v in b.w.items():
                if d.get(k, 0) < v:
                    d[k] = v
            if a.tensor.name.startswith("ps"):
                for k, v in b.r.items():
                    if k != e and d.get(k, 0) < v:
                        d[k] = v
        for b in wb:
            for k, v in b.w.items():
                if d.get(k, 0) < v:
                    d[k] = v
            for k, v in b.r.items():
                if d.get(k, 0) < v:
                    d[k] = v
        return d, rb, wb

    def _waits(self, e, d):
        kn = self.known[e]
        for k, v in d.items():
            if k == e and ((not SAME_ENGINE_SYNC) or e in ("pe", "sp")):
                continue
            if kn.get(k, 0) >= v:
                continue
            kn[k] = v
            self.ops[e].append(("wait", k, v))

    def op(self, e, fn, reads, writes):
        d, rb, wb = self._sched(e, reads, writes, None)
        self._waits(e, d)
        self.cnt[e] += 1
        k, v = e, self.cnt[e]
        self.ops[e].append(("op", fn))
        for b in rb:
            if b.r.get(k, 0) < v:
                b.r[k] = v
        for b in wb:
            b.w[k] = v
            b.r = {}

    def dma(self, q, out, in_, **kw):
        d, rb, wb = self._sched(q, [in_], [out], None)
        j = self.dq[q]
        self.dq[q] = (j + 1) % NDSEM
        key = ("d", q, j)
        prev = self.dval.get(key, 0)
        if prev:
            d[key] = max(d.get(key, 0), prev)
        self._waits(q, d)
        val = prev + 16
        self.dval[key] = val
        self.ops[q].append(("dma", out, in_, key, kw))
        for b in rb:
            if b.r.get(key, 0) < val:
                b.r[key] = val
        for b in wb:
            b.w[key] = val
            b.r = {}

    def mm(self, out, lhsT, rhs, start=True, stop=True):
        self.op("pe", lambda e: e.matmul(out, lhsT=lhsT, rhs=rhs, start=start, stop=stop), [lhsT, rhs], [out])

    def tr(self, out, in_, ident):
        self.op("pe", lambda e: e.transpose(out, in_, ident), [in_, ident], [out])

    def act(self, out, in_, func, bias=None, scale=None, accum_out=None, eng="act"):
        kw = {}
        rd = [in_]
        wr = [out]
        if bias is not None:
            kw["bias"] = bias
            rd.append(bias)
        if scale is not None:
            kw["scale"] = scale
            rd.append(scale)
        if accum_out is not None:
            kw["accum_out"] = accum_out
            wr.append(accum_out)
        self.op("act", lambda e: e.activation(out=out, in_=in_, func=func, **kw), rd, wr)

    def tt(self, out, in0, in1, op, eng="dve"):
        self.op(eng, lambda e: e.tensor_tensor(out=out, in0=in0, in1=in1, op=op), [in0, in1], [out])

    def ts(self, out, in0, s1, s2=None, op0=ALU.mult, op1=None, eng="dve"):
        kw = {}
        if op1 is not None:
            kw["op1"] = op1
        self.op(eng, lambda e: e.tensor_scalar(out=out, in0=in0, scalar1=s1, scalar2=s2, op0=op0, **kw),
                [in0, s1, s2], [out])

    def stt(self, out, in0, scalar, in1, op0, op1, eng="dve"):
        self.op(eng, lambda e: e.scalar_tensor_tensor(out=out, in0=in0, scalar=scalar, in1=in1, op0=op0, op1=op1),
                [in0, scalar, in1], [out])

    def copy(self, out, in_, eng="dve"):
        if eng == "act":
            self.act(out, in_, AF.Copy)
        else:
            self.op(eng, lambda e: e.tensor_copy(out=out, in_=in_), [in_], [out])

    def memset(self, out, val, eng="dve"):
        self.op(eng, lambda e: e.memset(out, val), [], [out])

    def recip(self, out, in_):
        self.op("dve", lambda e: e.reciprocal(out=out, in_=in_), [in_], [out])

    def scan(self, out, d0, d1, init, eng="dve"):
        self.op(eng, lambda e: e.tensor_tensor_scan(out=out, data0=d0, data1=d1, initial=init, op0=ALU.mult,
                                                    op1=ALU.add), [d0, d1, init], [out])

    def barrier(self):
        for e in self.ENG:
            d = {}
            for e2 in self.ENG:
                if e2 != e and self.cnt[e2]:
                    d[e2] = self.cnt[e2]
            for key, val in self.dval.items():
                d[key] = val
            self._waits(e, d)

    def setup(self):
        nc = self.nc
        self.gst = contextlib.ExitStack()
        self.sems = {}
        for e in self.ENG:
            self.sems[e] = self.gst.enter_context(nc.semaphore("s_" + e))
        for q in ("sp", "pool", "act"):
            for j in range(NDSEM):
                self.sems[("d", q, j)] = self.gst.enter_context(nc.semaphore("d_%s_%d" % (q, j)))
        self.st = contextlib.ExitStack()

    def gsb(self, name, shape, dtype=F32):
        return self.gst.enter_context(self.nc.sbuf_tensor(name, list(shape), dtype))

    def gpsum(self, name, shape, dtype=F32):
        return self.gst.enter_context(self.nc.psum_tensor(name, list(shape), dtype))

    def end_phase(self, final=False):
        nc = self.nc
        self.barrier()
        sems = self.sems
        ops = self.ops

        def run(e, eng):
            se = sems[e]
            for o in ops[e]:
                if o[0] == "wait":
                    eng.wait_ge(sems[o[1]], o[2])
                elif o[0] == "op":
                    o[1](eng).then_inc(se, 1)
                else:
                    _, out, in_, key, kw = o
                    eng.dma_start(out=out, in_=in_, **kw).then_inc(sems[key], 16)

        with nc.Block() as block:
            block.sync(lambda eng: run("sp", eng))
            block.tensor(lambda eng: run("pe", eng))
            block.scalar(lambda eng: run("act", eng))
            block.vector(lambda eng: run("dve", eng))
            block.gpsimd(lambda eng: run("pool", eng))
        self.ops = {e: [] for e in self.ENG}
        self.st.close()
        self.st = contextlib.ExitStack()
        if final:
            self.gst.close()


def rev(ap):
    a = [list(x) for x in ap.ap]
    step, n = a[-1]
    a[-1] = [-step, n]
    return bass.AP(ap.tensor, ap.offset + step * (n - 1), a)


def bcast_rows(ap, nparts):
    a = [list(x) for x in ap.ap]
    return bass.AP(ap.tensor, ap.offset, [[0, nparts]] + a[-1:])


WNAMES = ["w_ada", "b_ada", "g_mix", "g_ffn", "w_in", "lru_conv_w", "lru_conv_b", "lru_w_gate", "lru_b_gate",
          "lru_lam", "dn_conv_w", "dn_a_log", "dn_dt_bias", "dn_norm_g", "s5_lam_re", "s5_lam_im", "s5_log_dt",
          "s5_b_re", "s5_b_im", "s5_c_re", "s5_c_im", "s5_d", "s5_w_glu", "s5_b_glu", "w_br_a", "w_br_b", "w_br_c",
          "w_out", "w_router", "b_router", "w_e1", "b_e1", "w_e2", "b_e2", "g_final"]


WSHAPES = {
    "w_ada": [2, 1024, 6144], "b_ada": [2, 6144], "g_mix": [2, 1024], "g_ffn": [2, 1024], "w_in": [2, 1024, 8736],
    "lru_conv_w": [2, 4, 512], "lru_conv_b": [2, 512], "lru_w_gate": [2, 2, 2, 8, 64, 64], "lru_b_gate": [2, 2, 2, 512],
    "lru_lam": [2, 2, 512], "dn_conv_w": [2, 4, 3072], "dn_a_log": [2, 2, 8], "dn_dt_bias": [2, 2, 8],
    "dn_norm_g": [2, 128], "s5_lam_re": [2, 2, 32, 64], "s5_lam_im": [2, 2, 32, 64], "s5_log_dt": [2, 2, 32],
    "s5_b_re": [2, 2, 32, 64, 16], "s5_b_im": [2, 2, 32, 64, 16], "s5_c_re": [2, 2, 32, 16, 64],
    "s5_c_im": [2, 2, 32, 16, 64], "s5_d": [2, 512], "s5_w_glu": [2, 512, 512], "s5_b_glu": [2, 512],
    "w_br_a": [2, 512, 1024], "w_br_b": [2, 1024, 1024], "w_br_c": [2, 512, 1024], "w_out": [2, 1024, 1024],
    "w_router": [2, 1024, 32], "b_router": [2, 32], "w_e1": [2, 32, 1024, 2048], "b_e1": [2, 32, 2048],
    "w_e2": [2, 32, 1024, 1024], "b_e2": [2, 32, 1024], "g_final": [1, 1024],
}
BIG = 30000.0
C_ID, C_ONE, C_LTF, C_LTB, C_MGT, C_MLT, C_SGT, C_SLT, C_IOTA, C_END = 0, 128, 256, 320, 384, 448, 512, 576, 640, 640 + 2048


def make_consts():
    c = np.zeros((128, C_END), np.float32)
    c[:, C_ID:C_ID + 128] = np.eye(128, dtype=np.float32)
    c[:, C_ONE:C_ONE + 128] = 1.0
    p = np.arange(128)[:, None]
    f = np.arange(64)[None, :]
    c[:, C_LTF:C_LTF + 64] = (p <= f) & (p < 64)
    c[:, C_LTB:C_LTB + 64] = (p >= f) & (p < 64)
    c[:, C_MGT:C_MGT + 64] = BIG * (f > p)
    c[:, C_MLT:C_MLT + 64] = BIG * (f < p)
    c[:, C_SGT:C_SGT + 64] = (f > p)
    c[:, C_SLT:C_SLT + 64] = (f < p)
    c[:, C_IOTA:C_IOTA + 2048] = np.arange(2048, dtype=np.float32)[None, :]
    return c


def dap(t, offset, dims):
    return bass.AP(t.tensor if hasattr(t, "tensor") else t, offset, [list(d) for d in dims])


class Ctx:
    pass


def build_program(NB=4, layers=(0, 1), stop_after=None, dbg=(), skip=(), inject=()):
    nc = bass.Bass("TRN2", target_bir_lowering=False)
    p = Prog(nc)
    p.setup()
    NV = NB + 1
    g = Ctx()
    g.NB, g.NV, g.p, g.nc = NB, NV, p, nc
    g.skip = set(skip)
    x_in = nc.dram_tensor("x", [NB * SEQ, D], F32, kind="ExternalInput").ap()
    ctx_in = nc.dram_tensor("ctx", [NB * CTX, D], F32, kind="ExternalInput").ap()
    c_in = nc.dram_tensor("c", [NB, D], F32, kind="ExternalInput").ap()
    cctx_in = nc.dram_tensor("c_ctx", [1, D], F32, kind="ExternalInput").ap()
    consts_in = nc.dram_tensor("consts", [128, C_END], F32, kind="ExternalInput").ap()
    W = {}
    for name in WNAMES:
        W[name] = nc.dram_tensor(name, WSHAPES[name], F32, kind="ExternalInput").ap()
    out = nc.dram_tensor("out", [NB * SEQ, D], F32, kind="ExternalOutput").ap()
    g.W, g.out = W, out

    g.xs = {"lat": p.dram("xs_lat", [NB * SEQ, D]).ap(), "ctx": p.dram("xs_ctx", [NB * CTX, D]).ap()}
    g.T = {"lat": SEQ, "ctx": CTX}
    g.mod = p.dram("mod", [DEPTH, NV, 6 * D]).ap()
    g.proj = {"lat": [p.dram("proj_lat%d" % b, [D_IN, SEQ]).ap() for b in range(NB)],
              "ctx": [p.dram("proj_ctx%d" % b, [D_IN, CTX]).ap() for b in range(NB)]}
    g.ya = {s: p.dram("ya_" + s, [NB, 512, g.T[s]], BF16).ap() for s in ("lat", "ctx")}
    g.yb = {s: p.dram("yb_" + s, [NB, 1024, g.T[s]], BF16).ap() for s in ("lat", "ctx")}
    g.yc = {s: p.dram("yc_" + s, [NB, 512, g.T[s]], BF16).ap() for s in ("lat", "ctx")}
    g.ys5 = {s: p.dram("ys5_" + s, [NB, 512, g.T[s]]).ap() for s in ("lat", "ctx")}
    g.odn = {s: p.dram("odn_" + s, [2, NB, 1024, g.T[s]]).ap() for s in ("lat", "ctx")}
    g.e1bf = [p.dram("e1bf%d" % l_, [NE, D, 2 * D], BF16).ap() for l_ in range(DEPTH)]
    g.e2bf = [p.dram("e2bf%d" % l_, [NE, D, D], BF16).ap() for l_ in range(DEPTH)]

    g.cst = p.gsb("cst", [128, C_END])
    g.cstb = p.gsb("cstb", [128, 256], BF16)
    g.PS = [p.gpsum("ps%d" % i, [128, 1024]) for i in range(4)]
    g.psi = 0
    g.stl = p.gsb("st_lru", [128, NB, 4, 2])
    g.sts5 = p.gsb("st_s5", [128, NB, 2, 16, 2])
    g.hT2 = p.gsb("hT2", [128, 8, MT_], BF16)
    g.Wg = p.gsb("Wg", [128, MT_ // 128, NE])

    g.dumps = set(x for x in dbg if x.startswith("@"))

    def dump(name, ap, dtype=F32):
        if "@" + name not in g.dumps:
            return
        shp = list(ap.shape)
        dst = nc.dram_tensor("dbg_" + name, [shp[0], int(np.prod(shp[1:]))], dtype, kind="ExternalOutput").ap()
        if len(shp) == 3:
            dst = dst.rearrange("p (a b) -> p a b", b=shp[2])
        p.dma("sp", dst, ap)
    g.dump = dump

    def ps():
        t = g.PS[g.psi % 4]
        g.psi += 1
        return t
    g.ps = ps
    g.epsc = p.gsb("epsc", [128, 1])
    p.memset(g.epsc[:, :], EPS)
    g.negpi = p.gsb("negpi", [128, 1])
    p.memset(g.negpi[:, :], -math.pi)
    g.ident = g.cst[:, C_ID:C_ID + 128]
    g.ones = g.cst[:, C_ONE:C_ONE + 128]

    p.dma("sp", g.cst[:, :], consts_in[:, :])
    p.copy(g.cstb[:, :], g.cst[:, 0:256])
    for b in range(NB):
        p.dma("sp", g.xs["lat"][b * SEQ:(b + 1) * SEQ, :], x_in[b * SEQ:(b + 1) * SEQ, :])
    p.dma("sp", g.xs["ctx"][:, :], ctx_in[:, :])
    if "moe" not in (stop_after or ()):
        pass
    g.cast_moe = lambda: None
    p.end_phase()

    def cast_moe(l):
        for e in range(NE):
            for cb in range(4):
                p.dma("pool", g.e1bf[l][e, :, cb * 512:(cb + 1) * 512], W["w_e1"][l, e, :, cb * 512:(cb + 1) * 512])
            for cb in range(2):
                p.dma("pool", g.e2bf[l][e, :, cb * 512:(cb + 1) * 512], W["w_e2"][l, e, :, cb * 512:(cb + 1) * 512])
    g.cast_moe = cast_moe

    for l in layers:
        last = (l == DEPTH - 1)
        phase_adaln(g, l, c_in, cctx_in)
        if stop_after == "adaln":
            break
        for b in range(NB):
            for s in ("ctx", "lat"):
                phase_norm_proj(g, l, b, s)
        if stop_after == "proj":
            break
        if "lru" not in g.skip:
            phase_lru(g, l, last)
        if stop_after == "lru":
            break
        if "s5" not in g.skip:
            phase_s5(g, l, last)
        if stop_after == "s5":
            break
        if "dn" not in g.skip:
            phase_dn(g, l, last)
        if stop_after == "dn":
            break
        phase_merge(g, l, last)
        if stop_after == "merge":
            break
        if "moe" not in g.skip:
            import os
            if not os.environ.get("MOE_NOCAST"):
                g.cast_moe(l)
            phase_moe(g, l, last)
        if stop_after == "moe":
            break
    if stop_after is None:
        phase_final(g)
    srcs = {"mod": g.mod, "proj_lat": g.proj["lat"][0], "proj_ctx": g.proj["ctx"][0], "xs_lat": g.xs["lat"],
            "xs_ctx": g.xs["ctx"]}
    for s_ in ("lat", "ctx"):
        for nm, dd in (("ya", g.ya), ("yb", g.yb), ("yc", g.yc), ("ys5", g.ys5), ("odn", g.odn)):
            srcs[nm + "_" + s_] = dd[s_]
    for name in dbg:
        if name.startswith("@"):
            continue
        src = srcs[name]
        n0 = src.shape[0]
        rest = int(np.prod(src.shape[1:]))
        dst = nc.dram_tensor("dbg_" + name, [n0, rest], src.dtype, kind="ExternalOutput").ap()
        for i in range(n0):
            p.dma("sp", dst[i:i + 1, :], dap(src, src.offset + i * rest, [[rest, 1], [1, rest]]))
    if stop_after is None:
        pass
    p.end_phase(final=True)
    return nc


def load_mod_bcast(g, l, v, idx, name):
    p = g.p
    t = p.sb(name, [128, D])
    src = g.mod[l, v, idx * D:(idx + 1) * D]
    p.dma("sp", t[:, :], dap(src, src.offset, [[0, 128], [1, D]]))
    return t


def phase_adaln(g, l, c_in, cctx_in):
    p, NB, NV, W = g.p, g.NB, g.NV, g.W
    sT = p.sb("ad_sT", [128, 8, NV])
    sTb = p.sb("ad_sTb", [128, 8, NV], BF16)
    for v in range(NB):
        p.dma("sp", sT[:, :, v], dap(c_in, v * D, [[1, 128], [128, 8]]), allow_slow_non_contiguous=True)
    p.dma("sp", sT[:, :, NB], dap(cctx_in, 0, [[1, 128], [128, 8]]), allow_slow_non_contiguous=True)
    p.act(sTb[:, :, :], sT[:, :, :], AF.Silu)
    bias = p.sb("ad_bias", [NV, 6 * D])
    src = W["b_ada"][l, :]
    p.dma("sp", bias[:, :], dap(src, src.offset, [[0, NV], [1, 6 * D]]))
    modsb = p.sb("ad_mod", [NV, 6 * D])
    wts = [p.sb("ad_w%d" % i, [128, 8, 512], BF16) for i in range(2)]
    for ct in range(12):
        wt = wts[ct % 2]
        src = W["w_ada"][l, :, ct * 512:(ct + 1) * 512]
        p.dma("pool", wt[:, :, :], dap(src, src.offset, [[6 * D, 128], [128 * 6 * D, 8], [1, 512]]))
        pt = g.ps()
        for kc in range(8):
            p.mm(pt[0:NV, 0:512], sTb[:, kc, :], wt[:, kc, :], start=(kc == 0), stop=(kc == 7))
        p.tt(modsb[:, ct * 512:(ct + 1) * 512], pt[0:NV, 0:512], bias[:, ct * 512:(ct + 1) * 512], ALU.add)
    p.dma("sp", g.mod[l, :, :], modsb[:, :])
    g.dump("sT", sT[:, :, :])
    g.dump("bias", bias[:, :])
    g.dump("modsb", modsb[:, :])
    p.end_phase()


def phase_norm_proj(g, l, b, s):
    p, NB, W = g.p, g.NB, g.W
    T = g.T[s]
    v = b if s == "lat" else NB
    xs = g.xs[s][b * T:(b + 1) * T, :]
    hT = p.sb("np_hT", [128, 8, T], BF16)
    G1 = load_mod_bcast(g, l, v, 1, "np_G1")
    SH = load_mod_bcast(g, l, v, 0, "np_SH")
    gm = p.sb("np_gm", [128, D])
    src = W["g_mix"][l, :]
    p.dma("sp", gm[:, :], dap(src, src.offset, [[0, 128], [1, D]]))
    p.stt(G1[:, :], G1[:, :], 1.0, gm[:, :], ALU.add, ALU.mult)
    norm_tiles(g, xs, T, G1, SH, hT, None)
    wts = [p.sb("np_w%d" % i, [128, 8, 512], BF16) for i in range(2)]
    stg = [p.sb("np_stg%d" % i, [128, 512]) for i in range(4)]
    si = 0
    ngrp = (D_IN + 511) // 512
    TT = min(T, 512)
    for og in range(ngrp):
        c0 = og * 512
        ncol = min(512, D_IN - c0)
        wt = wts[og % 2]
        src = W["w_in"][l, :, c0:c0 + ncol]
        p.dma("pool", wt[:, :, 0:ncol], dap(src, src.offset, [[D_IN, 128], [128 * D_IN, 8], [1, ncol]]))
        for oc in range((ncol + 127) // 128):
            m = min(128, ncol - oc * 128)
            for tt in range(T // TT):
                pt = g.ps()
                for kc in range(8):
                    p.mm(pt[0:m, 0:TT], wt[:, kc, oc * 128:oc * 128 + m], hT[:, kc, tt * TT:(tt + 1) * TT],
                         start=(kc == 0), stop=(kc == 7))
                sg = stg[si % 4]
                p.copy(sg[0:m, 0:TT], pt[0:m, 0:TT], eng=("act" if si % 2 else "dve"))
                si += 1
                r0 = c0 + oc * 128
                p.dma("sp", g.proj[s][b][r0:r0 + m, tt * TT:(tt + 1) * TT], sg[0:m, 0:TT])
    p.end_phase()


def norm_tiles(g, xs, T, G1, SH, hT, h32cb):
    p = g.p
    xts = [p.sb("nt_x%d" % i, [128, D]) for i in range(2)]
    sq = p.sb("nt_sq", [128, D])
    hN = [p.sb("nt_h%d" % i, [128, D]) for i in range(2)]
    ss = p.sb("nt_ss", [128, 4])
    h32 = [p.sb("nt_h32_%d" % i, [128, 8, 128]) for i in range(2)] if h32cb else None
    for tt in range(T // 128):
        xt = xts[tt % 2]
        hn = hN[tt % 2]
        p.dma("sp", xt[:, :], xs[tt * 128:(tt + 1) * 128, :])
        p.act(sq[:, :], xt[:, :], AF.Square)
        p.op("dve", lambda e, o=ss[:, 0:1], i=sq[:, :]: e.reduce_sum(out=o, in_=i, axis=AX.X), [sq[:, :]], [ss[:, 0:1]])
        p.ts(ss[:, 1:2], ss[:, 0:1], 1.0 / D, EPS, op0=ALU.mult, op1=ALU.add)
        p.act(ss[:, 2:3], ss[:, 1:2], AF.Sqrt)
        p.recip(ss[:, 3:4], ss[:, 2:3])
        p.stt(hn[:, :], xt[:, :], ss[:, 3:4], G1[:, :], ALU.mult, ALU.mult)
        p.tt(hn[:, :], hn[:, :], SH[:, :], ALU.add)
        for half in range(2):
            pt = g.ps()
            for k4 in range(4):
                kc = half * 4 + k4
                p.tr(pt[:, k4 * 128:(k4 + 1) * 128], hn[:, kc * 128:(kc + 1) * 128], g.ident)
            src = pt[:, 0:512].rearrange("p (a b) -> p a b", b=128)
            p.act(hT[:, half * 4:half * 4 + 4, tt * 128:(tt + 1) * 128], src, AF.Copy)
            if h32cb:
                p.copy(h32[tt % 2][:, half * 4:half * 4 + 4, :], src)
        if h32cb:
            h32cb(tt, h32[tt % 2])


def phase_lru(g, l, last):
    p, NB, W = g.p, g.NB, g.W
    Wbd = p.sb("lr_Wbd", [128, 2, 2, 4, 128])
    p.memset(Wbd[:, :, :, :, :], 0.0)
    for d in range(2):
        for gg in range(2):
            for hh in range(2):
                off = W["lru_w_gate"][l, d, gg, hh, 0, 0].offset if False else (((l * 2 + d) * 2 + gg) * 8 + hh) * 4096
                p.dma("sp", Wbd[hh * 64:(hh + 1) * 64, d, gg, :, hh * 64:(hh + 1) * 64],
                      dap(W["lru_w_gate"], off, [[64, 64], [2 * 4096, 4], [1, 64]]))
    lam = p.sb("lr_lam", [128, 2, 4])
    cv = p.sb("lr_cv", [128, 2, 4])
    bg = p.sb("lr_bg", [128, 2, 2, 4])
    cw = p.sb("lr_cw", [128, 4, 4])
    cb = p.sb("lr_cb", [128, 4])
    p.dma("sp", lam[:, :, :], dap(W["lru_lam"], l * 1024, [[1, 128], [512, 2], [128, 4]]), allow_slow_non_contiguous=True)
    for d in range(2):
        p.dma("sp", bg[:, d, :, :], dap(W["lru_b_gate"], (l * 2 + d) * 1024, [[1, 128], [512, 2], [128, 4]]),
              allow_slow_non_contiguous=True)
    p.dma("sp", cw[:, :, :], dap(W["lru_conv_w"], l * 2048, [[1, 128], [512, 4], [128, 4]]), allow_slow_non_contiguous=True)
    p.dma("sp", cb[:, :], dap(W["lru_conv_b"], l * 512, [[1, 128], [128, 4]]), allow_slow_non_contiguous=True)
    p.act(cv[:, :, :], lam[:, :, :], AF.Exp, scale=-1.0)
    p.act(cv[:, :, :], cv[:, :, :], AF.Ln, bias=1.0)
    p.ts(cv[:, :, :], cv[:, :, :], -8.0, None, op0=ALU.mult)
    TM = SEQ
    ax = p.sb("lr_ax", [128, TM])
    xc = p.sb("lr_xc", [128, TM])
    ay = p.sb("lr_ay", [128, TM])
    rt = p.sb("lr_r", [128, TM])
    it = p.sb("lr_i", [128, TM])
    at = p.sb("lr_a", [128, TM])
    a2 = p.sb("lr_a2", [128, TM])
    bt = p.sb("lr_b", [128, TM])
    hd = [p.sb("lr_h%d" % d, [128, TM]) for d in range(2)]
    yo = p.sb("lr_y", [128, TM], BF16)
    for b in range(NB):
        for s in ("ctx", "lat"):
            T = g.T[s]
            L = 64 if s == "lat" else T
            TT = min(T, 512)
            emit = not (last and s == "ctx")
            for ch in range(4):
                p.dma("sp", ax[:, 0:T], g.proj[s][b][OFF_AX + ch * 128:OFF_AX + (ch + 1) * 128, :])
                if emit:
                    p.dma("sp", ay[:, 0:T], g.proj[s][b][OFF_AY + ch * 128:OFF_AY + (ch + 1) * 128, :])
                a3 = ax[:, 0:T].rearrange("p (r l) -> p r l", l=L)
                x3 = xc[:, 0:T].rearrange("p (r l) -> p r l", l=L)
                p.ts(xc[:, 0:T], ax[:, 0:T], cw[:, 2, ch:ch + 1], cb[:, ch:ch + 1], op0=ALU.mult, op1=ALU.add)
                p.stt(x3[:, :, 2:L], a3[:, :, 0:L - 2], cw[:, 0, ch:ch + 1], x3[:, :, 2:L], ALU.mult, ALU.add)
                p.stt(x3[:, :, 1:L], a3[:, :, 0:L - 1], cw[:, 1, ch:ch + 1], x3[:, :, 1:L], ALU.mult, ALU.add)
                p.stt(x3[:, :, 0:L - 1], a3[:, :, 1:L], cw[:, 3, ch:ch + 1], x3[:, :, 0:L - 1], ALU.mult, ALU.add)
                for d in range(2):
                    for tt in range(T // TT):
                        sl = slice(tt * TT, (tt + 1) * TT)
                        pr = g.ps()
                        p.mm(pr[:, 0:TT], Wbd[:, d, 0, ch, :], xc[:, sl])
                        p.mm(pr[:, 512:512 + TT], Wbd[:, d, 1, ch, :], xc[:, sl])
                        p.act(rt[:, sl], pr[:, 0:TT], AF.Sigmoid, bias=bg[:, d, 0, ch:ch + 1])
                        p.act(it[:, sl], pr[:, 512:512 + TT], AF.Sigmoid, bias=bg[:, d, 1, ch:ch + 1])
                    p.act(at[:, 0:T], rt[:, 0:T], AF.Exp, scale=cv[:, d, ch:ch + 1])
                    p.tt(a2[:, 0:T], at[:, 0:T], at[:, 0:T], ALU.mult, eng="pool")
                    p.act(a2[:, 0:T], a2[:, 0:T], AF.Sqrt, scale=-1.0, bias=1.0)
                    p.tt(bt[:, 0:T], it[:, 0:T], xc[:, 0:T], ALU.mult)
                    p.tt(bt[:, 0:T], bt[:, 0:T], a2[:, 0:T], ALU.mult)
                    init = 0.0 if s == "ctx" else g.stl[:, b, ch, d:d + 1]
                    h = hd[d]
                    if d == 0:
                        p.scan(h[:, 0:T], at[:, 0:T], bt[:, 0:T], init)
                    else:
                        p.scan(rev(h[:, 0:T]), rev(at[:, 0:T]), rev(bt[:, 0:T]), init)
                    if s == "ctx":
                        col = T - 1 if d == 0 else 0
                        p.copy(g.stl[:, b, ch, d:d + 1], h[:, col:col + 1])
                if emit:
                    p.act(ay[:, 0:T], ay[:, 0:T], AF.Gelu_apprx_tanh)
                    p.tt(hd[0][:, 0:T], hd[0][:, 0:T], hd[1][:, 0:T], ALU.add)
                    p.tt(yo[:, 0:T], hd[0][:, 0:T], ay[:, 0:T], ALU.mult)
                    p.dma("sp", g.ya[s][b, ch * 128:(ch + 1) * 128, :], yo[:, 0:T])
    p.end_phase()


def ins(ap, axis, n):
    a = [list(x) for x in ap.ap]
    a.insert(axis, [0, n])
    return bass.AP(ap.tensor, ap.offset, a)


PI = math.pi


def phase_s5(g, l, last):
    p, NB, W = g.p, g.NB, g.W
    TA = CTX + SEQ
    seqs = (("ctx", 0, CTX), ("lat", CTX, SEQ))
    iota = g.cst[:, C_IOTA:C_IOTA + SEQ]
    M = p.sb("s5_M", [128, 4, 8])
    p.memset(M[:, :, :], 0.0)
    for rr in range(4):
        p.memset(M[0:64, rr, 2 * rr:2 * rr + 1], 1.0)
        p.memset(M[64:128, rr, 2 * rr + 1:2 * rr + 2], 1.0)
    prm = []
    for d in range(2):
        t = {}
        for nm in ("lre", "lim", "dt", "mag", "th", "cth", "sth", "ar", "ai", "fr", "fi", "t0", "t1", "t2"):
            t[nm] = p.sb("s5_%s%d" % (nm, d), [128, 16])
        base = (l * 2 + d) * 32 * 64
        for gg in range(2):
            p.dma("sp", t["lre"][gg * 64:(gg + 1) * 64, :], dap(W["s5_lam_re"], base + gg * 64, [[1, 64], [128, 16]]),
                  allow_slow_non_contiguous=True)
            p.dma("sp", t["lim"][gg * 64:(gg + 1) * 64, :], dap(W["s5_lam_im"], base + gg * 64, [[1, 64], [128, 16]]),
                  allow_slow_non_contiguous=True)
            p.dma("sp", t["dt"][gg * 64:(gg + 1) * 64, :], dap(W["s5_log_dt"], (l * 2 + d) * 32 + gg, [[0, 64], [2, 16]]),
                  allow_slow_non_contiguous=True)
        p.act(t["dt"][:, :], t["dt"][:, :], AF.Exp)
        p.tt(t["t0"][:, :], t["lre"][:, :], t["dt"][:, :], ALU.mult)
        p.act(t["mag"][:, :], t["t0"][:, :], AF.Exp)
        p.tt(t["th"][:, :], t["lim"][:, :], t["dt"][:, :], ALU.mult)
        ki = p.sb("s5_ki%d" % d, [128, 16], mybir.dt.int32)
        p.ts(t["t0"][:, :], t["th"][:, :], 1.0 / (2 * PI), None, op0=ALU.mult)
        p.copy(ki[:, :], t["t0"][:, :])
        p.copy(t["t1"][:, :], ki[:, :])
        p.stt(t["t0"][:, :], t["t1"][:, :], -2 * PI, t["th"][:, :], ALU.mult, ALU.add)
        p.ts(t["t1"][:, :], t["t0"][:, :], PI, None, op0=ALU.is_gt)
        p.stt(t["t0"][:, :], t["t1"][:, :], -2 * PI, t["t0"][:, :], ALU.mult, ALU.add)
        p.ts(t["t1"][:, :], t["t0"][:, :], -PI, None, op0=ALU.is_lt)
        p.stt(t["t0"][:, :], t["t1"][:, :], 2 * PI, t["t0"][:, :], ALU.mult, ALU.add)
        p.act(t["sth"][:, :], t["t0"][:, :], AF.Sin)
        p.ts(t["t0"][:, :], t["t0"][:, :], 0.5 * PI, None, op0=ALU.add)
        p.ts(t["t1"][:, :], t["t0"][:, :], PI, None, op0=ALU.is_gt)
        p.stt(t["t0"][:, :], t["t1"][:, :], -2 * PI, t["t0"][:, :], ALU.mult, ALU.add)
        p.act(t["cth"][:, :], t["t0"][:, :], AF.Sin)
        p.tt(t["ar"][:, :], t["mag"][:, :], t["cth"][:, :], ALU.mult)
        p.tt(t["ai"][:, :], t["mag"][:, :], t["sth"][:, :], ALU.mult)
        p.tt(t["t0"][:, :], t["lre"][:, :], t["lre"][:, :], ALU.mult)
        p.tt(t["t1"][:, :], t["lim"][:, :], t["lim"][:, :], ALU.mult)
        p.tt(t["t0"][:, :], t["t0"][:, :], t["t1"][:, :], ALU.add)
        p.recip(t["t0"][:, :], t["t0"][:, :])
        p.ts(t["t1"][:, :], t["ar"][:, :], -1.0, None, op0=ALU.add)
        p.tt(t["fr"][:, :], t["t1"][:, :], t["lre"][:, :], ALU.mult)
        p.tt(t["t2"][:, :], t["ai"][:, :], t["lim"][:, :], ALU.mult)
        p.tt(t["fr"][:, :], t["fr"][:, :], t["t2"][:, :], ALU.add)
        p.tt(t["fr"][:, :], t["fr"][:, :], t["t0"][:, :], ALU.mult)
        p.tt(t["fi"][:, :], t["ai"][:, :], t["lre"][:, :], ALU.mult)
        p.tt(t["t2"][:, :], t["t1"][:, :], t["lim"][:, :], ALU.mult)
        p.tt(t["fi"][:, :], t["fi"][:, :], t["t2"][:, :], ALU.subtract)
        p.tt(t["fi"][:, :], t["fi"][:, :], t["t0"][:, :], ALU.mult)
        prm.append(t)
    dsk = p.sb("s5_dsk", [128, 4])
    p.dma("sp", dsk[:, :], dap(W["s5_d"], l * 512, [[1, 128], [128, 4]]), allow_slow_non_contiguous=True)
    uc = p.sb("s5_u", [128, NB, TA])
    yacc = p.sb("s5_y", [128, NB, TA])
    cosT = p.sb("s5_cos", [128, SEQ])
    sinT = p.sb("s5_sin", [128, SEQ])
    tmp = p.sb("s5_tmp", [128, SEQ])
    xr = p.sb("s5_xr", [128, SEQ])
    xi = p.sb("s5_xi", [128, SEQ])
    wr = p.sb("s5_wr", [128, SEQ])
    wi = p.sb("s5_wi", [128, SEQ])
    t3 = p.sb("s5_t3", [128, SEQ])
    t4 = p.sb("s5_t4", [128, SEQ])
    braw = [p.sb("s5_braw%d" % i, [128, 4, 16]) for i in range(2)]
    craw = [p.sb("s5_craw%d" % i, [128, 4, 16]) for i in range(2)]
    bbs = [p.sb("s5_bbs%d" % i, [128, 4, 16]) for i in range(2)]
    tb = p.sb("s5_tb", [128, 4, 16])
    E = p.sb("s5_E", [128, 8, 16])
    BbT = p.sb("s5_BbT", [128, 2, 4, 2, 128])
    CT = p.sb("s5_CT", [128, 2, 4, 2, 8, 16])
    ini = p.sb("s5_ini", [128, 4])
    cs2 = p.sb("s5_cs2", [128, 8])
    for c in range(4):
        for d in range(2):
            t = prm[d]
            base = (l * 2 + d) * 32 * 1024
            for gg in range(2):
                for r4 in range(4):
                    for (dst, nm) in ((braw[0], "s5_b_re"), (braw[1], "s5_b_im")):
                        p.dma("sp", dst[gg * 64:(gg + 1) * 64, r4, :],
                              dap(W[nm], base + (8 * c + 2 * r4 + gg) * 1024, [[16, 64], [1, 16]]))
                    for (dst, nm) in ((craw[0], "s5_c_re"), (craw[1], "s5_c_im")):
                        p.dma("sp", dst[gg * 64:(gg + 1) * 64, r4, :],
                              dap(W[nm], base + (8 * c + 2 * r4 + gg) * 1024, [[1, 64], [64, 16]]),
                              allow_slow_non_contiguous=True)
            frb = ins(t["fr"][:, 4 * c:4 * c + 4], 2, 16)
            fib = ins(t["fi"][:, 4 * c:4 * c + 4], 2, 16)
            p.tt(bbs[0][:, :, :], braw[0][:, :, :], frb, ALU.mult)
            p.tt(tb[:, :, :], braw[1][:, :, :], fib, ALU.mult)
            p.tt(bbs[0][:, :, :], bbs[0][:, :, :], tb[:, :, :], ALU.subtract)
            p.tt(bbs[1][:, :, :], braw[1][:, :, :], frb, ALU.mult)
            p.tt(tb[:, :, :], braw[0][:, :, :], fib, ALU.mult)
            p.tt(bbs[1][:, :, :], bbs[1][:, :, :], tb[:, :, :], ALU.add)
            for r4 in range(4):
                mk = ins(M[:, r4, :], 2, 16)
                for ri in range(2):
                    p.tt(E[:, :, :], ins(bbs[ri][:, r4, :], 1, 8), mk, ALU.mult)
                    pt = g.ps()
                    p.tr(pt[:, 0:128], E[:, :, :].rearrange("p a b -> p (a b)"), g.ident)
                    p.copy(BbT[:, d, r4, ri, :], pt[:, 0:128])
                p.tt(CT[:, d, r4, 0, :, :], ins(craw[0][:, r4, :], 1, 8), mk, ALU.mult)
                p.stt(CT[:, d, r4, 1, :, :], ins(craw[1][:, r4, :], 1, 8), -1.0, mk, ALU.mult, ALU.mult)
        for b in range(NB):
            for (s, o, T) in seqs:
                p.dma("sp", uc[:, b, o:o + T], g.proj[s][b][OFF_U + c * 128:OFF_U + (c + 1) * 128, :])
        p.ts(yacc[:, :, :], uc[:, :, :], dsk[:, c:c + 1], None, op0=ALU.mult)
        for d in range(2):
            t = prm[d]
            for r4 in range(4):
                r = 4 * c + r4
                p.memset(cosT[:, 0:1], 1.0)
                p.memset(sinT[:, 0:1], 0.0)
                p.copy(cs2[:, 0:1], t["cth"][:, r:r + 1])
                p.copy(cs2[:, 1:2], t["sth"][:, r:r + 1])
                n_ = 1
                while n_ < SEQ:
                    p.ts(cs2[:, 2:3], cs2[:, 1:2], -1.0, None, op0=ALU.mult)
                    p.ts(cosT[:, n_:2 * n_], cosT[:, 0:n_], cs2[:, 0:1], None, op0=ALU.mult)
                    p.stt(cosT[:, n_:2 * n_], sinT[:, 0:n_], cs2[:, 2:3], cosT[:, n_:2 * n_], ALU.mult, ALU.add)
                    p.ts(sinT[:, n_:2 * n_], sinT[:, 0:n_], cs2[:, 0:1], None, op0=ALU.mult)
                    p.stt(sinT[:, n_:2 * n_], cosT[:, 0:n_], cs2[:, 1:2], sinT[:, n_:2 * n_], ALU.mult, ALU.add)
                    n_ *= 2
                    if n_ < SEQ:
                        p.tt(cs2[:, 3:4], cs2[:, 0:1], cs2[:, 1:2], ALU.mult)
                        p.tt(cs2[:, 4:5], cs2[:, 0:1], cs2[:, 0:1], ALU.mult)
                        p.tt(cs2[:, 5:6], cs2[:, 1:2], cs2[:, 1:2], ALU.mult)
                        p.tt(cs2[:, 0:1], cs2[:, 4:5], cs2[:, 5:6], ALU.subtract)
                        p.ts(cs2[:, 1:2], cs2[:, 3:4], 2.0, None, op0=ALU.mult)
                magb = ins(t["mag"][:, r:r + 1], 1, SEQ)
                for b in range(NB):
                    for (s, o, T) in seqs:
                        TT = min(T, 512)
                        fw = (d == 0)
                        cs = cosT[:, 0:T] if fw else rev(cosT[:, 0:T])
                        sn = sinT[:, 0:T] if fw else rev(sinT[:, 0:T])
                        for tt in range(T // TT):
                            sl = slice(tt * TT, (tt + 1) * TT)
                            pt = g.ps()
                            p.mm(pt[:, 0:TT], BbT[:, d, r4, 0, :], uc[:, b, o + tt * TT:o + (tt + 1) * TT])
                            p.mm(pt[:, 512:512 + TT], BbT[:, d, r4, 1, :], uc[:, b, o + tt * TT:o + (tt + 1) * TT])
                            p.act(xr[:, sl], pt[:, 0:TT], AF.Copy)
                            p.act(xi[:, sl], pt[:, 512:512 + TT], AF.Copy)
                        X, Y = xr[:, 0:T], xi[:, 0:T]
                        p.tt(wr[:, 0:T], X, cs, ALU.mult)
                        p.tt(t3[:, 0:T], Y, sn, ALU.mult, eng="pool")
                        p.tt(wr[:, 0:T], wr[:, 0:T], t3[:, 0:T], ALU.add)
                        p.tt(wi[:, 0:T], Y, cs, ALU.mult, eng="pool")
                        p.tt(t4[:, 0:T], X, sn, ALU.mult)
                        p.tt(wi[:, 0:T], wi[:, 0:T], t4[:, 0:T], ALU.subtract, eng="pool")
                        if s == "ctx":
                            ir, ii = 0.0, 0.0
                        else:
                            h0 = g.sts5[:, b, d, r, :]
                            p.tt(ini[:, 0:1], h0[:, 0:1], t["cth"][:, r:r + 1], ALU.mult)
                            p.tt(ini[:, 1:2], h0[:, 1:2], t["sth"][:, r:r + 1], ALU.mult)
                            p.tt(ini[:, 0:1], ini[:, 0:1], ini[:, 1:2], ALU.subtract)
                            p.tt(ini[:, 2:3], h0[:, 0:1], t["sth"][:, r:r + 1], ALU.mult)
                            p.tt(ini[:, 3:4], h0[:, 1:2], t["cth"][:, r:r + 1], ALU.mult)
                            p.tt(ini[:, 2:3], ini[:, 2:3], ini[:, 3:4], ALU.add)
                            ir, ii = ini[:, 0:1], ini[:, 2:3]
                        mb = ins(t["mag"][:, r:r + 1], 1, T)
                        mb = dap(t["mag"], t["mag"][:, r:r + 1].offset, [list(t["mag"][:, r:r + 1].ap[0]), [0, T]])
                        if fw:
                            p.scan(xr[:, 0:T], mb, wr[:, 0:T], ir)
                            p.scan(xi[:, 0:T], mb, wi[:, 0:T], ii)
                        else:
                            p.scan(rev(xr[:, 0:T]), mb, rev(wr[:, 0:T]), ir)
                            p.scan(rev(xi[:, 0:T]), mb, rev(wi[:, 0:T]), ii)
                        p.tt(wr[:, 0:T], X, cs, ALU.mult)
                        p.tt(t3[:, 0:T], Y, sn, ALU.mult, eng="pool")
                        p.tt(wr[:, 0:T], wr[:, 0:T], t3[:, 0:T], ALU.subtract)
                        p.tt(wi[:, 0:T], Y, cs, ALU.mult, eng="pool")
                        p.tt(t4[:, 0:T], X, sn, ALU.mult)
                        p.tt(wi[:, 0:T], wi[:, 0:T], t4[:, 0:T], ALU.add, eng="pool")
                        if s == "ctx":
                            col = T - 1 if fw else 0
                            p.copy(g.sts5[:, b, d, r, 0:1], wr[:, col:col + 1])
                            p.copy(g.sts5[:, b, d, r, 1:2], wi[:, col:col + 1])
                        if last and s == "ctx":
                            continue
                        for tt in range(T // TT):
                            sl = slice(tt * TT, (tt + 1) * TT)
                            pt = g.ps()
                            p.mm(pt[:, 0:TT], CT[:, d, r4, 0, :, :].rearrange("p a b -> p (a b)"), wr[:, sl],
                                 start=True, stop=False)
                            p.mm(pt[:, 0:TT], CT[:, d, r4, 1, :, :].rearrange("p a b -> p (a b)"), wi[:, sl],
                                 start=False, stop=True)
                            ya = yacc[:, b, o + tt * TT:o + (tt + 1) * TT]
                            p.tt(ya, ya, pt[:, 0:TT], ALU.add)
        p.act(yacc[:, :, :], yacc[:, :, :], AF.Gelu_apprx_tanh)
        for b in range(NB):
            for (s, o, T) in seqs:
                if last and s == "ctx":
                    continue
                p.dma("sp", g.ys5[s][b, c * 128:(c + 1) * 128, :], yacc[:, b, o:o + T])
    p.end_phase()
    wg = p.sb("s5_wg", [128, 4, 512], BF16)
    p.dma("pool", wg[:, :, :], dap(W["s5_w_glu"], l * 512 * 512, [[512, 128], [128 * 512, 4], [1, 512]]))
    bgl = p.sb("s5_bgl", [128, 4])
    p.dma("sp", bgl[:, :], dap(W["s5_b_glu"], l * 512, [[1, 128], [128, 4]]), allow_slow_non_contiguous=True)
    yg = [p.sb("s5_yg%d" % i, [128, 4, 512]) for i in range(2)]
    ygb = [p.sb("s5_ygb%d" % i, [128, 4, 512], BF16) for i in range(2)]
    sg = [p.sb("s5_sg%d" % i, [128, 512]) for i in range(2)]
    yo = [p.sb("s5_yo%d" % i, [128, 512], BF16) for i in range(2)]
    it = 0
    for b in range(NB):
        for (s, o, T) in seqs:
            if last and s == "ctx":
                continue
            TT = min(T, 512)
            for tt in range(T // TT):
                y_, yb_ = yg[it % 2], ygb[it % 2]
                it += 1
                src = g.ys5[s][b, :, tt * TT:(tt + 1) * TT]
                p.dma("sp", y_[:, :, 0:TT], dap(src, src.offset, [[T, 128], [128 * T, 4], [1, TT]]))
                p.copy(yb_[:, :, 0:TT], y_[:, :, 0:TT], eng="pool")
                for oc in range(4):
                    pt = g.ps()
                    for kc in range(4):
                        p.mm(pt[:, 0:TT], wg[:, kc, oc * 128:(oc + 1) * 128], yb_[:, kc, 0:TT], start=(kc == 0),
                             stop=(kc == 3))
                    s_, o_ = sg[oc % 2], yo[oc % 2]
                    p.act(s_[:, 0:TT], pt[:, 0:TT], AF.Sigmoid, bias=bgl[:, oc:oc + 1])
                    p.tt(o_[:, 0:TT], y_[:, oc, 0:TT], s_[:, 0:TT], ALU.mult)
                    p.dma("sp", g.yc[s][b, oc * 128:(oc + 1) * 128, tt * TT:(tt + 1) * TT], o_[:, 0:TT])
    p.end_phase()


def phase_dn(g, l, last):
    p, NB, W = g.p, g.NB, g.W
    seqs = (("ctx", CTX), ("lat", SEQ))
    ident64 = g.cst[0:64, C_ID:C_ID + 64]
    ones = g.ones
    cwd = p.sb("dn_cw", [128, 4, 24])
    p.dma("sp", cwd[:, :, :], dap(W["dn_conv_w"], l * 4 * 3072, [[1, 128], [3072, 4], [128, 24]]),
          allow_slow_non_contiguous=True)
    raw = [p.sb("dn_raw%d" % i, [128, SEQ]) for i in range(2)]
    xc = [p.sb("dn_xc%d" % i, [128, SEQ]) for i in range(2)]
    sq = p.sb("dn_sq", [128, SEQ])
    rn = p.sb("dn_rn", [128, SEQ])
    it = 0
    for b in range(NB):
        for (s, T) in seqs:
            L = 64 if s == "lat" else T
            TT = min(T, 512)
            for j in range(24):
                rw, x_ = raw[it % 2], xc[it % 2]
                it += 1
                rows = g.proj[s][b][OFF_Q + j * 128:OFF_Q + (j + 1) * 128, :]
                p.dma("sp", rw[:, 0:T], rows)
                a3 = rw[:, 0:T].rearrange("p (r l) -> p r l", l=L)
                x3 = x_[:, 0:T].rearrange("p (r l) -> p r l", l=L)
                p.ts(x_[:, 0:T], rw[:, 0:T], cwd[:, 2, j:j + 1], None, op0=ALU.mult)
                p.stt(x3[:, :, 2:L], a3[:, :, 0:L - 2], cwd[:, 0, j:j + 1], x3[:, :, 2:L], ALU.mult, ALU.add)
                p.stt(x3[:, :, 1:L], a3[:, :, 0:L - 1], cwd[:, 1, j:j + 1], x3[:, :, 1:L], ALU.mult, ALU.add)
                p.stt(x3[:, :, 0:L - 1], a3[:, :, 1:L], cwd[:, 3, j:j + 1], x3[:, :, 0:L - 1], ALU.mult, ALU.add)
                p.act(x_[:, 0:T], x_[:, 0:T], AF.Silu)
                if j < 16:
                    p.tt(sq[:, 0:T], x_[:, 0:T], x_[:, 0:T], ALU.mult, eng="pool")
                    for tt in range(T // TT):
                        sl = slice(tt * TT, (tt + 1) * TT)
                        pt = g.ps()
                        p.mm(pt[:, 0:TT], ones, sq[:, sl])
                        p.act(rn[:, sl], pt[:, 0:TT], AF.Sqrt, bias=g.epsc[:, 0:1])
                    p.recip(rn[:, 0:T], rn[:, 0:T])
                    sc = (128.0 ** -0.5) if j < 8 else 1.0
                    p.stt(x_[:, 0:T], x_[:, 0:T], sc, rn[:, 0:T], ALU.mult, ALU.mult)
                p.dma("sp", rows, x_[:, 0:T])
    p.end_phase()
    CB = 4
    NCH = SEQ // 64
    alg = p.sb("dn_alg", [64, 16])
    dtb = p.sb("dn_dtb", [64, 16])
    p.dma("sp", alg[:, :], dap(W["dn_a_log"], l * 16, [[0, 64], [1, 16]]))
    p.dma("sp", dtb[:, :], dap(W["dn_dt_bias"], l * 16, [[0, 64], [1, 16]]))
    p.act(alg[:, :], alg[:, :], AF.Exp)
    p.ts(alg[:, :], alg[:, :], -1.0, None, op0=ALU.mult)
    bt = p.sb("dn_bt", [64, NCH, 32])
    bet = p.sb("dn_bet", [64, NCH, 16])
    gt = p.sb("dn_gt", [64, NCH, 16])
    qkv = [[p.sb("dn_%s%d" % (nm, i), [128, 8, CB * 64]) for nm in "qkv"] for i in range(2)]
    S8 = p.sb("dn_S", [128, 8, 128])
    stdn = p.sb("dn_st", [128, 2, 8, 128])
    sm = p.sb("dn_sm", [64, 4, 8])
    gtot = p.sb("dn_gtot", [128, 8])
    gL = p.sb("dn_gL", [64, 8, 64])
    X = p.sb("dn_X", [64, 8, 64])
    D8 = p.sb("dn_D8", [64, 8, 64])
    DT8 = p.sb("dn_DT8", [64, 8, 64])
    P1 = p.sb("dn_P1", [64, 8, 64])
    P2 = p.sb("dn_P2", [64, 8, 64])
    bD = p.sb("dn_bD", [64, 8, 64])
    AtT = p.sb("dn_AtT", [64, 8, 64])
    Nk = [p.sb("dn_N%d" % i, [64, 8, 64]) for i in range(2)]
    YR = [p.sb("dn_YR%d" % i, [64, 8, 2, 64]) for i in range(2)]
    Vb8 = p.sb("dn_Vb", [64, 8, 128])
    Kbg8 = p.sb("dn_Kbg", [64, 8, 128])
    Kd8 = p.sb("dn_Kd", [64, 8, 128])
    U8 = p.sb("dn_U", [64, 8, 128])
    Vn8 = p.sb("dn_Vn", [64, 8, 128])
    WT8 = p.sb("dn_WT", [128, 8, 64])
    Qd8 = p.sb("dn_Qd", [128, 8, 64])
    oc = [p.sb("dn_oc%d" % i, [128, 8, 64]) for i in range(2)]
    f3 = lambda ap: ap.rearrange("p a b -> p (a b)")
    for b in range(NB):
        for (s, T) in seqs:
            nch = T // 64
            src = g.proj[s][b][OFF_BETA, :]
            for n_ in range(nch):
                p.dma("sp", bt[:, n_, :], dap(src, src.offset + n_ * 64, [[1, 64], [T, 32]]), allow_slow_non_contiguous=True)
            p.act(bet[:, 0:nch, :], bt[:, 0:nch, 0:16], AF.Sigmoid)
            p.tt(gt[:, 0:nch, :], bt[:, 0:nch, 16:32], ins(dtb[:, :], 1, nch), ALU.add)
            p.act(gt[:, 0:nch, :], gt[:, 0:nch, :], AF.Exp)
            p.act(gt[:, 0:nch, :], gt[:, 0:nch, :], AF.Ln, bias=1.0)
            p.tt(gt[:, 0:nch, :], gt[:, 0:nch, :], ins(alg[:, :], 1, nch), ALU.mult)
            for d in range(2):
                fw = (d == 0)
                LTc = g.cst[0:64, C_LTF:C_LTF + 64] if fw else g.cst[0:64, C_LTB:C_LTB + 64]
                Mi = g.cst[0:64, C_MGT:C_MGT + 64] if fw else g.cst[0:64, C_MLT:C_MLT + 64]
                MT = g.cst[0:64, C_MLT:C_MLT + 64] if fw else g.cst[0:64, C_MGT:C_MGT + 64]
                Si = g.cst[0:64, C_SLT:C_SLT + 64] if fw else g.cst[0:64, C_SGT:C_SGT + 64]
                ST = g.cst[0:64, C_SGT:C_SGT + 64] if fw else g.cst[0:64, C_SLT:C_SLT + 64]
                if s == "ctx":
                    p.memset(S8[:, :, :], 0.0)
                else:
                    p.copy(S8[:, :, :], stdn[:, d, :, :])
                order = list(range(nch)) if fw else list(range(nch - 1, -1, -1))
                cur_blk = None
                for ci, n in enumerate(order):
                    blk = n // CB
                    if blk != cur_blk:
                        cur_blk = blk
                        qb = qkv[(ci // CB) % 2]
                        nb_ = min(CB, nch - blk * CB)
                        for qi, off in enumerate((OFF_Q, OFF_K, OFF_V)):
                            sr = g.proj[s][b][off, blk * CB * 64]
                            p.dma("sp", qb[qi][:, :, 0:nb_ * 64],
                                  dap(sr, sr.offset, [[T, 128], [128 * T, 8], [1, nb_ * 64]]))
                    c0 = (n - blk * CB) * 64
                    qT, kT, vT = (qb[i][:, :, c0:c0 + 64] for i in range(3))
                    g8 = gt[:, n, d * 8:(d + 1) * 8]
                    be8 = bet[:, n, d * 8:(d + 1) * 8]
                    pt = g.ps()
                    p.mm(pt[0:64, 0:8], LTc, g8)
                    p.mm(pt[0:64, 8:16], ones[0:64, 0:64], g8)
                    p.mm(pt[:, 16:24], ones[0:64, :], g8)
                    Gc, Gam, Kdsc, bg = sm[:, 0, :], sm[:, 1, :], sm[:, 2, :], sm[:, 3, :]
                    p.copy(Gc, pt[0:64, 0:8])
                    p.act(Gam, pt[0:64, 0:8], AF.Exp)
                    p.tt(Kdsc, pt[0:64, 8:16], Gc, ALU.subtract)
                    p.act(Kdsc, Kdsc, AF.Exp)
                    p.act(gtot[:, :], pt[:, 16:24], AF.Exp)
                    p.tt(bg, be8, Gam, ALU.mult)
                    p.tt(gL[:, :, :], ins(g8, 2, 64), ins(LTc, 1, 8), ALU.mult)
                    pG = g.ps()
                    p.mm(pG[0:64, 0:512], ones[0:64, 0:64], f3(gL[:, :, :]))
                    p.tt(X[:, :, :], ins(Gc, 2, 64), pG[0:64, 0:512].rearrange("p (a b) -> p a b", b=64), ALU.subtract)
                    p.tt(D8[:, :, :], X[:, :, :], ins(Mi, 1, 8), ALU.subtract)
                    p.act(D8[:, :, :], D8[:, :, :], AF.Exp)
                    p.stt(DT8[:, :, :], X[:, :, :], -1.0, ins(MT, 1, 8), ALU.mult, ALU.subtract)
                    p.act(DT8[:, :, :], DT8[:, :, :], AF.Exp)
                    pK = g.ps()
                    for h in range(8):
                        p.mm(pK[0:64, h * 64:(h + 1) * 64], kT[:, h, :], kT[:, h, :])
                        p.mm(pK[0:64, 512 + h * 64:512 + (h + 1) * 64], kT[:, h, :], qT[:, h, :])
                    pKK = pK[0:64, 0:512].rearrange("p (a b) -> p a b", b=64)
                    pKQ = pK[0:64, 512:1024].rearrange("p (a b) -> p a b", b=64)
                    p.tt(P1[:, :, :], D8[:, :, :], ins(Si, 1, 8), ALU.mult)
                    p.tt(P1[:, :, :], P1[:, :, :], ins(be8, 2, 64), ALU.mult)
                    p.stt(Nk[0][:, :, :], pKK, -1.0, P1[:, :, :], ALU.mult, ALU.mult)
                    p.tt(bD[:, :, :], ins(be8, 2, 64), ins(ident64, 1, 8), ALU.mult)
                    pB = g.ps()
                    p.mm(pB[0:64, 0:512], ones[0:64, 0:64], f3(bD[:, :, :]))
                    p.tt(P2[:, :, :], DT8[:, :, :], ins(ST, 1, 8), ALU.mult)
                    p.tt(P2[:, :, :], P2[:, :, :], pB[0:64, 0:512].rearrange("p (a b) -> p a b", b=64), ALU.mult)
                    p.stt(YR[0][:, :, 0, :], pKK, -1.0, P2[:, :, :], ALU.mult, ALU.mult)
                    p.copy(YR[0][:, :, 1, :], ins(ident64, 1, 8))
                    p.tt(AtT[:, :, :], pKQ, DT8[:, :, :], ALU.mult)
                    for k in range(1, 7):
                        a_, b_ = (k - 1) % 2, k % 2
                        pA = g.ps()
                        for h in range(8):
                            p.mm(pA[0:64, h * 128:(h + 1) * 128], Nk[a_][:, h, :],
                                 YR[a_][:, h, :, :].rearrange("p a b -> p (a b)"))
                        pA4 = pA[0:64, :].rearrange("p (h t c) -> p h t c", t=2, c=64)
                        if k <= 5:
                            pN = g.ps()
                            for h in range(8):
                                p.mm(pN[0:64, h * 64:(h + 1) * 64], YR[a_][:, h, 0, :], Nk[a_][:, h, :])
                            p.act(YR[b_][:, :, 0, :], pA4[:, :, 0, :], AF.Copy)
                        p.tt(YR[b_][:, :, 1, :], pA4[:, :, 1, :], YR[a_][:, :, 1, :], ALU.add)
                        if k <= 5:
                            p.act(Nk[b_][:, :, :], pN[0:64, 0:512].rearrange("p (a b) -> p a b", b=64), AF.Copy)
                    R = YR[0][:, :, 1, :]
                    pTk = g.ps()
                    pTv = g.ps()
                    for h in range(8):
                        p.tr(pTk[0:64, h * 128:(h + 1) * 128], kT[:, h, :], g.ident)
                        p.tr(pTv[0:64, h * 128:(h + 1) * 128], vT[:, h, :], g.ident)
                    pTk3 = pTk[0:64, :].rearrange("p (a b) -> p a b", b=128)
                    pTv3 = pTv[0:64, :].rearrange("p (a b) -> p a b", b=128)
                    p.tt(Vb8[:, :, :], pTv3, ins(be8, 2, 128), ALU.mult)
                    p.tt(Kbg8[:, :, :], pTk3, ins(bg, 2, 128), ALU.mult)
                    p.tt(Kd8[:, :, :], pTk3, ins(Kdsc, 2, 128), ALU.mult)
                    pU = g.ps()
                    pW = g.ps()
                    for h in range(8):
                        p.mm(pU[0:64, h * 128:(h + 1) * 128], R[:, h, :], Vb8[:, h, :])
                        p.mm(pW[:, h * 64:(h + 1) * 64], Kbg8[:, h, :], R[:, h, :])
                    p.act(f3(U8[:, :, :]), pU[0:64, :], AF.Copy)
                    p.act(f3(WT8[:, :, :]), pW[:, 0:512], AF.Copy)
                    p.tt(bD[:, :, :], ins(Gam, 2, 64), ins(ident64, 1, 8), ALU.mult)
                    pGm = g.ps()
                    p.mm(pGm[:, 0:512], ones[0:64, :], f3(bD[:, :, :]))
                    p.tt(Qd8[:, :, :], qT, pGm[:, 0:512].rearrange("p (a b) -> p a b", b=64), ALU.mult)
                    pWS = g.ps()
                    for h in range(8):
                        p.mm(pWS[0:64, h * 128:(h + 1) * 128], WT8[:, h, :], S8[:, h, :])
                    p.tt(f3(Vn8[:, :, :]), f3(U8[:, :, :]), pWS[0:64, :], ALU.subtract)
                    pO = g.ps()
                    for h in range(8):
                        p.mm(pO[:, h * 64:(h + 1) * 64], S8[:, h, :], Qd8[:, h, :], start=True, stop=False)
                        p.mm(pO[:, h * 64:(h + 1) * 64], Vn8[:, h, :], AtT[:, h, :], start=False, stop=True)
                    if not (last and s == "ctx"):
                        o_ = oc[ci % 2]
                        p.act(f3(o_[:, :, :]), pO[:, 0:512], AF.Copy)
                        dst = g.odn[s][d, b, 0, n * 64]
                        p.dma("sp", dap(dst, dst.offset, [[T, 128], [128 * T, 8], [1, 64]]), o_[:, :, :])
                    pdS = g.ps()
                    for h in range(8):
                        p.mm(pdS[:, h * 128:(h + 1) * 128], Kd8[:, h, :], Vn8[:, h, :])
                    p.tt(S8[:, :, :], S8[:, :, :], ins(gtot[:, :], 2, 128), ALU.mult)
                    p.tt(f3(S8[:, :, :]), f3(S8[:, :, :]), pdS[:, :], ALU.add)
                if s == "ctx":
                    p.copy(stdn[:, d, :, :], S8[:, :, :])
    p.end_phase()
    ng = p.sb("dn_ng", [128, 1])
    p.dma("sp", ng[:, :], dap(W["dn_norm_g"], l * 128, [[1, 128], [1, 1]]))
    of = [p.sb("dn_of%d" % i, [128, SEQ]) for i in range(2)]
    ob = [p.sb("dn_ob%d" % i, [128, SEQ]) for i in range(2)]
    zt = [p.sb("dn_z%d" % i, [128, SEQ]) for i in range(2)]
    yo = [p.sb("dn_yo%d" % i, [128, SEQ], BF16) for i in range(2)]
    rs = p.sb("dn_rs", [128, SEQ])
    it = 0
    for b in range(NB):
        for (s, T) in seqs:
            if last and s == "ctx":
                continue
            TT = min(T, 512)
            for h in range(8):
                o1, o2, z_, y_ = of[it % 2], ob[it % 2], zt[it % 2], yo[it % 2]
                it += 1
                p.dma("sp", o1[:, 0:T], g.odn[s][0, b, h * 128:(h + 1) * 128, :])
                p.dma("sp", o2[:, 0:T], g.odn[s][1, b, h * 128:(h + 1) * 128, :])
                p.dma("sp", z_[:, 0:T], g.proj[s][b][OFF_Z + h * 128:OFF_Z + (h + 1) * 128, :])
                p.tt(o1[:, 0:T], o1[:, 0:T], o2[:, 0:T], ALU.add)
                p.tt(o2[:, 0:T], o1[:, 0:T], o1[:, 0:T], ALU.mult, eng="pool")
                for tt in range(T // TT):
                    sl = slice(tt * TT, (tt + 1) * TT)
                    pt = g.ps()
                    p.mm(pt[:, 0:TT], ones, o2[:, sl])
                    p.act(rs[:, sl], pt[:, 0:TT], AF.Sqrt, scale=1.0 / 128, bias=g.epsc[:, 0:1])
                p.recip(rs[:, 0:T], rs[:, 0:T])
                p.act(z_[:, 0:T], z_[:, 0:T], AF.Silu)
                p.stt(o1[:, 0:T], o1[:, 0:T], ng[:, 0:1], rs[:, 0:T], ALU.mult, ALU.mult)
                p.tt(y_[:, 0:T], o1[:, 0:T], z_[:, 0:T], ALU.mult)
                p.dma("sp", g.yb[s][b, h * 128:(h + 1) * 128, :], y_[:, 0:T])
    p.end_phase()


def phase_merge(g, l, last):
    p, NB, W = g.p, g.NB, g.W
    wa = p.sb("mg_wa", [128, 4, D], BF16)
    wb = p.sb("mg_wb", [128, 8, D], BF16)
    wc = p.sb("mg_wc", [128, 4, D], BF16)
    wo = p.sb("mg_wo", [128, 8, D], BF16)
    import os
    MGS = int(os.environ.get("MG_STOP", "9"))
    for hh in range(2):
        cs = slice(hh * 512, (hh + 1) * 512)
        p.dma("pool", wa[:, :, cs], dap(W["w_br_a"], l * 512 * D + hh * 512, [[D, 128], [128 * D, 4], [1, 512]]))
        p.dma("pool", wb[:, :, cs], dap(W["w_br_b"], l * D * D + hh * 512, [[D, 128], [128 * D, 8], [1, 512]]))
        p.dma("pool", wc[:, :, cs], dap(W["w_br_c"], l * 512 * D + hh * 512, [[D, 128], [128 * D, 4], [1, 512]]))
        p.dma("pool", wo[:, :, cs], dap(W["w_out"], l * D * D + hh * 512, [[D, 128], [128 * D, 8], [1, 512]]))
    if MGS == 1:
        p.end_phase()
        return
    yat = p.sb("mg_ya", [128, 4, 512], BF16)
    ybt = p.sb("mg_yb", [128, 8, 512], BF16)
    yct = p.sb("mg_yc", [128, 4, 512], BF16)
    g3 = [p.sb("mg_g%d" % i, [128, 3, 512]) for i in range(2)]
    m32 = p.sb("mg_m", [128, 512])
    t32 = p.sb("mg_t", [128, 512])
    mT = p.sb("mg_mT", [128, 8, 512], BF16)
    xt = [p.sb("mg_x%d" % i, [128, D]) for i in range(2)]
    tx = p.sb("mg_tx", [128, 512])
    xi = 0
    for b in range(NB):
        for s in ("ctx", "lat"):
            if last and s == "ctx":
                continue
            T = g.T[s]
            v = b if s == "lat" else NB
            GM = load_mod_bcast(g, l, v, 2, "mg_GM")
            TT = min(T, 512)
            for tt in range(T // TT):
                c0 = tt * TT
                for (dst, srcd, nk) in ((yat, g.ya, 4), (ybt, g.yb, 8), (yct, g.yc, 4)):
                    sr = srcd[s][b, 0, c0]
                    p.dma("sp", dst[:, :, 0:TT], dap(sr, sr.offset, [[T, 128], [128 * T, nk], [1, TT]]))
                for oc in range(8):
                    gt_ = g3[oc % 2]
                    for br in range(3):
                        r0 = OFF_GATE + br * D + oc * 128
                        p.dma("sp", gt_[:, br, 0:TT], g.proj[s][b][r0:r0 + 128, c0:c0 + TT])
                    p.act(gt_[:, :, 0:TT], gt_[:, :, 0:TT], AF.Sigmoid)
                    for br, (wt, yt, nk) in enumerate(((wa, yat, 4), (wb, ybt, 8), (wc, yct, 4))):
                        pt = g.ps()
                        for kc in range(nk):
                            p.mm(pt[:, 0:TT], wt[:, kc, oc * 128:(oc + 1) * 128], yt[:, kc, 0:TT], start=(kc == 0),
                                 stop=(kc == nk - 1))
                        if br == 0:
                            p.tt(m32[:, 0:TT], gt_[:, 0, 0:TT], pt[:, 0:TT], ALU.mult)
                        else:
                            p.tt(t32[:, 0:TT], gt_[:, br, 0:TT], pt[:, 0:TT], ALU.mult)
                            if br == 1:
                                p.tt(m32[:, 0:TT], m32[:, 0:TT], t32[:, 0:TT], ALU.add)
                            else:
                                p.tt(mT[:, oc, 0:TT], m32[:, 0:TT], t32[:, 0:TT], ALU.add)
                for st in range(TT // 128):
                    x_ = xt[xi % 2]
                    xi += 1
                    r0 = b * T + c0 + st * 128
                    p.dma("sp", x_[:, :], g.xs[s][r0:r0 + 128, :])
                    for half in range(2):
                        po = g.ps()
                        for kc in range(8):
                            p.mm(po[:, 0:512], mT[:, kc, st * 128:(st + 1) * 128], wo[:, kc, half * 512:(half + 1) * 512],
                                 start=(kc == 0), stop=(kc == 7))
                        p.tt(tx[:, :], po[:, 0:512], GM[:, half * 512:(half + 1) * 512], ALU.mult)
                        p.tt(x_[:, half * 512:(half + 1) * 512], x_[:, half * 512:(half + 1) * 512], tx[:, :], ALU.add)
                    p.dma("sp", g.xs[s][r0:r0 + 128, :], x_[:, :])
    p.end_phase()


MT_ = 512


def phase_moe(g, l, last):
    p, NB, W = g.p, g.NB, g.W
    macros = []
    for b in range(NB):
        if not last:
            macros.append(("ctx", b, 0, CTX))
        for t0 in range(0, SEQ, MT_):
            macros.append(("lat", b, t0, MT_))
    for (s, b, t0, MT) in macros:
        T = g.T[s]
        v = b if s == "lat" else NB
        r0 = b * T + t0
        G2 = load_mod_bcast(g, l, v, 4, "mo_G2")
        SH2 = load_mod_bcast(g, l, v, 3, "mo_SH2")
        gf = p.sb("mo_gf", [128, D])
        src = W["g_ffn"][l, :]
        p.dma("sp", gf[:, :], dap(src, src.offset, [[0, 128], [1, D]]))
        p.stt(G2[:, :], G2[:, :], 1.0, gf[:, :], ALU.add, ALU.mult)
        wr = p.sb("mo_wr", [128, 8, NE])
        p.dma("sp", wr[:, :, :], dap(W["w_router"], l * D * NE, [[NE, 128], [128 * NE, 8], [1, NE]]))
        brt = p.sb("mo_br", [128, NE])
        p.dma("sp", brt[:, :], dap(W["b_router"], l * NE, [[0, 128], [1, NE]]))
        lg = p.sb("mo_lg", [128, NE])
        ex = p.sb("mo_ex", [128, NE])
        mk = p.sb("mo_mk", [128, NE])
        mx = p.sb("mo_mx", [128, 8])
        sc = p.sb("mo_sc", [128, 4])

        def router(tt, h32):
            pr = g.ps()
            for kc in range(8):
                p.mm(pr[:, 0:NE], h32[:, kc, :], wr[:, kc, :], start=(kc == 0), stop=(kc == 7))
            p.tt(lg[:, :], pr[:, 0:NE], brt[:, :], ALU.add)
            p.op("dve", lambda e: e.max(out=mx[:, :], in_=lg[:, :]), [lg[:, :]], [mx[:, :]])
            p.ts(mk[:, :], lg[:, :], mx[:, 3:4], None, op0=ALU.is_ge)
            p.ts(sc[:, 0:1], mx[:, 0:1], -1.0, None, op0=ALU.mult)
            p.act(ex[:, :], lg[:, :], AF.Exp, bias=sc[:, 0:1])
            p.tt(ex[:, :], ex[:, :], mk[:, :], ALU.mult)
            p.op("dve", lambda e: e.reduce_sum(out=sc[:, 1:2], in_=ex[:, :], axis=AX.X), [ex[:, :]], [sc[:, 1:2]])
            p.recip(sc[:, 2:3], sc[:, 1:2])
            p.ts(g.Wg[:, tt, :], ex[:, :], sc[:, 2:3], None, op0=ALU.mult)

        norm_tiles(g, g.xs[s][r0:r0 + MT, :], MT, G2, SH2, g.hT2, router)
        p.end_phase()
        import os
        MOS = int(os.environ.get("MOE_STOP", "9"))
        if MOS == 1:
            return
        nt = MT // 128
        yacc = p.sb("mo_y", [128, nt, D])
        p.memset(yacc[:, :, :], 0.0)
        W1 = [p.sb("mo_W1_%d" % i, [128, 8, 2 * D], BF16) for i in range(2)]
        W2 = [p.sb("mo_W2_%d" % i, [128, 8, D], BF16) for i in range(2)]
        b1 = [p.sb("mo_b1_%d" % i, [128, 16]) for i in range(2)]
        b2 = [p.sb("mo_b2_%d" % i, [128, D]) for i in range(2)]
        actT = p.sb("mo_act", [128, 8, MT], BF16)
        gl = [p.sb("mo_gl%d" % i, [128, MT]) for i in range(2)]
        sg = [p.sb("mo_sg%d" % i, [128, MT]) for i in range(2)]
        l1 = [p.sb("mo_l1%d" % i, [128, MT]) for i in range(2)]
        tz = [p.sb("mo_tz%d" % i, [128, 512]) for i in range(2)]
        zi = 0
        for e in range(NE):
            w1, w2, b1t, b2t = W1[e % 2], W2[e % 2], b1[e % 2], b2[e % 2]
            for hh in range(2):
                sr = g.e1bf[l][e, 0, hh * D]
                p.dma("sp", w1[:, :, hh * D:(hh + 1) * D], dap(sr, sr.offset, [[2 * D, 128], [128 * 2 * D, 8], [1, D]]))
            sr = g.e2bf[l][e, 0, 0]
            p.dma("sp", w2[:, :, :], dap(sr, sr.offset, [[D, 128], [128 * D, 8], [1, D]]))
            p.dma("sp", b1t[:, :], dap(W["b_e1"], (l * NE + e) * 2 * D, [[1, 128], [128, 16]]), allow_slow_non_contiguous=True)
            p.dma("sp", b2t[:, :], dap(W["b_e2"], (l * NE + e) * D, [[0, 128], [1, D]]))
            if MOS == 2:
                p.copy(yacc[:, 0, 0:16], b1t[:, :])
                p.copy(yacc[:, 0, 0:16], b2t[:, 0:16])
                p.copy(actT[:, 0, 0:16], w1[:, 0, 0:16])
                p.copy(actT[:, 0, 0:16], w2[:, 0, 0:16])
                continue
            for j in range(8):
                pg = g.ps()
                for kc in range(8):
                    p.mm(pg[:, 0:MT], w1[:, kc, j * 128:(j + 1) * 128], g.hT2[:, kc, 0:MT], start=(kc == 0), stop=(kc == 7))
                for kc in range(8):
                    p.mm(pg[:, 512:512 + MT], w1[:, kc, D + j * 128:D + (j + 1) * 128], g.hT2[:, kc, 0:MT], start=(kc == 0),
                         stop=(kc == 7))
                g_, s_, l_ = gl[j % 2], sg[j % 2], l1[j % 2]
                p.ts(g_[:, :], pg[:, 0:MT], b1t[:, j:j + 1], 7.0, op0=ALU.add, op1=ALU.min)
                p.act(s_[:, :], g_[:, :], AF.Sigmoid, scale=1.702)
                p.ts(l_[:, :], pg[:, 512:512 + MT], b1t[:, 8 + j:9 + j], -7.0, op0=ALU.add, op1=ALU.max)
                p.ts(l_[:, :], l_[:, :], 7.0, 1.0, op0=ALU.min, op1=ALU.add, eng="pool")
                p.tt(g_[:, :], g_[:, :], s_[:, :], ALU.mult)
                p.tt(actT[:, j, :], g_[:, :], l_[:, :], ALU.mult, eng="pool")
            for st in range(nt):
                for half in range(2):
                    po = g.ps()
                    for kc in range(8):
                        p.mm(po[:, 0:512], actT[:, kc, st * 128:(st + 1) * 128], w2[:, kc, half * 512:(half + 1) * 512],
                             start=(kc == 0), stop=(kc == 7))
                    z_ = tz[zi % 2]
                    zi += 1
                    p.tt(z_[:, :], po[:, 0:512], b2t[:, half * 512:(half + 1) * 512], ALU.add)
                    ya = yacc[:, st, half * 512:(half + 1) * 512]
                    p.stt(ya, z_[:, :], g.Wg[:, st, e:e + 1], ya, ALU.mult, ALU.add)
        GMLP = load_mod_bcast(g, l, v, 5, "mo_GMLP")
        xt = [p.sb("mo_x%d" % i, [128, D]) for i in range(2)]
        for st in range(nt):
            x_ = xt[st % 2]
            rr = r0 + st * 128
            p.dma("sp", x_[:, :], g.xs[s][rr:rr + 128, :])
            p.tt(yacc[:, st, :], yacc[:, st, :], GMLP[:, :], ALU.mult)
            p.tt(x_[:, :], x_[:, :], yacc[:, st, :], ALU.add)
            p.dma("sp", g.xs[s][rr:rr + 128, :], x_[:, :])
        p.end_phase()
        if MOS in (2, 3):
            return


def phase_final(g):
    p, NB, W = g.p, g.NB, g.W
    gfin = p.sb("fn_g", [128, D])
    p.dma("sp", gfin[:, :], dap(W["g_final"], 0, [[0, 128], [1, D]]))
    xts = [p.sb("fn_x%d" % i, [128, D]) for i in range(2)]
    sq = p.sb("fn_sq", [128, D])
    ss = p.sb("fn_ss", [128, 4])
    for tt in range(NB * SEQ // 128):
        xt = xts[tt % 2]
        p.dma("sp", xt[:, :], g.xs["lat"][tt * 128:(tt + 1) * 128, :])
        p.act(sq[:, :], xt[:, :], AF.Square)
        p.op("dve", lambda e, o=ss[:, 0:1], i=sq[:, :]: e.reduce_sum(out=o, in_=i, axis=AX.X), [sq[:, :]], [ss[:, 0:1]])
        p.ts(ss[:, 1:2], ss[:, 0:1], 1.0 / D, EPS, op0=ALU.mult, op1=ALU.add)
        p.act(ss[:, 2:3], ss[:, 1:2], AF.Sqrt)
        p.recip(ss[:, 3:4], ss[:, 2:3])
        p.stt(xt[:, :], xt[:, :], ss[:, 3:4], gfin[:, :], ALU.mult, ALU.mult)
        p.dma("sp", g.out[tt * 128:(tt + 1) * 128, :], xt[:, :])
    p.end_phase()


_NC_CACHE = {}


def kernel(**inputs):
    NCORES = 8
    NB = 32 // NCORES
    if "nc" not in _NC_CACHE:
        _NC_CACHE["nc"] = build_program(NB=NB)
    nc = _NC_CACHE["nc"]
    consts = make_consts()
    f32 = lambda a: np.ascontiguousarray(np.asarray(a, dtype=np.float32))
    wts = {n: f32(inputs[n]).reshape(WSHAPES[n]) for n in WNAMES}
    x = f32(inputs["x"])
    ctx = f32(inputs["ctx"])
    c = f32(inputs["c"])
    cctx = f32(inputs["c_ctx"]).reshape(1, D)
    in_maps = []
    for i in range(NCORES):
        m = {"x": x[i * NB:(i + 1) * NB].reshape(NB * SEQ, D), "ctx": ctx[i * NB:(i + 1) * NB].reshape(NB * CTX, D),
             "c": c[i * NB:(i + 1) * NB], "c_ctx": cctx, "consts": consts}
        m.update(wts)
        in_maps.append(m)
    res = run_bass_kernel_spmd(nc, in_maps, core_ids=list(range(NCORES)))
    out = np.concatenate([r["out"].reshape(NB, SEQ, D) for r in res.results], axis=0)
    return out.astype(np.float32)
```

```python
import contextlib
import math
import numpy as np
import concourse.bass as bass
import concourse.mybir as mybir
from concourse.bass_utils import run_bass_kernel_spmd

F32 = mybir.dt.float32
BF16 = mybir.dt.bfloat16
ALU = mybir.AluOpType
AF = mybir.ActivationFunctionType
AX = mybir.AxisListType

D = 1024
SEQ = 2048
CTX = 256
DEPTH = 2
D_IN = 8736
NE = 32
EPS = 1e-6
OFF_AX, OFF_AY, OFF_Q, OFF_K, OFF_V, OFF_Z, OFF_BETA, OFF_ALPHA, OFF_U, OFF_GATE = (
    0, 512, 1024, 2048, 3072, 4096, 5120, 5136, 5152, 5664)

SAME_ENGINE_SYNC = True
RELAX_SAME = True
NDSEM = 12


class Buf:
    __slots__ = ("w", "r")

    def __init__(self):
        self.w = {}
        self.r = {}


class Prog:
    ENG = ("pe", "act", "dve", "pool", "sp")

    def __init__(self, nc):
        self.nc = nc
        self.ops = {e: [] for e in self.ENG}
        self.cnt = {e: 0 for e in self.ENG}
        self.known = {e: {} for e in self.ENG}
        self.dq = {e: 0 for e in self.ENG}
        self.dval = {}
        self.bufs = {}
        self.st = contextlib.ExitStack()
        self.uid = 0
        self.raw_same = {}

    def sb(self, name, shape, dtype=F32):
        self.uid += 1
        t = self.st.enter_context(self.nc.sbuf_tensor("%s_%d" % (name, self.uid), list(shape), dtype))
        return t

    def psum(self, name, shape, dtype=F32):
        return self.st.enter_context(self.nc.psum_tensor(name, list(shape), dtype))

    def dram(self, name, shape, dtype=F32, kind="Internal"):
        return self.nc.dram_tensor(name, list(shape), dtype, kind=kind)

    def _buf(self, ap):
        n = ap.tensor.name
        b = self.bufs.get(n)
        if b is None:
            b = self.bufs[n] = Buf()
        return b

    def _sched(self, e, reads, writes, tok_fn):
        d = {}
        rb = [self._buf(a) for a in reads if a is not None and not isinstance(a, (int, float))]
        wb = [self._buf(a) for a in writes]
        rawv = 0
        for b in rb:
            rawv = max(rawv, b.w.get(e, 0))
        self.raw_same = {e: (self.cnt[e] if rawv < self.cnt[e] - 3 else rawv - 1) if rawv else self.cnt[e]}
        if rawv and rawv >= self.cnt[e] - 3:
            self.raw_same = {e: rawv - 1}
        else:
            self.raw_same = {e: self.cnt[e]}
        for a, b in zip([a for a in reads if a is not None and not isinstance(a, (int, float))], rb):
            for k, v in b.w.items():
                if d.get(k, 0) < v:
                    d[k] = v
            if a.tensor.name.startswith("ps"):
                for k, v in b.r.items():
                    if k != e and d.get(k, 0) < v:
                        d[k] = v
        for b in wb:
            for k, v in b.w.items():
                if d.get(k, 0) < v:
                    d[k] = v
            for k, v in b.r.items():
                if d.get(k, 0) < v:
                    d[k] = v
        return d, rb, wb

    def _waits(self, e, d):
        kn = self.known[e]
        for k, v in d.items():
            if k == e and ((not SAME_ENGINE_SYNC) or e in ("pe", "sp")):
                continue
            if k == e and RELAX_SAME and v <= self.raw_same.get(e, 0):
                continue
            if kn.get(k, 0) >= v:
                continue
            kn[k] = v
            self.ops[e].append(("wait", k, v))

    def op(self, e, fn, reads, writes):
        d, rb, wb = self._sched(e, reads, writes, None)
        self._waits(e, d)
        self.cnt[e] += 1
        k, v = e, self.cnt[e]
        self.ops[e].append(("op", fn))
        for b in rb:
            if b.r.get(k, 0) < v:
                b.r[k] = v
        for b in wb:
            b.w[k] = v
            b.r = {}

    def dma(self, q, out, in_, **kw):
        d, rb, wb = self._sched(q, [in_], [out], None)
        j = self.dq[q]
        self.dq[q] = (j + 1) % (4 if q == "pool" else NDSEM)
        key = ("d", q, j)
        prev = self.dval.get(key, 0)
        if prev:
            d[key] = max(d.get(key, 0), prev)
        self._waits(q, d)
        val = prev + 16
        self.dval[key] = val
        self.ops[q].append(("dma", out, in_, key, kw))
        for b in rb:
            if b.r.get(key, 0) < val:
                b.r[key] = val
        for b in wb:
            b.w[key] = val
            b.r = {}

    def mm(self, out, lhsT, rhs, start=True, stop=True):
        self.op("pe", lambda e: e.matmul(out, lhsT=lhsT, rhs=rhs, start=start, stop=stop), [lhsT, rhs], [out])

    def tr(self, out, in_, ident):
        self.op("pe", lambda e: e.transpose(out, in_, ident), [in_, ident], [out])

    def act(self, out, in_, func, bias=None, scale=None, accum_out=None, eng="act"):
        kw = {}
        rd = [in_]
        wr = [out]
        if bias is not None:
            kw["bias"] = bias
            rd.append(bias)
        if scale is not None:
            kw["scale"] = scale
            rd.append(scale)
        if accum_out is not None:
            kw["accum_out"] = accum_out
            wr.append(accum_out)
        self.op("act", lambda e: e.activation(out=out, in_=in_, func=func, **kw), rd, wr)

    def tt(self, out, in0, in1, op, eng="dve"):
        self.op(eng, lambda e: e.tensor_tensor(out=out, in0=in0, in1=in1, op=op), [in0, in1], [out])

    def ts(self, out, in0, s1, s2=None, op0=ALU.mult, op1=None, eng="dve"):
        kw = {}
        if op1 is not None:
            kw["op1"] = op1
        self.op(eng, lambda e: e.tensor_scalar(out=out, in0=in0, scalar1=s1, scalar2=s2, op0=op0, **kw),
                [in0, s1, s2], [out])

    def stt(self, out, in0, scalar, in1, op0, op1, eng="dve"):
        self.op(eng, lambda e: e.scalar_tensor_tensor(out=out, in0=in0, scalar=scalar, in1=in1, op0=op0, op1=op1),
                [in0, scalar, in1], [out])

    def copy(self, out, in_, eng="dve"):
        if eng == "act":
            self.act(out, in_, AF.Copy)
        else:
            self.op(eng, lambda e: e.tensor_copy(out=out, in_=in_), [in_], [out])

    def memset(self, out, val, eng="dve"):
        self.op(eng, lambda e: e.memset(out, val), [], [out])

    def recip(self, out, in_):
        self.op("dve", lambda e: e.reciprocal(out=out, in_=in_), [in_], [out])

    def scan(self, out, d0, d1, init, eng="dve"):
        self.op(eng, lambda e: e.tensor_tensor_scan(out=out, data0=d0, data1=d1, initial=init, op0=ALU.mult,
                                                    op1=ALU.add), [d0, d1, init], [out])

    def barrier(self):
        for e in self.ENG:
            d = {}
            for e2 in self.ENG:
                if e2 != e and self.cnt[e2]:
                    d[e2] = self.cnt[e2]
            for key, val in self.dval.items():
                d[key] = val
            self._waits(e, d)

    def setup(self):
        nc = self.nc
        self.gst = contextlib.ExitStack()
        self.sems = {}
        for e in self.ENG:
            self.sems[e] = self.gst.enter_context(nc.semaphore("s_" + e))
        for q in ("sp", "pool", "act"):
            for j in range(NDSEM):
                self.sems[("d", q, j)] = self.gst.enter_context(nc.semaphore("d_%s_%d" % (q, j)))
        self.st = contextlib.ExitStack()

    def gsb(self, name, shape, dtype=F32):
        return self.gst.enter_context(self.nc.sbuf_tensor(name, list(shape), dtype))

    def gpsum(self, name, shape, dtype=F32):
        return self.gst.enter_context(self.nc.psum_tensor(name, list(shape), dtype))

    def end_phase(self, final=False):
        nc = self.nc
        self.barrier()
        sems = self.sems
        ops = self.ops

        def run(e, eng):
            se = sems[e]
            for o in ops[e]:
                if o[0] == "wait":
                    eng.wait_ge(sems[o[1]], o[2])
                elif o[0] == "op":
                    o[1](eng).then_inc(se, 1)
                else:
                    _, out, in_, key, kw = o
                    eng.dma_start(out=out, in_=in_, **kw).then_inc(sems[key], 16)

        with nc.Block() as block:
            block.sync(lambda eng: run("sp", eng))
            block.tensor(lambda eng: run("pe", eng))
            block.scalar(lambda eng: run("act", eng))
            block.vector(lambda eng: run("dve", eng))
            block.gpsimd(lambda eng: run("pool", eng))
        self.ops = {e: [] for e in self.ENG}
        self.st.close()
        self.st = contextlib.ExitStack()
        if final:
            self.gst.close()


def rev(ap):
    a = [list(x) for x in ap.ap]
    step, n = a[-1]
    a[-1] = [-step, n]
    return bass.AP(ap.tensor, ap.offset + step * (n - 1), a)


def bcast_rows(ap, nparts):
    a = [list(x) for x in ap.ap]
    return bass.AP(ap.tensor, ap.offset, [[0, nparts]] + a[-1:])


WNAMES = ["w_ada", "b_ada", "g_mix", "g_ffn", "w_in", "lru_conv_w", "lru_conv_b", "lru_w_gate", "lru_b_gate",
          "lru_lam", "dn_conv_w", "dn_a_log", "dn_dt_bias", "dn_norm_g", "s5_lam_re", "s5_lam_im", "s5_log_dt",
          "s5_b_re", "s5_b_im", "s5_c_re", "s5_c_im", "s5_d", "s5_w_glu", "s5_b_glu", "w_br_a", "w_br_b", "w_br_c",
          "w_out", "w_router", "b_router", "w_e1", "b_e1", "w_e2", "b_e2", "g_final"]


WSHAPES = {
    "w_ada": [2, 1024, 6144], "b_ada": [2, 6144], "g_mix": [2, 1024], "g_ffn": [2, 1024], "w_in": [2, 1024, 8736],
    "lru_conv_w": [2, 4, 512], "lru_conv_b": [2, 512], "lru_w_gate": [2, 2, 2, 8, 64, 64], "lru_b_gate": [2, 2, 2, 512],
    "lru_lam": [2, 2, 512], "dn_conv_w": [2, 4, 3072], "dn_a_log": [2, 2, 8], "dn_dt_bias": [2, 2, 8],
    "dn_norm_g": [2, 128], "s5_lam_re": [2, 2, 32, 64], "s5_lam_im": [2, 2, 32, 64], "s5_log_dt": [2, 2, 32],
    "s5_b_re": [2, 2, 32, 64, 16], "s5_b_im": [2, 2, 32, 64, 16], "s5_c_re": [2, 2, 32, 16, 64],
    "s5_c_im": [2, 2, 32, 16, 64], "s5_d": [2, 512], "s5_w_glu": [2, 512, 512], "s5_b_glu": [2, 512],
    "w_br_a": [2, 512, 1024], "w_br_b": [2, 1024, 1024], "w_br_c": [2, 512, 1024], "w_out": [2, 1024, 1024],
    "w_router": [2, 1024, 32], "b_router": [2, 32], "w_e1": [2, 32, 1024, 2048], "b_e1": [2, 32, 2048],
    "w_e2": [2, 32, 1024, 1024], "b_e2": [2, 32, 1024], "g_final": [1, 1024],
}
BIG = 30000.0
C_ID, C_ONE, C_LTF, C_LTB, C_MGT, C_MLT, C_SGT, C_SLT, C_IOTA, C_END = 0, 128, 256, 320, 384, 448, 512, 576, 640, 640 + 2048


def make_consts():
    c = np.zeros((128, C_END), np.float32)
    c[:, C_ID:C_ID + 128] = np.eye(128, dtype=np.float32)
    c[:, C_ONE:C_ONE + 128] = 1.0
    p = np.arange(128)[:, None]
    f = np.arange(64)[None, :]
    c[:, C_LTF:C_LTF + 64] = (p <= f) & (p < 64)
    c[:, C_LTB:C_LTB + 64] = (p >= f) & (p < 64)
    c[:, C_MGT:C_MGT + 64] = BIG * (f > p)
    c[:, C_MLT:C_MLT + 64] = BIG * (f < p)
    c[:, C_SGT:C_SGT + 64] = (f > p)
    c[:, C_SLT:C_SLT + 64] = (f < p)
    c[:, C_IOTA:C_IOTA + 2048] = np.arange(2048, dtype=np.float32)[None, :]
    return c


def dap(t, offset, dims):
    return bass.AP(t.tensor if hasattr(t, "tensor") else t, offset, [list(d) for d in dims])


class Ctx:
    pass


def build_program(NB=4, layers=(0, 1), stop_after=None, dbg=(), skip=(), inject=()):
    nc = bass.Bass("TRN2", target_bir_lowering=False)
    p = Prog(nc)
    p.setup()
    NV = NB + 1
    g = Ctx()
    g.NB, g.NV, g.p, g.nc = NB, NV, p, nc
    g.skip = set(skip)
    x_in = nc.dram_tensor("x", [NB * SEQ, D], F32, kind="ExternalInput").ap()
    ctx_in = nc.dram_tensor("ctx", [NB * CTX, D], F32, kind="ExternalInput").ap()
    c_in = nc.dram_tensor("c", [NB, D], F32, kind="ExternalInput").ap()
    cctx_in = nc.dram_tensor("c_ctx", [1, D], F32, kind="ExternalInput").ap()
    consts_in = nc.dram_tensor("consts", [128, C_END], F32, kind="ExternalInput").ap()
    W = {}
    for name in WNAMES:
        W[name] = nc.dram_tensor(name, WSHAPES[name], F32, kind="ExternalInput").ap()
    out = nc.dram_tensor("out", [NB * SEQ, D], F32, kind="ExternalOutput").ap()
    g.W, g.out = W, out

    g.xs = {"lat": p.dram("xs_lat", [NB * SEQ, D]).ap(), "ctx": p.dram("xs_ctx", [NB * CTX, D]).ap()}
    g.T = {"lat": SEQ, "ctx": CTX}
    g.mod = p.dram("mod", [DEPTH, NV, 6 * D]).ap()
    g.proj = {"lat": [p.dram("proj_lat%d" % b, [D_IN, SEQ]).ap() for b in range(NB)],
              "ctx": [p.dram("proj_ctx%d" % b, [D_IN, CTX]).ap() for b in range(NB)]}
    g.ya = {s: p.dram("ya_" + s, [NB, 512, g.T[s]], BF16).ap() for s in ("lat", "ctx")}
    g.yb = {s: p.dram("yb_" + s, [NB, 1024, g.T[s]], BF16).ap() for s in ("lat", "ctx")}
    g.yc = {s: p.dram("yc_" + s, [NB, 512, g.T[s]], BF16).ap() for s in ("lat", "ctx")}
    g.ys5 = {s: p.dram("ys5_" + s, [NB, 512, g.T[s]]).ap() for s in ("lat", "ctx")}
    g.odn = {s: p.dram("odn_" + s, [2, NB, 1024, g.T[s]]).ap() for s in ("lat", "ctx")}
    g.e1bf = [p.dram("e1bf%d" % l_, [NE, D, 2 * D], BF16).ap() for l_ in range(DEPTH)]
    g.e2bf = [p.dram("e2bf%d" % l_, [NE, D, D], BF16).ap() for l_ in range(DEPTH)]

    g.cst = p.gsb("cst", [128, C_END])
    g.cstb = p.gsb("cstb", [128, 256], BF16)
    g.PS = [p.gpsum("ps%d" % i, [128, 1024]) for i in range(4)]
    g.psi = 0
    g.stl = p.gsb("st_lru", [128, NB, 4, 2])
    g.sts5 = p.gsb("st_s5", [128, NB, 2, 16, 2])
    g.hT2 = p.gsb("hT2", [128, 8, MT_], BF16)
    g.Wg = p.gsb("Wg", [128, MT_ // 128, NE])
    g.WgT = p.gsb("WgT", [NE, MT_ // 128, 128])

    g.dumps = set(x for x in dbg if x.startswith("@"))

    def dump(name, ap, dtype=F32):
        if "@" + name not in g.dumps:
            return
        shp = list(ap.shape)
        dst = nc.dram_tensor("dbg_" + name, [shp[0], int(np.prod(shp[1:]))], dtype, kind="ExternalOutput").ap()
        if len(shp) == 3:
            dst = dst.rearrange("p (a b) -> p a b", b=shp[2])
        p.dma("sp", dst, ap)
    g.dump = dump

    def ps():
        t = g.PS[g.psi % 4]
        g.psi += 1
        return t
    g.ps = ps
    g.epsc = p.gsb("epsc", [128, 1])
    p.memset(g.epsc[:, :], EPS)
    g.negpi = p.gsb("negpi", [128, 1])
    p.memset(g.negpi[:, :], -math.pi)
    g.ident = g.cst[:, C_ID:C_ID + 128]
    g.ones = g.cst[:, C_ONE:C_ONE + 128]

    p.dma("sp", g.cst[:, :], consts_in[:, :])
    p.copy(g.cstb[:, :], g.cst[:, 0:256])
    for b in range(NB):
        p.dma("sp", g.xs["lat"][b * SEQ:(b + 1) * SEQ, :], x_in[b * SEQ:(b + 1) * SEQ, :])
    p.dma("sp", g.xs["ctx"][:, :], ctx_in[:, :])
    if "moe" not in (stop_after or ()):
        pass
    g.cast_moe = lambda: None
    p.end_phase()

    def cast_moe(l):
        for e in range(NE):
            for cb in range(4):
                p.dma("pool", g.e1bf[l][e, :, cb * 512:(cb + 1) * 512], W["w_e1"][l, e, :, cb * 512:(cb + 1) * 512])
            for cb in range(2):
                p.dma("pool", g.e2bf[l][e, :, cb * 512:(cb + 1) * 512], W["w_e2"][l, e, :, cb * 512:(cb + 1) * 512])
    g.cast_moe = cast_moe

    for l in layers:
        last = (l == DEPTH - 1)
        phase_adaln(g, l, c_in, cctx_in)
        if stop_after == "adaln":
            break
        for b in range(NB):
            for s in ("ctx", "lat"):
                phase_norm_proj(g, l, b, s)
        if stop_after == "proj":
            break
        if "lru" not in g.skip:
            phase_lru(g, l, last)
        if stop_after == "lru":
            break
        if "s5" not in g.skip:
            phase_s5(g, l, last)
        if stop_after == "s5":
            break
        if "dn" not in g.skip:
            phase_dn(g, l, last)
        if stop_after == "dn":
            break
        phase_merge(g, l, last)
        if stop_after == "merge":
            break
        if "moe" not in g.skip:
            import os
            if not os.environ.get("MOE_NOCAST"):
                g.cast_moe(l)
            phase_moe(g, l, last)
        if stop_after == "moe":
            break
    if stop_after is None:
        phase_final(g)
    srcs = {"mod": g.mod, "proj_lat": g.proj["lat"][0], "proj_ctx": g.proj["ctx"][0], "xs_lat": g.xs["lat"],
            "xs_ctx": g.xs["ctx"]}
    for s_ in ("lat", "ctx"):
        for nm, dd in (("ya", g.ya), ("yb", g.yb), ("yc", g.yc), ("ys5", g.ys5), ("odn", g.odn)):
            srcs[nm + "_" + s_] = dd[s_]
    for name in dbg:
        if name.startswith("@"):
            continue
        src = srcs[name]
        n0 = src.shape[0]
        rest = int(np.prod(src.shape[1:]))
        dst = nc.dram_tensor("dbg_" + name, [n0, rest], src.dtype, kind="ExternalOutput").ap()
        for i in range(n0):
            p.dma("sp", dst[i:i + 1, :], dap(src, src.offset + i * rest, [[rest, 1], [1, rest]]))
    if stop_after is None:
        pass
    p.end_phase(final=True)
    return nc


def load_mod_bcast(g, l, v, idx, name):
    p = g.p
    t = p.sb(name, [128, D])
    src = g.mod[l, v, idx * D:(idx + 1) * D]
    p.dma("sp", t[:, :], dap(src, src.offset, [[0, 128], [1, D]]))
    return t


def phase_adaln(g, l, c_in, cctx_in):
    p, NB, NV, W = g.p, g.NB, g.NV, g.W
    sT = p.sb("ad_sT", [128, 8, NV])
    sTb = p.sb("ad_sTb", [128, 8, NV], BF16)
    for v in range(NB):
        p.dma("sp", sT[:, :, v], dap(c_in, v * D, [[1, 128], [128, 8]]), allow_slow_non_contiguous=True)
    p.dma("sp", sT[:, :, NB], dap(cctx_in, 0, [[1, 128], [128, 8]]), allow_slow_non_contiguous=True)
    p.act(sTb[:, :, :], sT[:, :, :], AF.Silu)
    bias = p.sb("ad_bias", [NV, 6 * D])
    src = W["b_ada"][l, :]
    p.dma("sp", bias[:, :], dap(src, src.offset, [[0, NV], [1, 6 * D]]))
    modsb = p.sb("ad_mod", [NV, 6 * D])
    wts = [p.sb("ad_w%d" % i, [128, 8, 512], BF16) for i in range(2)]
    for ct in range(12):
        wt = wts[ct % 2]
        src = W["w_ada"][l, :, ct * 512:(ct + 1) * 512]
        p.dma("pool", wt[:, :, :], dap(src, src.offset, [[6 * D, 128], [128 * 6 * D, 8], [1, 512]]))
        pt = g.ps()
        for kc in range(8):
            p.mm(pt[0:NV, 0:512], sTb[:, kc, :], wt[:, kc, :], start=(kc == 0), stop=(kc == 7))
        p.tt(modsb[:, ct * 512:(ct + 1) * 512], pt[0:NV, 0:512], bias[:, ct * 512:(ct + 1) * 512], ALU.add)
    p.dma("sp", g.mod[l, :, :], modsb[:, :])
    g.dump("sT", sT[:, :, :])
    g.dump("bias", bias[:, :])
    g.dump("modsb", modsb[:, :])
    p.end_phase()


def phase_norm_proj(g, l, b, s):
    p, NB, W = g.p, g.NB, g.W
    T = g.T[s]
    v = b if s == "lat" else NB
    xs = g.xs[s][b * T:(b + 1) * T, :]
    hT = p.sb("np_hT", [128, 8, T], BF16)
    G1 = load_mod_bcast(g, l, v, 1, "np_G1")
    SH = load_mod_bcast(g, l, v, 0, "np_SH")
    gm = p.sb("np_gm", [128, D])
    src = W["g_mix"][l, :]
    p.dma("sp", gm[:, :], dap(src, src.offset, [[0, 128], [1, D]]))
    p.stt(G1[:, :], G1[:, :], 1.0, gm[:, :], ALU.add, ALU.mult)
    norm_tiles(g, xs, T, G1, SH, hT, None)
    wts = [p.sb("np_w%d" % i, [128, 8, 512], BF16) for i in range(2)]
    stg = [p.sb("np_stg%d" % i, [128, 512]) for i in range(4)]
    si = 0
    ngrp = (D_IN + 511) // 512
    TT = min(T, 512)
    for og in range(ngrp):
        c0 = og * 512
        ncol = min(512, D_IN - c0)
        wt = wts[og % 2]
        src = W["w_in"][l, :, c0:c0 + ncol]
        p.dma("pool", wt[:, :, 0:ncol], dap(src, src.offset, [[D_IN, 128], [128 * D_IN, 8], [1, ncol]]))
        for oc in range((ncol + 127) // 128):
            m = min(128, ncol - oc * 128)
            for tt in range(T // TT):
                pt = g.ps()
                for kc in range(8):
                    p.mm(pt[0:m, 0:TT], wt[:, kc, oc * 128:oc * 128 + m], hT[:, kc, tt * TT:(tt + 1) * TT],
                         start=(kc == 0), stop=(kc == 7))
                sg = stg[si % 4]
                p.copy(sg[0:m, 0:TT], pt[0:m, 0:TT], eng=("act" if si % 2 else "dve"))
                si += 1
                r0 = c0 + oc * 128
                p.dma("sp", g.proj[s][b][r0:r0 + m, tt * TT:(tt + 1) * TT], sg[0:m, 0:TT])
    p.end_phase()


def norm_tiles(g, xs, T, G1, SH, hT, h32cb):
    p = g.p
    xts = [p.sb("nt_x%d" % i, [128, D]) for i in range(2)]
    sq = p.sb("nt_sq", [128, D])
    hN = [p.sb("nt_h%d" % i, [128, D]) for i in range(2)]
    ss = p.sb("nt_ss", [128, 4])
    h32 = [p.sb("nt_h32_%d" % i, [128, 8, 128]) for i in range(2)] if h32cb else None
    for tt in range(T // 128):
        xt = xts[tt % 2]
        hn = hN[tt % 2]
        p.dma("sp", xt[:, :], xs[tt * 128:(tt + 1) * 128, :])
        p.act(sq[:, :], xt[:, :], AF.Square)
        p.op("dve", lambda e, o=ss[:, 0:1], i=sq[:, :]: e.reduce_sum(out=o, in_=i, axis=AX.X), [sq[:, :]], [ss[:, 0:1]])
        p.ts(ss[:, 1:2], ss[:, 0:1], 1.0 / D, EPS, op0=ALU.mult, op1=ALU.add)
        p.act(ss[:, 2:3], ss[:, 1:2], AF.Sqrt)
        p.recip(ss[:, 3:4], ss[:, 2:3])
        p.stt(hn[:, :], xt[:, :], ss[:, 3:4], G1[:, :], ALU.mult, ALU.mult)
        p.tt(hn[:, :], hn[:, :], SH[:, :], ALU.add)
        for half in range(2):
            pt = g.ps()
            for k4 in range(4):
                kc = half * 4 + k4
                p.tr(pt[:, k4 * 128:(k4 + 1) * 128], hn[:, kc * 128:(kc + 1) * 128], g.ident)
            src = pt[:, 0:512].rearrange("p (a b) -> p a b", b=128)
            p.act(hT[:, half * 4:half * 4 + 4, tt * 128:(tt + 1) * 128], src, AF.Copy)
            if h32cb:
                p.copy(h32[tt % 2][:, half * 4:half * 4 + 4, :], src)
        if h32cb:
            h32cb(tt, h32[tt % 2])


def phase_lru(g, l, last):
    p, NB, W = g.p, g.NB, g.W
    Wbd = p.sb("lr_Wbd", [128, 2, 2, 4, 128])
    p.memset(Wbd[:, :, :, :, :], 0.0)
    for d in range(2):
        for gg in range(2):
            for hh in range(2):
                off = W["lru_w_gate"][l, d, gg, hh, 0, 0].offset if False else (((l * 2 + d) * 2 + gg) * 8 + hh) * 4096
                p.dma("sp", Wbd[hh * 64:(hh + 1) * 64, d, gg, :, hh * 64:(hh + 1) * 64],
                      dap(W["lru_w_gate"], off, [[64, 64], [2 * 4096, 4], [1, 64]]))
    lam = p.sb("lr_lam", [128, 2, 4])
    cv = p.sb("lr_cv", [128, 2, 4])
    bg = p.sb("lr_bg", [128, 2, 2, 4])
    cw = p.sb("lr_cw", [128, 4, 4])
    cb = p.sb("lr_cb", [128, 4])
    p.dma("sp", lam[:, :, :], dap(W["lru_lam"], l * 1024, [[1, 128], [512, 2], [128, 4]]), allow_slow_non_contiguous=True)
    for d in range(2):
        p.dma("sp", bg[:, d, :, :], dap(W["lru_b_gate"], (l * 2 + d) * 1024, [[1, 128], [512, 2], [128, 4]]),
              allow_slow_non_contiguous=True)
    p.dma("sp", cw[:, :, :], dap(W["lru_conv_w"], l * 2048, [[1, 128], [512, 4], [128, 4]]), allow_slow_non_contiguous=True)
    p.dma("sp", cb[:, :], dap(W["lru_conv_b"], l * 512, [[1, 128], [128, 4]]), allow_slow_non_contiguous=True)
    p.act(cv[:, :, :], lam[:, :, :], AF.Exp, scale=-1.0)
    p.act(cv[:, :, :], cv[:, :, :], AF.Ln, bias=1.0)
    p.ts(cv[:, :, :], cv[:, :, :], -8.0, None, op0=ALU.mult)
    TM = SEQ
    ax = p.sb("lr_ax", [128, TM])
    xc = p.sb("lr_xc", [128, TM])
    ay = p.sb("lr_ay", [128, TM])
    rt = p.sb("lr_r", [128, TM])
    it = p.sb("lr_i", [128, TM])
    at = p.sb("lr_a", [128, TM])
    a2 = p.sb("lr_a2", [128, TM])
    bt = p.sb("lr_b", [128, TM])
    hd = [p.sb("lr_h%d" % d, [128, TM]) for d in range(2)]
    yo = p.sb("lr_y", [128, TM], BF16)
    for b in range(NB):
        for s in ("ctx", "lat"):
            T = g.T[s]
            L = 64 if s == "lat" else T
            TT = min(T, 512)
            emit = not (last and s == "ctx")
            for ch in range(4):
                p.dma("sp", ax[:, 0:T], g.proj[s][b][OFF_AX + ch * 128:OFF_AX + (ch + 1) * 128, :])
                if emit:
                    p.dma("sp", ay[:, 0:T], g.proj[s][b][OFF_AY + ch * 128:OFF_AY + (ch + 1) * 128, :])
                a3 = ax[:, 0:T].rearrange("p (r l) -> p r l", l=L)
                x3 = xc[:, 0:T].rearrange("p (r l) -> p r l", l=L)
                p.ts(xc[:, 0:T], ax[:, 0:T], cw[:, 2, ch:ch + 1], cb[:, ch:ch + 1], op0=ALU.mult, op1=ALU.add)
                p.stt(x3[:, :, 2:L], a3[:, :, 0:L - 2], cw[:, 0, ch:ch + 1], x3[:, :, 2:L], ALU.mult, ALU.add)
                p.stt(x3[:, :, 1:L], a3[:, :, 0:L - 1], cw[:, 1, ch:ch + 1], x3[:, :, 1:L], ALU.mult, ALU.add)
                p.stt(x3[:, :, 0:L - 1], a3[:, :, 1:L], cw[:, 3, ch:ch + 1], x3[:, :, 0:L - 1], ALU.mult, ALU.add)
                for d in range(2):
                    for tt in range(T // TT):
                        sl = slice(tt * TT, (tt + 1) * TT)
                        pr = g.ps()
                        p.mm(pr[:, 0:TT], Wbd[:, d, 0, ch, :], xc[:, sl])
                        p.mm(pr[:, 512:512 + TT], Wbd[:, d, 1, ch, :], xc[:, sl])
                        p.act(rt[:, sl], pr[:, 0:TT], AF.Sigmoid, bias=bg[:, d, 0, ch:ch + 1])
                        p.act(it[:, sl], pr[:, 512:512 + TT], AF.Sigmoid, bias=bg[:, d, 1, ch:ch + 1])
                    p.act(at[:, 0:T], rt[:, 0:T], AF.Exp, scale=cv[:, d, ch:ch + 1])
                    p.tt(a2[:, 0:T], at[:, 0:T], at[:, 0:T], ALU.mult, eng="pool")
                    p.act(a2[:, 0:T], a2[:, 0:T], AF.Sqrt, scale=-1.0, bias=1.0)
                    p.tt(bt[:, 0:T], it[:, 0:T], xc[:, 0:T], ALU.mult)
                    p.tt(bt[:, 0:T], bt[:, 0:T], a2[:, 0:T], ALU.mult)
                    init = 0.0 if s == "ctx" else g.stl[:, b, ch, d:d + 1]
                    h = hd[d]
                    if d == 0:
                        p.scan(h[:, 0:T], at[:, 0:T], bt[:, 0:T], init)
                    else:
                        p.scan(rev(h[:, 0:T]), rev(at[:, 0:T]), rev(bt[:, 0:T]), init)
                    if s == "ctx":
                        col = T - 1 if d == 0 else 0
                        p.copy(g.stl[:, b, ch, d:d + 1], h[:, col:col + 1])
                if emit:
                    p.act(ay[:, 0:T], ay[:, 0:T], AF.Gelu_apprx_tanh)
                    p.tt(hd[0][:, 0:T], hd[0][:, 0:T], hd[1][:, 0:T], ALU.add)
                    p.tt(yo[:, 0:T], hd[0][:, 0:T], ay[:, 0:T], ALU.mult)
                    p.dma("sp", g.ya[s][b, ch * 128:(ch + 1) * 128, :], yo[:, 0:T])
    p.end_phase()


def ins(ap, axis, n):
    a = [list(x) for x in ap.ap]
    a.insert(axis, [0, n])
    return bass.AP(ap.tensor, ap.offset, a)


PI = math.pi


def phase_s5(g, l, last):
    p, NB, W = g.p, g.NB, g.W
    TA = CTX + SEQ
    seqs = (("ctx", 0, CTX), ("lat", CTX, SEQ))
    iota = g.cst[:, C_IOTA:C_IOTA + SEQ]
    M = p.sb("s5_M", [128, 4, 8])
    p.memset(M[:, :, :], 0.0)
    for rr in range(4):
        p.memset(M[0:64, rr, 2 * rr:2 * rr + 1], 1.0)
        p.memset(M[64:128, rr, 2 * rr + 1:2 * rr + 2], 1.0)
    prm = []
    for d in range(2):
        t = {}
        for nm in ("lre", "lim", "dt", "mag", "th", "cth", "sth", "ar", "ai", "fr", "fi", "t0", "t1", "t2"):
            t[nm] = p.sb("s5_%s%d" % (nm, d), [128, 16])
        base = (l * 2 + d) * 32 * 64
        for gg in range(2):
            p.dma("sp", t["lre"][gg * 64:(gg + 1) * 64, :], dap(W["s5_lam_re"], base + gg * 64, [[1, 64], [128, 16]]),
                  allow_slow_non_contiguous=True)
            p.dma("sp", t["lim"][gg * 64:(gg + 1) * 64, :], dap(W["s5_lam_im"], base + gg * 64, [[1, 64], [128, 16]]),
                  allow_slow_non_contiguous=True)
            p.dma("sp", t["dt"][gg * 64:(gg + 1) * 64, :], dap(W["s5_log_dt"], (l * 2 + d) * 32 + gg, [[0, 64], [2, 16]]),
                  allow_slow_non_contiguous=True)
        p.act(t["dt"][:, :], t["dt"][:, :], AF.Exp)
        p.tt(t["t0"][:, :], t["lre"][:, :], t["dt"][:, :], ALU.mult)
        p.act(t["mag"][:, :], t["t0"][:, :], AF.Exp)
        p.tt(t["th"][:, :], t["lim"][:, :], t["dt"][:, :], ALU.mult)
        ki = p.sb("s5_ki%d" % d, [128, 16], mybir.dt.int32)
        p.ts(t["t0"][:, :], t["th"][:, :], 1.0 / (2 * PI), None, op0=ALU.mult)
        p.copy(ki[:, :], t["t0"][:, :])
        p.copy(t["t1"][:, :], ki[:, :])
        p.stt(t["t0"][:, :], t["t1"][:, :], -2 * PI, t["th"][:, :], ALU.mult, ALU.add)
        p.ts(t["t1"][:, :], t["t0"][:, :], PI, None, op0=ALU.is_gt)
        p.stt(t["t0"][:, :], t["t1"][:, :], -2 * PI, t["t0"][:, :], ALU.mult, ALU.add)
        p.ts(t["t1"][:, :], t["t0"][:, :], -PI, None, op0=ALU.is_lt)
        p.stt(t["t0"][:, :], t["t1"][:, :], 2 * PI, t["t0"][:, :], ALU.mult, ALU.add)
        p.act(t["sth"][:, :], t["t0"][:, :], AF.Sin)
        p.ts(t["t0"][:, :], t["t0"][:, :], 0.5 * PI, None, op0=ALU.add)
        p.ts(t["t1"][:, :], t["t0"][:, :], PI, None, op0=ALU.is_gt)
        p.stt(t["t0"][:, :], t["t1"][:, :], -2 * PI, t["t0"][:, :], ALU.mult, ALU.add)
        p.act(t["cth"][:, :], t["t0"][:, :], AF.Sin)
        p.tt(t["ar"][:, :], t["mag"][:, :], t["cth"][:, :], ALU.mult)
        p.tt(t["ai"][:, :], t["mag"][:, :], t["sth"][:, :], ALU.mult)
        p.tt(t["t0"][:, :], t["lre"][:, :], t["lre"][:, :], ALU.mult)
        p.tt(t["t1"][:, :], t["lim"][:, :], t["lim"][:, :], ALU.mult)
        p.tt(t["t0"][:, :], t["t0"][:, :], t["t1"][:, :], ALU.add)
        p.recip(t["t0"][:, :], t["t0"][:, :])
        p.ts(t["t1"][:, :], t["ar"][:, :], -1.0, None, op0=ALU.add)
        p.tt(t["fr"][:, :], t["t1"][:, :], t["lre"][:, :], ALU.mult)
        p.tt(t["t2"][:, :], t["ai"][:, :], t["lim"][:, :], ALU.mult)
        p.tt(t["fr"][:, :], t["fr"][:, :], t["t2"][:, :], ALU.add)
        p.tt(t["fr"][:, :], t["fr"][:, :], t["t0"][:, :], ALU.mult)
        p.tt(t["fi"][:, :], t["ai"][:, :], t["lre"][:, :], ALU.mult)
        p.tt(t["t2"][:, :], t["t1"][:, :], t["lim"][:, :], ALU.mult)
        p.tt(t["fi"][:, :], t["fi"][:, :], t["t2"][:, :], ALU.subtract)
        p.tt(t["fi"][:, :], t["fi"][:, :], t["t0"][:, :], ALU.mult)
        prm.append(t)
    dsk = p.sb("s5_dsk", [128, 4])
    p.dma("sp", dsk[:, :], dap(W["s5_d"], l * 512, [[1, 128], [128, 4]]), allow_slow_non_contiguous=True)
    uc = p.sb("s5_u", [128, NB, TA])
    yacc = p.sb("s5_y", [128, NB, TA])
    cosT = p.sb("s5_cos", [128, SEQ])
    sinT = p.sb("s5_sin", [128, SEQ])
    tmp = p.sb("s5_tmp", [128, SEQ])
    xr = p.sb("s5_xr", [128, SEQ])
    xi = p.sb("s5_xi", [128, SEQ])
    wr = p.sb("s5_wr", [128, SEQ])
    wi = p.sb("s5_wi", [128, SEQ])
    t3 = p.sb("s5_t3", [128, SEQ])
    t4 = p.sb("s5_t4", [128, SEQ])
    braw = [p.sb("s5_braw%d" % i, [128, 4, 16]) for i in range(2)]
    craw = [p.sb("s5_craw%d" % i, [128, 4, 16]) for i in range(2)]
    bbs = [p.sb("s5_bbs%d" % i, [128, 4, 16]) for i in range(2)]
    tb = p.sb("s5_tb", [128, 4, 16])
    E = p.sb("s5_E", [128, 8, 16])
    BbT = p.sb("s5_BbT", [128, 2, 4, 2, 128])
    CT = p.sb("s5_CT", [128, 2, 4, 2, 8, 16])
    ini = p.sb("s5_ini", [128, 4])
    cs2 = p.sb("s5_cs2", [128, 8])
    for c in range(4):
        for d in range(2):
            t = prm[d]
            base = (l * 2 + d) * 32 * 1024
            for gg in range(2):
                for r4 in range(4):
                    for (dst, nm) in ((braw[0], "s5_b_re"), (braw[1], "s5_b_im")):
                        p.dma("sp", dst[gg * 64:(gg + 1) * 64, r4, :],
                              dap(W[nm], base + (8 * c + 2 * r4 + gg) * 1024, [[16, 64], [1, 16]]))
                    for (dst, nm) in ((craw[0], "s5_c_re"), (craw[1], "s5_c_im")):
                        p.dma("sp", dst[gg * 64:(gg + 1) * 64, r4, :],
                              dap(W[nm], base + (8 * c + 2 * r4 + gg) * 1024, [[1, 64], [64, 16]]),
                              allow_slow_non_contiguous=True)
            frb = ins(t["fr"][:, 4 * c:4 * c + 4], 2, 16)
            fib = ins(t["fi"][:, 4 * c:4 * c + 4], 2, 16)
            p.tt(bbs[0][:, :, :], braw[0][:, :, :], frb, ALU.mult)
            p.tt(tb[:, :, :], braw[1][:, :, :], fib, ALU.mult)
            p.tt(bbs[0][:, :, :], bbs[0][:, :, :], tb[:, :, :], ALU.subtract)
            p.tt(bbs[1][:, :, :], braw[1][:, :, :], frb, ALU.mult)
            p.tt(tb[:, :, :], braw[0][:, :, :], fib, ALU.mult)
            p.tt(bbs[1][:, :, :], bbs[1][:, :, :], tb[:, :, :], ALU.add)
            for r4 in range(4):
                mk = ins(M[:, r4, :], 2, 16)
                for ri in range(2):
                    p.tt(E[:, :, :], ins(bbs[ri][:, r4, :], 1, 8), mk, ALU.mult)
                    pt = g.ps()
                    p.tr(pt[:, 0:128], E[:, :, :].rearrange("p a b -> p (a b)"), g.ident)
                    p.copy(BbT[:, d, r4, ri, :], pt[:, 0:128])
                p.tt(CT[:, d, r4, 0, :, :], ins(craw[0][:, r4, :], 1, 8), mk, ALU.mult)
                p.stt(CT[:, d, r4, 1, :, :], ins(craw[1][:, r4, :], 1, 8), -1.0, mk, ALU.mult, ALU.mult)
        for b in range(NB):
            for (s, o, T) in seqs:
                p.dma("sp", uc[:, b, o:o + T], g.proj[s][b][OFF_U + c * 128:OFF_U + (c + 1) * 128, :])
        p.ts(yacc[:, :, :], uc[:, :, :], dsk[:, c:c + 1], None, op0=ALU.mult)
        for d in range(2):
            t = prm[d]
            for r4 in range(4):
                r = 4 * c + r4
                p.memset(cosT[:, 0:1], 1.0)
                p.memset(sinT[:, 0:1], 0.0)
                p.copy(cs2[:, 0:1], t["cth"][:, r:r + 1])
                p.copy(cs2[:, 1:2], t["sth"][:, r:r + 1])
                n_ = 1
                while n_ < SEQ:
                    p.ts(cs2[:, 2:3], cs2[:, 1:2], -1.0, None, op0=ALU.mult)
                    p.ts(cosT[:, n_:2 * n_], cosT[:, 0:n_], cs2[:, 0:1], None, op0=ALU.mult)
                    p.stt(cosT[:, n_:2 * n_], sinT[:, 0:n_], cs2[:, 2:3], cosT[:, n_:2 * n_], ALU.mult, ALU.add)
                    p.ts(sinT[:, n_:2 * n_], sinT[:, 0:n_], cs2[:, 0:1], None, op0=ALU.mult)
                    p.stt(sinT[:, n_:2 * n_], cosT[:, 0:n_], cs2[:, 1:2], sinT[:, n_:2 * n_], ALU.mult, ALU.add)
                    n_ *= 2
                    if n_ < SEQ:
                        p.tt(cs2[:, 3:4], cs2[:, 0:1], cs2[:, 1:2], ALU.mult)
                        p.tt(cs2[:, 4:5], cs2[:, 0:1], cs2[:, 0:1], ALU.mult)
                        p.tt(cs2[:, 5:6], cs2[:, 1:2], cs2[:, 1:2], ALU.mult)
                        p.tt(cs2[:, 0:1], cs2[:, 4:5], cs2[:, 5:6], ALU.subtract)
                        p.ts(cs2[:, 1:2], cs2[:, 3:4], 2.0, None, op0=ALU.mult)
                magb = ins(t["mag"][:, r:r + 1], 1, SEQ)
                for b in range(NB):
                    for (s, o, T) in seqs:
                        TT = min(T, 512)
                        fw = (d == 0)
                        cs = cosT[:, 0:T] if fw else rev(cosT[:, 0:T])
                        sn = sinT[:, 0:T] if fw else rev(sinT[:, 0:T])
                        for tt in range(T // TT):
                            sl = slice(tt * TT, (tt + 1) * TT)
                            pt = g.ps()
                            p.mm(pt[:, 0:TT], BbT[:, d, r4, 0, :], uc[:, b, o + tt * TT:o + (tt + 1) * TT])
                            p.mm(pt[:, 512:512 + TT], BbT[:, d, r4, 1, :], uc[:, b, o + tt * TT:o + (tt + 1) * TT])
                            p.act(xr[:, sl], pt[:, 0:TT], AF.Copy)
                            p.act(xi[:, sl], pt[:, 512:512 + TT], AF.Copy)
                        X, Y = xr[:, 0:T], xi[:, 0:T]
                        p.tt(wr[:, 0:T], X, cs, ALU.mult)
                        p.tt(t3[:, 0:T], Y, sn, ALU.mult, eng="pool")
                        p.tt(wr[:, 0:T], wr[:, 0:T], t3[:, 0:T], ALU.add)
                        p.tt(wi[:, 0:T], Y, cs, ALU.mult, eng="pool")
                        p.tt(t4[:, 0:T], X, sn, ALU.mult)
                        p.tt(wi[:, 0:T], wi[:, 0:T], t4[:, 0:T], ALU.subtract, eng="pool")
                        if s == "ctx":
                            ir, ii = 0.0, 0.0
                        else:
                            h0 = g.sts5[:, b, d, r, :]
                            p.tt(ini[:, 0:1], h0[:, 0:1], t["cth"][:, r:r + 1], ALU.mult)
                            p.tt(ini[:, 1:2], h0[:, 1:2], t["sth"][:, r:r + 1], ALU.mult)
                            p.tt(ini[:, 0:1], ini[:, 0:1], ini[:, 1:2], ALU.subtract)
                            p.tt(ini[:, 2:3], h0[:, 0:1], t["sth"][:, r:r + 1], ALU.mult)
                            p.tt(ini[:, 3:4], h0[:, 1:2], t["cth"][:, r:r + 1], ALU.mult)
                            p.tt(ini[:, 2:3], ini[:, 2:3], ini[:, 3:4], ALU.add)
                            ir, ii = ini[:, 0:1], ini[:, 2:3]
                        mb = ins(t["mag"][:, r:r + 1], 1, T)
                        mb = dap(t["mag"], t["mag"][:, r:r + 1].offset, [list(t["mag"][:, r:r + 1].ap[0]), [0, T]])
                        if fw:
                            p.scan(xr[:, 0:T], mb, wr[:, 0:T], ir)
                            p.scan(xi[:, 0:T], mb, wi[:, 0:T], ii)
                        else:
                            p.scan(rev(xr[:, 0:T]), mb, rev(wr[:, 0:T]), ir)
                            p.scan(rev(xi[:, 0:T]), mb, rev(wi[:, 0:T]), ii)
                        p.tt(wr[:, 0:T], X, cs, ALU.mult)
                        p.tt(t3[:, 0:T], Y, sn, ALU.mult, eng="pool")
                        p.tt(wr[:, 0:T], wr[:, 0:T], t3[:, 0:T], ALU.subtract)
                        p.tt(wi[:, 0:T], Y, cs, ALU.mult, eng="pool")
                        p.tt(t4[:, 0:T], X, sn, ALU.mult)
                        p.tt(wi[:, 0:T], wi[:, 0:T], t4[:, 0:T], ALU.add, eng="pool")
                        if s == "ctx":
                            col = T - 1 if fw else 0
                            p.copy(g.sts5[:, b, d, r, 0:1], wr[:, col:col + 1])
                            p.copy(g.sts5[:, b, d, r, 1:2], wi[:, col:col + 1])
                        if last and s == "ctx":
                            continue
                        for tt in range(T // TT):
                            sl = slice(tt * TT, (tt + 1) * TT)
                            pt = g.ps()
                            p.mm(pt[:, 0:TT], CT[:, d, r4, 0, :, :].rearrange("p a b -> p (a b)"), wr[:, sl],
                                 start=True, stop=False)
                            p.mm(pt[:, 0:TT], CT[:, d, r4, 1, :, :].rearrange("p a b -> p (a b)"), wi[:, sl],
                                 start=False, stop=True)
                            ya = yacc[:, b, o + tt * TT:o + (tt + 1) * TT]
                            p.tt(ya, ya, pt[:, 0:TT], ALU.add)
        p.act(yacc[:, :, :], yacc[:, :, :], AF.Gelu_apprx_tanh)
        for b in range(NB):
            for (s, o, T) in seqs:
                if last and s == "ctx":
                    continue
                p.dma("sp", g.ys5[s][b, c * 128:(c + 1) * 128, :], yacc[:, b, o:o + T])
    p.end_phase()
    wg = p.sb("s5_wg", [128, 4, 512], BF16)
    p.dma("pool", wg[:, :, :], dap(W["s5_w_glu"], l * 512 * 512, [[512, 128], [128 * 512, 4], [1, 512]]))
    bgl = p.sb("s5_bgl", [128, 4])
    p.dma("sp", bgl[:, :], dap(W["s5_b_glu"], l * 512, [[1, 128], [128, 4]]), allow_slow_non_contiguous=True)
    yg = [p.sb("s5_yg%d" % i, [128, 4, 512]) for i in range(2)]
    ygb = [p.sb("s5_ygb%d" % i, [128, 4, 512], BF16) for i in range(2)]
    sg = [p.sb("s5_sg%d" % i, [128, 512]) for i in range(2)]
    yo = [p.sb("s5_yo%d" % i, [128, 512], BF16) for i in range(2)]
    it = 0
    for b in range(NB):
        for (s, o, T) in seqs:
            if last and s == "ctx":
                continue
            TT = min(T, 512)
            for tt in range(T // TT):
                y_, yb_ = yg[it % 2], ygb[it % 2]
                it += 1
                src = g.ys5[s][b, :, tt * TT:(tt + 1) * TT]
                p.dma("sp", y_[:, :, 0:TT], dap(src, src.offset, [[T, 128], [128 * T, 4], [1, TT]]))
                p.copy(yb_[:, :, 0:TT], y_[:, :, 0:TT], eng="pool")
                for oc in range(4):
                    pt = g.ps()
                    for kc in range(4):
                        p.mm(pt[:, 0:TT], wg[:, kc, oc * 128:(oc + 1) * 128], yb_[:, kc, 0:TT], start=(kc == 0),
                             stop=(kc == 3))
                    s_, o_ = sg[oc % 2], yo[oc % 2]
                    p.act(s_[:, 0:TT], pt[:, 0:TT], AF.Sigmoid, bias=bgl[:, oc:oc + 1])
                    p.tt(o_[:, 0:TT], y_[:, oc, 0:TT], s_[:, 0:TT], ALU.mult)
                    p.dma("sp", g.yc[s][b, oc * 128:(oc + 1) * 128, tt * TT:(tt + 1) * TT], o_[:, 0:TT])
    p.end_phase()


def phase_dn(g, l, last):
    p, NB, W = g.p, g.NB, g.W
    seqs = (("ctx", CTX), ("lat", SEQ))
    ident64 = g.cst[0:64, C_ID:C_ID + 64]
    ones = g.ones
    cwd = p.sb("dn_cw", [128, 4, 24])
    p.dma("sp", cwd[:, :, :], dap(W["dn_conv_w"], l * 4 * 3072, [[1, 128], [3072, 4], [128, 24]]),
          allow_slow_non_contiguous=True)
    raw = [p.sb("dn_raw%d" % i, [128, SEQ]) for i in range(2)]
    xc = [p.sb("dn_xc%d" % i, [128, SEQ]) for i in range(2)]
    sq = p.sb("dn_sq", [128, SEQ])
    rn = p.sb("dn_rn", [128, SEQ])
    it = 0
    for b in range(NB):
        for (s, T) in seqs:
            L = 64 if s == "lat" else T
            TT = min(T, 512)
            for j in range(24):
                rw, x_ = raw[it % 2], xc[it % 2]
                it += 1
                rows = g.proj[s][b][OFF_Q + j * 128:OFF_Q + (j + 1) * 128, :]
                p.dma("sp", rw[:, 0:T], rows)
                a3 = rw[:, 0:T].rearrange("p (r l) -> p r l", l=L)
                x3 = x_[:, 0:T].rearrange("p (r l) -> p r l", l=L)
                p.ts(x_[:, 0:T], rw[:, 0:T], cwd[:, 2, j:j + 1], None, op0=ALU.mult)
                p.stt(x3[:, :, 2:L], a3[:, :, 0:L - 2], cwd[:, 0, j:j + 1], x3[:, :, 2:L], ALU.mult, ALU.add)
                p.stt(x3[:, :, 1:L], a3[:, :, 0:L - 1], cwd[:, 1, j:j + 1], x3[:, :, 1:L], ALU.mult, ALU.add)
                p.stt(x3[:, :, 0:L - 1], a3[:, :, 1:L], cwd[:, 3, j:j + 1], x3[:, :, 0:L - 1], ALU.mult, ALU.add)
                p.act(x_[:, 0:T], x_[:, 0:T], AF.Silu)
                if j < 16:
                    p.tt(sq[:, 0:T], x_[:, 0:T], x_[:, 0:T], ALU.mult, eng="pool")
                    for tt in range(T // TT):
                        sl = slice(tt * TT, (tt + 1) * TT)
                        pt = g.ps()
                        p.mm(pt[:, 0:TT], ones, sq[:, sl])
                        p.act(rn[:, sl], pt[:, 0:TT], AF.Sqrt, bias=g.epsc[:, 0:1])
                    p.recip(rn[:, 0:T], rn[:, 0:T])
                    sc = (128.0 ** -0.5) if j < 8 else 1.0
                    p.stt(x_[:, 0:T], x_[:, 0:T], sc, rn[:, 0:T], ALU.mult, ALU.mult)
                p.dma("sp", rows, x_[:, 0:T])
    p.end_phase()
    CB = 4
    NCH = SEQ // 64
    alg = p.sb("dn_alg", [64, 16])
    dtb = p.sb("dn_dtb", [64, 16])
    p.dma("sp", alg[:, :], dap(W["dn_a_log"], l * 16, [[0, 64], [1, 16]]))
    p.dma("sp", dtb[:, :], dap(W["dn_dt_bias"], l * 16, [[0, 64], [1, 16]]))
    p.act(alg[:, :], alg[:, :], AF.Exp)
    p.ts(alg[:, :], alg[:, :], -1.0, None, op0=ALU.mult)
    bt = p.sb("dn_bt", [64, NCH, 32])
    bet = p.sb("dn_bet", [64, NCH, 16])
    gt = p.sb("dn_gt", [64, NCH, 16])
    qkv = [[p.sb("dn_%s%d" % (nm, i), [128, 8, CB * 64]) for nm in "qkv"] for i in range(2)]
    S8 = p.sb("dn_S", [128, 8, 128])
    stdn = p.sb("dn_st", [128, 2, 8, 128])
    sm = p.sb("dn_sm", [64, 4, 8])
    gtot = p.sb("dn_gtot", [128, 8])
    gL = p.sb("dn_gL", [64, 8, 64])
    X = p.sb("dn_X", [64, 8, 64])
    D8 = p.sb("dn_D8", [64, 8, 64])
    DT8 = p.sb("dn_DT8", [64, 8, 64])
    P1 = p.sb("dn_P1", [64, 8, 64])
    P2 = p.sb("dn_P2", [64, 8, 64])
    bD = p.sb("dn_bD", [64, 8, 64])
    AtT = p.sb("dn_AtT", [64, 8, 64])
    Nk = [p.sb("dn_N%d" % i, [64, 8, 64]) for i in range(2)]
    YR = [p.sb("dn_YR%d" % i, [64, 8, 2, 64]) for i in range(2)]
    Vb8 = p.sb("dn_Vb", [64, 8, 128])
    Kbg8 = p.sb("dn_Kbg", [64, 8, 128])
    Kd8 = p.sb("dn_Kd", [64, 8, 128])
    U8 = p.sb("dn_U", [64, 8, 128])
    Vn8 = p.sb("dn_Vn", [64, 8, 128])
    WT8 = p.sb("dn_WT", [128, 8, 64])
    Qd8 = p.sb("dn_Qd", [128, 8, 64])
    oc = [p.sb("dn_oc%d" % i, [128, 8, 64]) for i in range(2)]
    f3 = lambda ap: ap.rearrange("p a b -> p (a b)")
    for b in range(NB):
        for (s, T) in seqs:
            nch = T // 64
            src = g.proj[s][b][OFF_BETA, :]
            for n_ in range(nch):
                p.dma("sp", bt[:, n_, :], dap(src, src.offset + n_ * 64, [[1, 64], [T, 32]]), allow_slow_non_contiguous=True)
            p.act(bet[:, 0:nch, :], bt[:, 0:nch, 0:16], AF.Sigmoid)
            p.tt(gt[:, 0:nch, :], bt[:, 0:nch, 16:32], ins(dtb[:, :], 1, nch), ALU.add)
            p.act(gt[:, 0:nch, :], gt[:, 0:nch, :], AF.Exp)
            p.act(gt[:, 0:nch, :], gt[:, 0:nch, :], AF.Ln, bias=1.0)
            p.tt(gt[:, 0:nch, :], gt[:, 0:nch, :], ins(alg[:, :], 1, nch), ALU.mult)
            for d in range(2):
                fw = (d == 0)
                LTc = g.cst[0:64, C_LTF:C_LTF + 64] if fw else g.cst[0:64, C_LTB:C_LTB + 64]
                Mi = g.cst[0:64, C_MGT:C_MGT + 64] if fw else g.cst[0:64, C_MLT:C_MLT + 64]
                MT = g.cst[0:64, C_MLT:C_MLT + 64] if fw else g.cst[0:64, C_MGT:C_MGT + 64]
                Si = g.cst[0:64, C_SLT:C_SLT + 64] if fw else g.cst[0:64, C_SGT:C_SGT + 64]
                ST = g.cst[0:64, C_SGT:C_SGT + 64] if fw else g.cst[0:64, C_SLT:C_SLT + 64]
                if s == "ctx":
                    p.memset(S8[:, :, :], 0.0)
                else:
                    p.copy(S8[:, :, :], stdn[:, d, :, :])
                order = list(range(nch)) if fw else list(range(nch - 1, -1, -1))
                cur_blk = None
                for ci, n in enumerate(order):
                    blk = n // CB
                    if blk != cur_blk:
                        cur_blk = blk
                        qb = qkv[(ci // CB) % 2]
                        nb_ = min(CB, nch - blk * CB)
                        for qi, off in enumerate((OFF_Q, OFF_K, OFF_V)):
                            sr = g.proj[s][b][off, blk * CB * 64]
                            p.dma("sp", qb[qi][:, :, 0:nb_ * 64],
                                  dap(sr, sr.offset, [[T, 128], [128 * T, 8], [1, nb_ * 64]]))
                    c0 = (n - blk * CB) * 64
                    qT, kT, vT = (qb[i][:, :, c0:c0 + 64] for i in range(3))
                    g8 = gt[:, n, d * 8:(d + 1) * 8]
                    be8 = bet[:, n, d * 8:(d + 1) * 8]
                    pt = g.ps()
                    p.mm(pt[0:64, 0:8], LTc, g8)
                    p.mm(pt[0:64, 8:16], ones[0:64, 0:64], g8)
                    p.mm(pt[:, 16:24], ones[0:64, :], g8)
                    Gc, Gam, Kdsc, bg = sm[:, 0, :], sm[:, 1, :], sm[:, 2, :], sm[:, 3, :]
                    p.copy(Gc, pt[0:64, 0:8])
                    p.act(Gam, pt[0:64, 0:8], AF.Exp)
                    p.tt(Kdsc, pt[0:64, 8:16], Gc, ALU.subtract)
                    p.act(Kdsc, Kdsc, AF.Exp)
                    p.act(gtot[:, :], pt[:, 16:24], AF.Exp)
                    p.tt(bg, be8, Gam, ALU.mult)
                    p.tt(gL[:, :, :], ins(g8, 2, 64), ins(LTc, 1, 8), ALU.mult)
                    pG = g.ps()
                    p.mm(pG[0:64, 0:512], ones[0:64, 0:64], f3(gL[:, :, :]))
                    p.tt(X[:, :, :], ins(Gc, 2, 64), pG[0:64, 0:512].rearrange("p (a b) -> p a b", b=64), ALU.subtract)
                    p.tt(D8[:, :, :], X[:, :, :], ins(Mi, 1, 8), ALU.subtract)
                    p.act(D8[:, :, :], D8[:, :, :], AF.Exp)
                    p.stt(DT8[:, :, :], X[:, :, :], -1.0, ins(MT, 1, 8), ALU.mult, ALU.subtract)
                    p.act(DT8[:, :, :], DT8[:, :, :], AF.Exp)
                    pK = g.ps()
                    for h in range(8):
                        p.mm(pK[0:64, h * 64:(h + 1) * 64], kT[:, h, :], kT[:, h, :])
                        p.mm(pK[0:64, 512 + h * 64:512 + (h + 1) * 64], kT[:, h, :], qT[:, h, :])
                    pKK = pK[0:64, 0:512].rearrange("p (a b) -> p a b", b=64)
                    pKQ = pK[0:64, 512:1024].rearrange("p (a b) -> p a b", b=64)
                    p.tt(P1[:, :, :], D8[:, :, :], ins(Si, 1, 8), ALU.mult)
                    p.tt(P1[:, :, :], P1[:, :, :], ins(be8, 2, 64), ALU.mult)
                    p.stt(Nk[0][:, :, :], pKK, -1.0, P1[:, :, :], ALU.mult, ALU.mult)
                    p.tt(bD[:, :, :], ins(be8, 2, 64), ins(ident64, 1, 8), ALU.mult)
                    pB = g.ps()
                    p.mm(pB[0:64, 0:512], ones[0:64, 0:64], f3(bD[:, :, :]))
                    p.tt(P2[:, :, :], DT8[:, :, :], ins(ST, 1, 8), ALU.mult)
                    p.tt(P2[:, :, :], P2[:, :, :], pB[0:64, 0:512].rearrange("p (a b) -> p a b", b=64), ALU.mult)
                    p.stt(YR[0][:, :, 0, :], pKK, -1.0, P2[:, :, :], ALU.mult, ALU.mult)
                    p.copy(YR[0][:, :, 1, :], ins(ident64, 1, 8))
                    p.tt(AtT[:, :, :], pKQ, DT8[:, :, :], ALU.mult)
                    for k in range(1, 7):
                        a_, b_ = (k - 1) % 2, k % 2
                        pA = g.ps()
                        for h in range(8):
                            p.mm(pA[0:64, h * 128:(h + 1) * 128], Nk[a_][:, h, :],
                                 YR[a_][:, h, :, :].rearrange("p a b -> p (a b)"))
                        pA4 = pA[0:64, :].rearrange("p (h t c) -> p h t c", t=2, c=64)
                        if k <= 5:
                            pN = g.ps()
                            for h in range(8):
                                p.mm(pN[0:64, h * 64:(h + 1) * 64], YR[a_][:, h, 0, :], Nk[a_][:, h, :])
                            p.act(YR[b_][:, :, 0, :], pA4[:, :, 0, :], AF.Copy)
                        p.tt(YR[b_][:, :, 1, :], pA4[:, :, 1, :], YR[a_][:, :, 1, :], ALU.add)
                        if k <= 5:
                            p.act(Nk[b_][:, :, :], pN[0:64, 0:512].rearrange("p (a b) -> p a b", b=64), AF.Copy)
                    R = YR[0][:, :, 1, :]
                    pTk = g.ps()
                    pTv = g.ps()
                    for h in range(8):
                        p.tr(pTk[0:64, h * 128:(h + 1) * 128], kT[:, h, :], g.ident)
                        p.tr(pTv[0:64, h * 128:(h + 1) * 128], vT[:, h, :], g.ident)
                    pTk3 = pTk[0:64, :].rearrange("p (a b) -> p a b", b=128)
                    pTv3 = pTv[0:64, :].rearrange("p (a b) -> p a b", b=128)
                    p.tt(Vb8[:, :, :], pTv3, ins(be8, 2, 128), ALU.mult)
                    p.tt(Kbg8[:, :, :], pTk3, ins(bg, 2, 128), ALU.mult)
                    p.tt(Kd8[:, :, :], pTk3, ins(Kdsc, 2, 128), ALU.mult)
                    pU = g.ps()
                    pW = g.ps()
                    for h in range(8):
                        p.mm(pU[0:64, h * 128:(h + 1) * 128], R[:, h, :], Vb8[:, h, :])
                        p.mm(pW[:, h * 64:(h + 1) * 64], Kbg8[:, h, :], R[:, h, :])
                    p.act(f3(U8[:, :, :]), pU[0:64, :], AF.Copy)
                    p.act(f3(WT8[:, :, :]), pW[:, 0:512], AF.Copy)
                    p.tt(bD[:, :, :], ins(Gam, 2, 64), ins(ident64, 1, 8), ALU.mult)
                    pGm = g.ps()
                    p.mm(pGm[:, 0:512], ones[0:64, :], f3(bD[:, :, :]))
                    p.tt(Qd8[:, :, :], qT, pGm[:, 0:512].rearrange("p (a b) -> p a b", b=64), ALU.mult)
                    pWS = g.ps()
                    for h in range(8):
                        p.mm(pWS[0:64, h * 128:(h + 1) * 128], WT8[:, h, :], S8[:, h, :])
                    p.tt(f3(Vn8[:, :, :]), f3(U8[:, :, :]), pWS[0:64, :], ALU.subtract)
                    pO = g.ps()
                    for h in range(8):
                        p.mm(pO[:, h * 64:(h + 1) * 64], S8[:, h, :], Qd8[:, h, :], start=True, stop=False)
                        p.mm(pO[:, h * 64:(h + 1) * 64], Vn8[:, h, :], AtT[:, h, :], start=False, stop=True)
                    if not (last and s == "ctx"):
                        o_ = oc[ci % 2]
                        p.act(f3(o_[:, :, :]), pO[:, 0:512], AF.Copy)
                        dst = g.odn[s][d, b, 0, n * 64]
                        p.dma("sp", dap(dst, dst.offset, [[T, 128], [128 * T, 8], [1, 64]]), o_[:, :, :])
                    pdS = g.ps()
                    for h in range(8):
                        p.mm(pdS[:, h * 128:(h + 1) * 128], Kd8[:, h, :], Vn8[:, h, :])
                    p.tt(S8[:, :, :], S8[:, :, :], ins(gtot[:, :], 2, 128), ALU.mult)
                    p.tt(f3(S8[:, :, :]), f3(S8[:, :, :]), pdS[:, :], ALU.add)
                if s == "ctx":
                    p.copy(stdn[:, d, :, :], S8[:, :, :])
    p.end_phase()
    ng = p.sb("dn_ng", [128, 1])
    p.dma("sp", ng[:, :], dap(W["dn_norm_g"], l * 128, [[1, 128], [1, 1]]))
    of = [p.sb("dn_of%d" % i, [128, SEQ]) for i in range(2)]
    ob = [p.sb("dn_ob%d" % i, [128, SEQ]) for i in range(2)]
    zt = [p.sb("dn_z%d" % i, [128, SEQ]) for i in range(2)]
    yo = [p.sb("dn_yo%d" % i, [128, SEQ], BF16) for i in range(2)]
    rs = p.sb("dn_rs", [128, SEQ])
    it = 0
    for b in range(NB):
        for (s, T) in seqs:
            if last and s == "ctx":
                continue
            TT = min(T, 512)
            for h in range(8):
                o1, o2, z_, y_ = of[it % 2], ob[it % 2], zt[it % 2], yo[it % 2]
                it += 1
                p.dma("sp", o1[:, 0:T], g.odn[s][0, b, h * 128:(h + 1) * 128, :])
                p.dma("sp", o2[:, 0:T], g.odn[s][1, b, h * 128:(h + 1) * 128, :])
                p.dma("sp", z_[:, 0:T], g.proj[s][b][OFF_Z + h * 128:OFF_Z + (h + 1) * 128, :])
                p.tt(o1[:, 0:T], o1[:, 0:T], o2[:, 0:T], ALU.add)
                p.tt(o2[:, 0:T], o1[:, 0:T], o1[:, 0:T], ALU.mult, eng="pool")
                for tt in range(T // TT):
                    sl = slice(tt * TT, (tt + 1) * TT)
                    pt = g.ps()
                    p.mm(pt[:, 0:TT], ones, o2[:, sl])
                    p.act(rs[:, sl], pt[:, 0:TT], AF.Sqrt, scale=1.0 / 128, bias=g.epsc[:, 0:1])
                p.recip(rs[:, 0:T], rs[:, 0:T])
                p.act(z_[:, 0:T], z_[:, 0:T], AF.Silu)
                p.stt(o1[:, 0:T], o1[:, 0:T], ng[:, 0:1], rs[:, 0:T], ALU.mult, ALU.mult)
                p.tt(y_[:, 0:T], o1[:, 0:T], z_[:, 0:T], ALU.mult)
                p.dma("sp", g.yb[s][b, h * 128:(h + 1) * 128, :], y_[:, 0:T])
    p.end_phase()


def phase_merge(g, l, last):
    p, NB, W = g.p, g.NB, g.W
    wa = p.sb("mg_wa", [128, 4, D], BF16)
    wb = p.sb("mg_wb", [128, 8, D], BF16)
    wc = p.sb("mg_wc", [128, 4, D], BF16)
    wo = p.sb("mg_wo", [128, 8, D], BF16)
    import os
    MGS = int(os.environ.get("MG_STOP", "9"))
    for hh in range(2):
        cs = slice(hh * 512, (hh + 1) * 512)
        p.dma("pool", wa[:, :, cs], dap(W["w_br_a"], l * 512 * D + hh * 512, [[D, 128], [128 * D, 4], [1, 512]]))
        p.dma("pool", wb[:, :, cs], dap(W["w_br_b"], l * D * D + hh * 512, [[D, 128], [128 * D, 8], [1, 512]]))
        p.dma("pool", wc[:, :, cs], dap(W["w_br_c"], l * 512 * D + hh * 512, [[D, 128], [128 * D, 4], [1, 512]]))
        p.dma("pool", wo[:, :, cs], dap(W["w_out"], l * D * D + hh * 512, [[D, 128], [128 * D, 8], [1, 512]]))
    if MGS == 1:
        p.end_phase()
        return
    yat = p.sb("mg_ya", [128, 4, 512], BF16)
    ybt = p.sb("mg_yb", [128, 8, 512], BF16)
    yct = p.sb("mg_yc", [128, 4, 512], BF16)
    g3 = [p.sb("mg_g%d" % i, [128, 3, 512]) for i in range(2)]
    m32 = p.sb("mg_m", [128, 512])
    t32 = p.sb("mg_t", [128, 512])
    mT = p.sb("mg_mT", [128, 8, 512], BF16)
    xt = [p.sb("mg_x%d" % i, [128, D]) for i in range(2)]
    tx = p.sb("mg_tx", [128, 512])
    xi = 0
    for b in range(NB):
        for s in ("ctx", "lat"):
            if last and s == "ctx":
                continue
            T = g.T[s]
            v = b if s == "lat" else NB
            GM = load_mod_bcast(g, l, v, 2, "mg_GM")
            TT = min(T, 512)
            for tt in range(T // TT):
                c0 = tt * TT
                for (dst, srcd, nk) in ((yat, g.ya, 4), (ybt, g.yb, 8), (yct, g.yc, 4)):
                    sr = srcd[s][b, 0, c0]
                    p.dma("sp", dst[:, :, 0:TT], dap(sr, sr.offset, [[T, 128], [128 * T, nk], [1, TT]]))
                for oc in range(8):
                    gt_ = g3[oc % 2]
                    for br in range(3):
                        r0 = OFF_GATE + br * D + oc * 128
                        p.dma("sp", gt_[:, br, 0:TT], g.proj[s][b][r0:r0 + 128, c0:c0 + TT])
                    p.act(gt_[:, :, 0:TT], gt_[:, :, 0:TT], AF.Sigmoid)
                    for br, (wt, yt, nk) in enumerate(((wa, yat, 4), (wb, ybt, 8), (wc, yct, 4))):
                        pt = g.ps()
                        for kc in range(nk):
                            p.mm(pt[:, 0:TT], wt[:, kc, oc * 128:(oc + 1) * 128], yt[:, kc, 0:TT], start=(kc == 0),
                                 stop=(kc == nk - 1))
                        if br == 0:
                            p.tt(m32[:, 0:TT], gt_[:, 0, 0:TT], pt[:, 0:TT], ALU.mult)
                        else:
                            p.tt(t32[:, 0:TT], gt_[:, br, 0:TT], pt[:, 0:TT], ALU.mult)
                            if br == 1:
                                p.tt(m32[:, 0:TT], m32[:, 0:TT], t32[:, 0:TT], ALU.add)
                            else:
                                p.tt(mT[:, oc, 0:TT], m32[:, 0:TT], t32[:, 0:TT], ALU.add)
                for st in range(TT // 128):
                    x_ = xt[xi % 2]
                    xi += 1
                    r0 = b * T + c0 + st * 128
                    p.dma("sp", x_[:, :], g.xs[s][r0:r0 + 128, :])
                    for half in range(2):
                        po = g.ps()
                        for kc in range(8):
                            p.mm(po[:, 0:512], mT[:, kc, st * 128:(st + 1) * 128], wo[:, kc, half * 512:(half + 1) * 512],
                                 start=(kc == 0), stop=(kc == 7))
                        p.tt(tx[:, :], po[:, 0:512], GM[:, half * 512:(half + 1) * 512], ALU.mult)
                        p.tt(x_[:, half * 512:(half + 1) * 512], x_[:, half * 512:(half + 1) * 512], tx[:, :], ALU.add)
                    p.dma("sp", g.xs[s][r0:r0 + 128, :], x_[:, :])
    p.end_phase()


MT_ = 512


def phase_moe(g, l, last):
    p, NB, W = g.p, g.NB, g.W
    macros = []
    for b in range(NB):
        if not last:
            macros.append(("ctx", b, 0, CTX))
        for t0 in range(0, SEQ, MT_):
            macros.append(("lat", b, t0, MT_))
    for (s, b, t0, MT) in macros:
        T = g.T[s]
        v = b if s == "lat" else NB
        r0 = b * T + t0
        G2 = load_mod_bcast(g, l, v, 4, "mo_G2")
        SH2 = load_mod_bcast(g, l, v, 3, "mo_SH2")
        gf = p.sb("mo_gf", [128, D])
        src = W["g_ffn"][l, :]
        p.dma("sp", gf[:, :], dap(src, src.offset, [[0, 128], [1, D]]))
        p.stt(G2[:, :], G2[:, :], 1.0, gf[:, :], ALU.add, ALU.mult)
        wr = p.sb("mo_wr", [128, 8, NE])
        p.dma("sp", wr[:, :, :], dap(W["w_router"], l * D * NE, [[NE, 128], [128 * NE, 8], [1, NE]]))
        brt = p.sb("mo_br", [128, NE])
        p.dma("sp", brt[:, :], dap(W["b_router"], l * NE, [[0, 128], [1, NE]]))
        lg = p.sb("mo_lg", [128, NE])
        ex = p.sb("mo_ex", [128, NE])
        mk = p.sb("mo_mk", [128, NE])
        mx = p.sb("mo_mx", [128, 8])
        sc = p.sb("mo_sc", [128, 4])

        def router(tt, h32):
            pr = g.ps()
            for kc in range(8):
                p.mm(pr[:, 0:NE], h32[:, kc, :], wr[:, kc, :], start=(kc == 0), stop=(kc == 7))
            p.tt(lg[:, :], pr[:, 0:NE], brt[:, :], ALU.add)
            p.op("dve", lambda e: e.max(out=mx[:, :], in_=lg[:, :]), [lg[:, :]], [mx[:, :]])
            p.ts(mk[:, :], lg[:, :], mx[:, 3:4], None, op0=ALU.is_ge)
            p.ts(sc[:, 0:1], mx[:, 0:1], -1.0, None, op0=ALU.mult)
            p.act(ex[:, :], lg[:, :], AF.Exp, bias=sc[:, 0:1])
            p.tt(ex[:, :], ex[:, :], mk[:, :], ALU.mult)
            p.op("dve", lambda e: e.reduce_sum(out=sc[:, 1:2], in_=ex[:, :], axis=AX.X), [ex[:, :]], [sc[:, 1:2]])
            p.recip(sc[:, 2:3], sc[:, 1:2])
            p.ts(g.Wg[:, tt, :], ex[:, :], sc[:, 2:3], None, op0=ALU.mult)
            pw = g.ps()
            p.tr(pw[0:NE, 0:128], g.Wg[:, tt, :], g.ident)
            p.copy(g.WgT[:, tt, :], pw[0:NE, 0:128])

        norm_tiles(g, g.xs[s][r0:r0 + MT, :], MT, G2, SH2, g.hT2, router)
        p.end_phase()
        import os
        MOS = int(os.environ.get("MOE_STOP", "9"))
        if MOS == 1:
            return
        nt = MT // 128
        yacc = p.sb("mo_y", [128, nt, D])
        b2all = p.sb("mo_b2all", [NE, D])
        p.dma("sp", b2all[:, :], dap(W["b_e2"], l * NE * D, [[D, NE], [1, D]]))
        for st in range(nt):
            for half in range(2):
                po = g.ps()
                p.mm(po[:, 0:512], g.WgT[:, st, :], b2all[:, half * 512:(half + 1) * 512])
                p.copy(yacc[:, st, half * 512:(half + 1) * 512], po[:, 0:512], eng="act")
        W1 = [p.sb("mo_W1_%d" % i, [128, 8, 2 * D], BF16) for i in range(2)]
        W2 = [p.sb("mo_W2_%d" % i, [128, 8, D], BF16) for i in range(2)]
        b1 = [p.sb("mo_b1_%d" % i, [128, 16]) for i in range(2)]
        actT = p.sb("mo_act", [128, 8, MT], BF16)
        gl = [p.sb("mo_gl%d" % i, [128, MT]) for i in range(2)]
        sg = [p.sb("mo_sg%d" % i, [128, MT]) for i in range(2)]
        l1 = [p.sb("mo_l1%d" % i, [128, MT]) for i in range(2)]
        tz = [p.sb("mo_tz%d" % i, [128, 512]) for i in range(2)]
        zi = 0
        for e in range(NE):
            w1, w2, b1t = W1[e % 2], W2[e % 2], b1[e % 2]
            for hh in range(2):
                sr = g.e1bf[l][e, 0, hh * D]
                p.dma("sp", w1[:, :, hh * D:(hh + 1) * D], dap(sr, sr.offset, [[2 * D, 128], [128 * 2 * D, 8], [1, D]]))
            sr = g.e2bf[l][e, 0, 0]
            p.dma("sp", w2[:, :, :], dap(sr, sr.offset, [[D, 128], [128 * D, 8], [1, D]]))
            p.dma("sp", b1t[:, :], dap(W["b_e1"], (l * NE + e) * 2 * D, [[1, 128], [128, 16]]), allow_slow_non_contiguous=True)
            for j in range(8):
                pg = g.ps()
                for kc in range(8):
                    p.mm(pg[:, 0:MT], w1[:, kc, j * 128:(j + 1) * 128], g.hT2[:, kc, 0:MT], start=(kc == 0), stop=(kc == 7))
                for kc in range(8):
                    p.mm(pg[:, 512:512 + MT], w1[:, kc, D + j * 128:D + (j + 1) * 128], g.hT2[:, kc, 0:MT], start=(kc == 0),
                         stop=(kc == 7))
                g_, s_, l_ = gl[j % 2], sg[j % 2], l1[j % 2]
                p.ts(g_[:, :], pg[:, 0:MT], b1t[:, j:j + 1], 7.0, op0=ALU.add, op1=ALU.min)
                p.act(l_[:, :], pg[:, 512:512 + MT], AF.Identity, bias=b1t[:, 8 + j:9 + j])
                p.act(s_[:, :], g_[:, :], AF.Sigmoid, scale=1.702)
                p.ts(l_[:, :], l_[:, :], -7.0, 7.0, op0=ALU.max, op1=ALU.min)
                p.tt(g_[:, :], g_[:, :], s_[:, :], ALU.mult)
                p.stt(actT[:, j, :], l_[:, :], 1.0, g_[:, :], ALU.add, ALU.mult)
            for st in range(nt):
                for half in range(2):
                    po = g.ps()
                    for kc in range(8):
                        p.mm(po[:, 0:512], actT[:, kc, st * 128:(st + 1) * 128], w2[:, kc, half * 512:(half + 1) * 512],
                             start=(kc == 0), stop=(kc == 7))
                    ya = yacc[:, st, half * 512:(half + 1) * 512]
                    p.stt(ya, po[:, 0:512], g.Wg[:, st, e:e + 1], ya, ALU.mult, ALU.add)
        GMLP = load_mod_bcast(g, l, v, 5, "mo_GMLP")
        xt = [p.sb("mo_x%d" % i, [128, D]) for i in range(2)]
        for st in range(nt):
            x_ = xt[st % 2]
            rr = r0 + st * 128
            p.dma("sp", x_[:, :], g.xs[s][rr:rr + 128, :])
            p.tt(yacc[:, st, :], yacc[:, st, :], GMLP[:, :], ALU.mult)
            p.tt(x_[:, :], x_[:, :], yacc[:, st, :], ALU.add)
            p.dma("sp", g.xs[s][rr:rr + 128, :], x_[:, :])
        p.end_phase()
        if MOS in (2, 3):
            return


def phase_final(g):
    p, NB, W = g.p, g.NB, g.W
    gfin = p.sb("fn_g", [128, D])
    p.dma("sp", gfin[:, :], dap(W["g_final"], 0, [[0, 128], [1, D]]))
    xts = [p.sb("fn_x%d" % i, [128, D]) for i in range(2)]
    sq = p.sb("fn_sq", [128, D])
    ss = p.sb("fn_ss", [128, 4])
    for tt in range(NB * SEQ // 128):
        xt = xts[tt % 2]
        p.dma("sp", xt[:, :], g.xs["lat"][tt * 128:(tt + 1) * 128, :])
        p.act(sq[:, :], xt[:, :], AF.Square)
        p.op("dve", lambda e, o=ss[:, 0:1], i=sq[:, :]: e.reduce_sum(out=o, in_=i, axis=AX.X), [sq[:, :]], [ss[:, 0:1]])
        p.ts(ss[:, 1:2], ss[:, 0:1], 1.0 / D, EPS, op0=ALU.mult, op1=ALU.add)
        p.act(ss[:, 2:3], ss[:, 1:2], AF.Sqrt)
        p.recip(ss[:, 3:4], ss[:, 2:3])
        p.stt(xt[:, :], xt[:, :], ss[:, 3:4], gfin[:, :], ALU.mult, ALU.mult)
        p.dma("sp", g.out[tt * 128:(tt + 1) * 128, :], xt[:, :])
    p.end_phase()


_NC_CACHE = {}


def kernel(**inputs):
    NCORES = 8
    NB = 32 // NCORES
    if "nc" not in _NC_CACHE:
        _NC_CACHE["nc"] = build_program(NB=NB)
    nc = _NC_CACHE["nc"]
    consts = make_consts()
    f32 = lambda a: np.ascontiguousarray(np.asarray(a, dtype=np.float32))
    wts = {n: f32(inputs[n]).reshape(WSHAPES[n]) for n in WNAMES}
    x = f32(inputs["x"])
    ctx = f32(inputs["ctx"])
    c = f32(inputs["c"])
    cctx = f32(inputs["c_ctx"]).reshape(1, D)
    in_maps = []
    for i in range(NCORES):
        m = {"x": x[i * NB:(i + 1) * NB].reshape(NB * SEQ, D), "ctx": ctx[i * NB:(i + 1) * NB].reshape(NB * CTX, D),
             "c": c[i * NB:(i + 1) * NB], "c_ctx": cctx, "consts": consts}
        m.update(wts)
        in_maps.append(m)
    res = run_bass_kernel_spmd(nc, in_maps, core_ids=list(range(NCORES)))
    out = np.concatenate([r["out"].reshape(NB, SEQ, D) for r in res.results], axis=0)
    return out.astype(np.float32)
```

```python
import contextlib
import math
import numpy as np
import concourse.bass as bass
import concourse.mybir as mybir
from concourse.bass_utils import run_bass_kernel_spmd

F32 = mybir.dt.float32
BF16 = mybir.dt.bfloat16
ALU = mybir.AluOpType
AF = mybir.ActivationFunctionType
AX = mybir.AxisListType

D = 1024
SEQ = 2048
CTX = 256
DEPTH = 2
D_IN = 8736
NE = 32
EPS = 1e-6
OFF_AX, OFF_AY, OFF_Q, OFF_K, OFF_V, OFF_Z, OFF_BETA, OFF_ALPHA, OFF_U, OFF_GATE = (
    0, 512, 1024, 2048, 3072, 4096, 5120, 5136, 5152, 5664)

SAME_ENGINE_SYNC = True
RELAX_SAME = True
NDSEM = 12


class Buf:
    __slots__ = ("w", "r")

    def __init__(self):
        self.w = {}
        self.r = {}


class Prog:
    ENG = ("pe", "act", "dve", "pool", "sp")

    def __init__(self, nc):
        self.nc = nc
        self.ops = {e: [] for e in self.ENG}
        self.cnt = {e: 0 for e in self.ENG}
        self.known = {e: {} for e in self.ENG}
        self.dq = {e: 0 for e in self.ENG}
        self.dval = {}
        self.bufs = {}
        self.st = contextlib.ExitStack()
        self.uid = 0
        self.raw_same = {}

    def sb(self, name, shape, dtype=F32):
        self.uid += 1
        t = self.st.enter_context(self.nc.sbuf_tensor("%s_%d" % (name, self.uid), list(shape), dtype))
        return t

    def psum(self, name, shape, dtype=F32):
        return self.st.enter_context(self.nc.psum_tensor(name, list(shape), dtype))

    def dram(self, name, shape, dtype=F32, kind="Internal"):
        return self.nc.dram_tensor(name, list(shape), dtype, kind=kind)

    def _buf(self, ap):
        n = ap.tensor.name
        b = self.bufs.get(n)
        if b is None:
            b = self.bufs[n] = Buf()
        return b

    def _sched(self, e, reads, writes, tok_fn):
        d = {}
        rb = [self._buf(a) for a in reads if a is not None and not isinstance(a, (int, float))]
        wb = [self._buf(a) for a in writes]
        rawv = 0
        for b in rb:
            rawv = max(rawv, b.w.get(e, 0))
        self.raw_same = {e: (self.cnt[e] if rawv < self.cnt[e] - 3 else rawv - 1) if rawv else self.cnt[e]}
        if rawv and rawv >= self.cnt[e] - 3:
            self.raw_same = {e: rawv - 1}
        else:
            self.raw_same = {e: self.cnt[e]}
        for a, b in zip([a for a in reads if a is not None and not isinstance(a, (int, float))], rb):
            for k, v in b.w.items():
                if d.get(k, 0) < v:
                    d[k] = v
            if a.tensor.name.startswith("ps"):
                for k, v in b.r.items():
                    if k != e and d.get(k, 0) < v:
                        d[k] = v
        for b in wb:
            for k, v in b.w.items():
                if d.get(k, 0) < v:
                    d[k] = v
            for k, v in b.r.items():
                if d.get(k, 0) < v:
                    d[k] = v
        return d, rb, wb

    def _waits(self, e, d):
        kn = self.known[e]
        for k, v in d.items():
            if k == e and ((not SAME_ENGINE_SYNC) or e in ("pe", "sp")):
                continue
            if k == e and RELAX_SAME and v <= self.raw_same.get(e, 0):
                continue
            if kn.get(k, 0) >= v:
                continue
            kn[k] = v
            self.ops[e].append(("wait", k, v))

    def op(self, e, fn, reads, writes):
        d, rb, wb = self._sched(e, reads, writes, None)
        self._waits(e, d)
        self.cnt[e] += 1
        k, v = e, self.cnt[e]
        self.ops[e].append(("op", fn))
        for b in rb:
            if b.r.get(k, 0) < v:
                b.r[k] = v
        for b in wb:
            b.w[k] = v
            b.r = {}

    def dma(self, q, out, in_, **kw):
        d, rb, wb = self._sched(q, [in_], [out], None)
        j = self.dq[q]
        self.dq[q] = (j + 1) % (4 if q == "pool" else NDSEM)
        key = ("d", q, j)
        prev = self.dval.get(key, 0)
        if prev:
            d[key] = max(d.get(key, 0), prev)
        self._waits(q, d)
        val = prev + 16
        self.dval[key] = val
        self.ops[q].append(("dma", out, in_, key, kw))
        for b in rb:
            if b.r.get(key, 0) < val:
                b.r[key] = val
        for b in wb:
            b.w[key] = val
            b.r = {}

    def mm(self, out, lhsT, rhs, start=True, stop=True):
        self.op("pe", lambda e: e.matmul(out, lhsT=lhsT, rhs=rhs, start=start, stop=stop), [lhsT, rhs], [out])

    def tr(self, out, in_, ident):
        self.op("pe", lambda e: e.transpose(out, in_, ident), [in_, ident], [out])

    def act(self, out, in_, func, bias=None, scale=None, accum_out=None, eng="act"):
        kw = {}
        rd = [in_]
        wr = [out]
        if bias is not None:
            kw["bias"] = bias
            rd.append(bias)
        if scale is not None:
            kw["scale"] = scale
            rd.append(scale)
        if accum_out is not None:
            kw["accum_out"] = accum_out
            wr.append(accum_out)
        self.op("act", lambda e: e.activation(out=out, in_=in_, func=func, **kw), rd, wr)

    def tt(self, out, in0, in1, op, eng="dve"):
        self.op(eng, lambda e: e.tensor_tensor(out=out, in0=in0, in1=in1, op=op), [in0, in1], [out])

    def ts(self, out, in0, s1, s2=None, op0=ALU.mult, op1=None, eng="dve"):
        kw = {}
        if op1 is not None:
            kw["op1"] = op1
        self.op(eng, lambda e: e.tensor_scalar(out=out, in0=in0, scalar1=s1, scalar2=s2, op0=op0, **kw),
                [in0, s1, s2], [out])

    def stt(self, out, in0, scalar, in1, op0, op1, eng="dve"):
        self.op(eng, lambda e: e.scalar_tensor_tensor(out=out, in0=in0, scalar=scalar, in1=in1, op0=op0, op1=op1),
                [in0, scalar, in1], [out])

    def copy(self, out, in_, eng="dve"):
        if eng == "act":
            self.act(out, in_, AF.Copy)
        else:
            self.op(eng, lambda e: e.tensor_copy(out=out, in_=in_), [in_], [out])

    def memset(self, out, val, eng="dve"):
        self.op(eng, lambda e: e.memset(out, val), [], [out])

    def recip(self, out, in_):
        self.op("dve", lambda e: e.reciprocal(out=out, in_=in_), [in_], [out])

    def scan(self, out, d0, d1, init, eng="dve"):
        self.op(eng, lambda e: e.tensor_tensor_scan(out=out, data0=d0, data1=d1, initial=init, op0=ALU.mult,
                                                    op1=ALU.add), [d0, d1, init], [out])

    def barrier(self):
        for e in self.ENG:
            d = {}
            for e2 in self.ENG:
                if e2 != e and self.cnt[e2]:
                    d[e2] = self.cnt[e2]
            for key, val in self.dval.items():
                d[key] = val
            self._waits(e, d)

    def setup(self):
        nc = self.nc
        self.gst = contextlib.ExitStack()
        self.sems = {}
        for e in self.ENG:
            self.sems[e] = self.gst.enter_context(nc.semaphore("s_" + e))
        for q in ("sp", "pool", "act"):
            for j in range(NDSEM):
                self.sems[("d", q, j)] = self.gst.enter_context(nc.semaphore("d_%s_%d" % (q, j)))
        self.st = contextlib.ExitStack()

    def gsb(self, name, shape, dtype=F32):
        return self.gst.enter_context(self.nc.sbuf_tensor(name, list(shape), dtype))

    def gpsum(self, name, shape, dtype=F32):
        return self.gst.enter_context(self.nc.psum_tensor(name, list(shape), dtype))

    def end_phase(self, final=False):
        nc = self.nc
        self.barrier()
        sems = self.sems
        ops = self.ops

        def run(e, eng):
            se = sems[e]
            for o in ops[e]:
                if o[0] == "wait":
                    eng.wait_ge(sems[o[1]], o[2])
                elif o[0] == "op":
                    o[1](eng).then_inc(se, 1)
                else:
                    _, out, in_, key, kw = o
                    eng.dma_start(out=out, in_=in_, **kw).then_inc(sems[key], 16)

        with nc.Block() as block:
            block.sync(lambda eng: run("sp", eng))
            block.tensor(lambda eng: run("pe", eng))
            block.scalar(lambda eng: run("act", eng))
            block.vector(lambda eng: run("dve", eng))
            block.gpsimd(lambda eng: run("pool", eng))
        self.ops = {e: [] for e in self.ENG}
        self.st.close()
        self.st = contextlib.ExitStack()
        if final:
            self.gst.close()


def rev(ap):
    a = [list(x) for x in ap.ap]
    step, n = a[-1]
    a[-1] = [-step, n]
    return bass.AP(ap.tensor, ap.offset + step * (n - 1), a)


def bcast_rows(ap, nparts):
    a = [list(x) for x in ap.ap]
    return bass.AP(ap.tensor, ap.offset, [[0, nparts]] + a[-1:])


WNAMES = ["w_ada", "b_ada", "g_mix", "g_ffn", "w_in", "lru_conv_w", "lru_conv_b", "lru_w_gate", "lru_b_gate",
          "lru_lam", "dn_conv_w", "dn_a_log", "dn_dt_bias", "dn_norm_g", "s5_lam_re", "s5_lam_im", "s5_log_dt",
          "s5_b_re", "s5_b_im", "s5_c_re", "s5_c_im", "s5_d", "s5_w_glu", "s5_b_glu", "w_br_a", "w_br_b", "w_br_c",
          "w_out", "w_router", "b_router", "w_e1", "b_e1", "w_e2", "b_e2", "g_final"]


WSHAPES = {
    "w_ada": [2, 1024, 6144], "b_ada": [2, 6144], "g_mix": [2, 1024], "g_ffn": [2, 1024], "w_in": [2, 1024, 8736],
    "lru_conv_w": [2, 4, 512], "lru_conv_b": [2, 512], "lru_w_gate": [2, 2, 2, 8, 64, 64], "lru_b_gate": [2, 2, 2, 512],
    "lru_lam": [2, 2, 512], "dn_conv_w": [2, 4, 3072], "dn_a_log": [2, 2, 8], "dn_dt_bias": [2, 2, 8],
    "dn_norm_g": [2, 128], "s5_lam_re": [2, 2, 32, 64], "s5_lam_im": [2, 2, 32, 64], "s5_log_dt": [2, 2, 32],
    "s5_b_re": [2, 2, 32, 64, 16], "s5_b_im": [2, 2, 32, 64, 16], "s5_c_re": [2, 2, 32, 16, 64],
    "s5_c_im": [2, 2, 32, 16, 64], "s5_d": [2, 512], "s5_w_glu": [2, 512, 512], "s5_b_glu": [2, 512],
    "w_br_a": [2, 512, 1024], "w_br_b": [2, 1024, 1024], "w_br_c": [2, 512, 1024], "w_out": [2, 1024, 1024],
    "w_router": [2, 1024, 32], "b_router": [2, 32], "w_e1": [2, 32, 1024, 2048], "b_e1": [2, 32, 2048],
    "w_e2": [2, 32, 1024, 1024], "b_e2": [2, 32, 1024], "g_final": [1, 1024],
}
BIG = 30000.0
C_ID, C_ONE, C_LTF, C_LTB, C_MGT, C_MLT, C_SGT, C_SLT, C_IOTA, C_END = 0, 128, 256, 320, 384, 448, 512, 576, 640, 640 + 2048


def make_consts():
    c = np.zeros((128, C_END), np.float32)
    c[:, C_ID:C_ID + 128] = np.eye(128, dtype=np.float32)
    c[:, C_ONE:C_ONE + 128] = 1.0
    p = np.arange(128)[:, None]
    f = np.arange(64)[None, :]
    c[:, C_LTF:C_LTF + 64] = (p <= f) & (p < 64)
    c[:, C_LTB:C_LTB + 64] = (p >= f) & (p < 64)
    c[:, C_MGT:C_MGT + 64] = BIG * (f > p)
    c[:, C_MLT:C_MLT + 64] = BIG * (f < p)
    c[:, C_SGT:C_SGT + 64] = (f > p)
    c[:, C_SLT:C_SLT + 64] = (f < p)
    c[:, C_IOTA:C_IOTA + 2048] = np.arange(2048, dtype=np.float32)[None, :]
    return c


def dap(t, offset, dims):
    return bass.AP(t.tensor if hasattr(t, "tensor") else t, offset, [list(d) for d in dims])


class Ctx:
    pass


def build_program(NB=4, layers=(0, 1), stop_after=None, dbg=(), skip=(), inject=()):
    nc = bass.Bass("TRN2", target_bir_lowering=False)
    p = Prog(nc)
    p.setup()
    NV = NB + 1
    g = Ctx()
    g.NB, g.NV, g.p, g.nc = NB, NV, p, nc
    g.skip = set(skip)
    x_in = nc.dram_tensor("x", [NB * SEQ, D], F32, kind="ExternalInput").ap()
    ctx_in = nc.dram_tensor("ctx", [NB * CTX, D], F32, kind="ExternalInput").ap()
    c_in = nc.dram_tensor("c", [NB, D], F32, kind="ExternalInput").ap()
    cctx_in = nc.dram_tensor("c_ctx", [1, D], F32, kind="ExternalInput").ap()
    consts_in = nc.dram_tensor("consts", [128, C_END], F32, kind="ExternalInput").ap()
    W = {}
    for name in WNAMES:
        W[name] = nc.dram_tensor(name, WSHAPES[name], F32, kind="ExternalInput").ap()
    out = nc.dram_tensor("out", [NB * SEQ, D], F32, kind="ExternalOutput").ap()
    g.W, g.out = W, out

    g.xs = {"lat": p.dram("xs_lat", [NB * SEQ, D]).ap(), "ctx": p.dram("xs_ctx", [NB * CTX, D]).ap()}
    g.T = {"lat": SEQ, "ctx": CTX}
    g.mod = p.dram("mod", [DEPTH, NV, 6 * D]).ap()
    g.proj = {"lat": [p.dram("proj_lat%d" % b, [D_IN, SEQ]).ap() for b in range(NB)],
              "ctx": [p.dram("proj_ctx%d" % b, [D_IN, CTX]).ap() for b in range(NB)]}
    g.ya = {s: p.dram("ya_" + s, [NB, 512, g.T[s]], BF16).ap() for s in ("lat", "ctx")}
    g.yb = {s: p.dram("yb_" + s, [NB, 1024, g.T[s]], BF16).ap() for s in ("lat", "ctx")}
    g.yc = {s: p.dram("yc_" + s, [NB, 512, g.T[s]], BF16).ap() for s in ("lat", "ctx")}
    g.ys5 = {s: p.dram("ys5_" + s, [NB, 512, g.T[s]]).ap() for s in ("lat", "ctx")}
    g.odn = {s: p.dram("odn_" + s, [2, NB, 1024, g.T[s]]).ap() for s in ("lat", "ctx")}
    g.e1bf = [p.dram("e1bf%d" % l_, [NE, D, 2 * D], BF16).ap() for l_ in range(DEPTH)]
    g.e2bf = [p.dram("e2bf%d" % l_, [NE, D, D], BF16).ap() for l_ in range(DEPTH)]

    g.cst = p.gsb("cst", [128, C_END])
    g.cstb = p.gsb("cstb", [128, 256], BF16)
    g.PS = [p.gpsum("ps%d" % i, [128, 1024]) for i in range(4)]
    g.psi = 0
    g.stl = p.gsb("st_lru", [128, NB, 4, 2])
    g.sts5 = p.gsb("st_s5", [128, NB, 2, 16, 2])
    g.hT2 = p.gsb("hT2", [128, 8, MT_], BF16)
    g.Wg = p.gsb("Wg", [128, MT_ // 128, NE])
    g.WgT = p.gsb("WgT", [NE, MT_ // 128, 128])

    g.dumps = set(x for x in dbg if x.startswith("@"))

    def dump(name, ap, dtype=F32):
        if "@" + name not in g.dumps:
            return
        shp = list(ap.shape)
        dst = nc.dram_tensor("dbg_" + name, [shp[0], int(np.prod(shp[1:]))], dtype, kind="ExternalOutput").ap()
        if len(shp) == 3:
            dst = dst.rearrange("p (a b) -> p a b", b=shp[2])
        p.dma("sp", dst, ap)
    g.dump = dump

    def ps():
        t = g.PS[g.psi % 4]
        g.psi += 1
        return t
    g.ps = ps
    g.epsc = p.gsb("epsc", [128, 1])
    p.memset(g.epsc[:, :], EPS)
    g.negpi = p.gsb("negpi", [128, 1])
    p.memset(g.negpi[:, :], -math.pi)
    g.ident = g.cst[:, C_ID:C_ID + 128]
    g.ones = g.cst[:, C_ONE:C_ONE + 128]

    p.dma("sp", g.cst[:, :], consts_in[:, :])
    p.copy(g.cstb[:, :], g.cst[:, 0:256])
    for b in range(NB):
        p.dma("sp", g.xs["lat"][b * SEQ:(b + 1) * SEQ, :], x_in[b * SEQ:(b + 1) * SEQ, :])
    p.dma("sp", g.xs["ctx"][:, :], ctx_in[:, :])
    if "moe" not in (stop_after or ()):
        pass
    g.cast_moe = lambda: None
    p.end_phase()

    def cast_moe(l):
        for e in range(NE):
            for cb in range(4):
                p.dma("pool", g.e1bf[l][e, :, cb * 512:(cb + 1) * 512], W["w_e1"][l, e, :, cb * 512:(cb + 1) * 512])
            for cb in range(2):
                p.dma("pool", g.e2bf[l][e, :, cb * 512:(cb + 1) * 512], W["w_e2"][l, e, :, cb * 512:(cb + 1) * 512])
    g.cast_moe = cast_moe

    for l in layers:
        last = (l == DEPTH - 1)
        phase_adaln(g, l, c_in, cctx_in)
        if stop_after == "adaln":
            break
        for b in range(NB):
            for s in ("ctx", "lat"):
                phase_norm_proj(g, l, b, s)
        if stop_after == "proj":
            break
        if "lru" not in g.skip:
            phase_lru(g, l, last)
        if stop_after == "lru":
            break
        if "s5" not in g.skip:
            phase_s5(g, l, last)
        if stop_after == "s5":
            break
        if "dn" not in g.skip:
            phase_dn(g, l, last)
        if stop_after == "dn":
            break
        phase_merge(g, l, last)
        if stop_after == "merge":
            break
        if "moe" not in g.skip:
            import os
            if not os.environ.get("MOE_NOCAST"):
                g.cast_moe(l)
            phase_moe(g, l, last)
        if stop_after == "moe":
            break
    if stop_after is None:
        phase_final(g)
    srcs = {"mod": g.mod, "proj_lat": g.proj["lat"][0], "proj_ctx": g.proj["ctx"][0], "xs_lat": g.xs["lat"],
            "xs_ctx": g.xs["ctx"]}
    for s_ in ("lat", "ctx"):
        for nm, dd in (("ya", g.ya), ("yb", g.yb), ("yc", g.yc), ("ys5", g.ys5), ("odn", g.odn)):
            srcs[nm + "_" + s_] = dd[s_]
    for name in dbg:
        if name.startswith("@"):
            continue
        src = srcs[name]
        n0 = src.shape[0]
        rest = int(np.prod(src.shape[1:]))
        dst = nc.dram_tensor("dbg_" + name, [n0, rest], src.dtype, kind="ExternalOutput").ap()
        for i in range(n0):
            p.dma("sp", dst[i:i + 1, :], dap(src, src.offset + i * rest, [[rest, 1], [1, rest]]))
    if stop_after is None:
        pass
    p.end_phase(final=True)
    return nc


def load_mod_bcast(g, l, v, idx, name):
    p = g.p
    t = p.sb(name, [128, D])
    src = g.mod[l, v, idx * D:(idx + 1) * D]
    p.dma("sp", t[:, :], dap(src, src.offset, [[0, 128], [1, D]]))
    return t


def phase_adaln(g, l, c_in, cctx_in):
    p, NB, NV, W = g.p, g.NB, g.NV, g.W
    sT = p.sb("ad_sT", [128, 8, NV])
    sTb = p.sb("ad_sTb", [128, 8, NV], BF16)
    for v in range(NB):
        p.dma("sp", sT[:, :, v], dap(c_in, v * D, [[1, 128], [128, 8]]), allow_slow_non_contiguous=True)
    p.dma("sp", sT[:, :, NB], dap(cctx_in, 0, [[1, 128], [128, 8]]), allow_slow_non_contiguous=True)
    p.act(sTb[:, :, :], sT[:, :, :], AF.Silu)
    bias = p.sb("ad_bias", [NV, 6 * D])
    src = W["b_ada"][l, :]
    p.dma("sp", bias[:, :], dap(src, src.offset, [[0, NV], [1, 6 * D]]))
    modsb = p.sb("ad_mod", [NV, 6 * D])
    wts = [p.sb("ad_w%d" % i, [128, 8, 512], BF16) for i in range(2)]
    for ct in range(12):
        wt = wts[ct % 2]
        src = W["w_ada"][l, :, ct * 512:(ct + 1) * 512]
        p.dma("pool", wt[:, :, :], dap(src, src.offset, [[6 * D, 128], [128 * 6 * D, 8], [1, 512]]))
        pt = g.ps()
        for kc in range(8):
            p.mm(pt[0:NV, 0:512], sTb[:, kc, :], wt[:, kc, :], start=(kc == 0), stop=(kc == 7))
        p.tt(modsb[:, ct * 512:(ct + 1) * 512], pt[0:NV, 0:512], bias[:, ct * 512:(ct + 1) * 512], ALU.add)
    p.dma("sp", g.mod[l, :, :], modsb[:, :])
    g.dump("sT", sT[:, :, :])
    g.dump("bias", bias[:, :])
    g.dump("modsb", modsb[:, :])
    p.end_phase()


def phase_norm_proj(g, l, b, s):
    p, NB, W = g.p, g.NB, g.W
    T = g.T[s]
    v = b if s == "lat" else NB
    xs = g.xs[s][b * T:(b + 1) * T, :]
    hT = p.sb("np_hT", [128, 8, T], BF16)
    G1 = load_mod_bcast(g, l, v, 1, "np_G1")
    SH = load_mod_bcast(g, l, v, 0, "np_SH")
    gm = p.sb("np_gm", [128, D])
    src = W["g_mix"][l, :]
    p.dma("sp", gm[:, :], dap(src, src.offset, [[0, 128], [1, D]]))
    p.stt(G1[:, :], G1[:, :], 1.0, gm[:, :], ALU.add, ALU.mult)
    norm_tiles(g, xs, T, G1, SH, hT, None)
    wts = [p.sb("np_w%d" % i, [128, 8, 512], BF16) for i in range(2)]
    stg = [p.sb("np_stg%d" % i, [128, 512]) for i in range(4)]
    si = 0
    ngrp = (D_IN + 511) // 512
    TT = min(T, 512)
    for og in range(ngrp):
        c0 = og * 512
        ncol = min(512, D_IN - c0)
        wt = wts[og % 2]
        src = W["w_in"][l, :, c0:c0 + ncol]
        p.dma("pool", wt[:, :, 0:ncol], dap(src, src.offset, [[D_IN, 128], [128 * D_IN, 8], [1, ncol]]))
        for oc in range((ncol + 127) // 128):
            m = min(128, ncol - oc * 128)
            for tt in range(T // TT):
                pt = g.ps()
                for kc in range(8):
                    p.mm(pt[0:m, 0:TT], wt[:, kc, oc * 128:oc * 128 + m], hT[:, kc, tt * TT:(tt + 1) * TT],
                         start=(kc == 0), stop=(kc == 7))
                sg = stg[si % 4]
                p.copy(sg[0:m, 0:TT], pt[0:m, 0:TT], eng=("act" if si % 2 else "dve"))
                si += 1
                r0 = c0 + oc * 128
                p.dma("sp", g.proj[s][b][r0:r0 + m, tt * TT:(tt + 1) * TT], sg[0:m, 0:TT])
    p.end_phase()


def norm_tiles(g, xs, T, G1, SH, hT, h32cb):
    p = g.p
    xts = [p.sb("nt_x%d" % i, [128, D]) for i in range(2)]
    sq = p.sb("nt_sq", [128, D])
    hN = [p.sb("nt_h%d" % i, [128, D]) for i in range(2)]
    ss = p.sb("nt_ss", [128, 4])
    h32 = [p.sb("nt_h32_%d" % i, [128, 8, 128]) for i in range(2)] if h32cb else None
    for tt in range(T // 128):
        xt = xts[tt % 2]
        hn = hN[tt % 2]
        p.dma("sp", xt[:, :], xs[tt * 128:(tt + 1) * 128, :])
        p.act(sq[:, :], xt[:, :], AF.Square)
        p.op("dve", lambda e, o=ss[:, 0:1], i=sq[:, :]: e.reduce_sum(out=o, in_=i, axis=AX.X), [sq[:, :]], [ss[:, 0:1]])
        p.ts(ss[:, 1:2], ss[:, 0:1], 1.0 / D, EPS, op0=ALU.mult, op1=ALU.add)
        p.act(ss[:, 2:3], ss[:, 1:2], AF.Sqrt)
        p.recip(ss[:, 3:4], ss[:, 2:3])
        p.stt(hn[:, :], xt[:, :], ss[:, 3:4], G1[:, :], ALU.mult, ALU.mult)
        p.tt(hn[:, :], hn[:, :], SH[:, :], ALU.add)
        for half in range(2):
            pt = g.ps()
            for k4 in range(4):
                kc = half * 4 + k4
                p.tr(pt[:, k4 * 128:(k4 + 1) * 128], hn[:, kc * 128:(kc + 1) * 128], g.ident)
            src = pt[:, 0:512].rearrange("p (a b) -> p a b", b=128)
            p.act(hT[:, half * 4:half * 4 + 4, tt * 128:(tt + 1) * 128], src, AF.Copy)
            if h32cb:
                p.copy(h32[tt % 2][:, half * 4:half * 4 + 4, :], src)
        if h32cb:
            h32cb(tt, h32[tt % 2])


def phase_lru(g, l, last):
    p, NB, W = g.p, g.NB, g.W
    Wbd = p.sb("lr_Wbd", [128, 2, 2, 4, 128])
    p.memset(Wbd[:, :, :, :, :], 0.0)
    for d in range(2):
        for gg in range(2):
            for hh in range(2):
                off = W["lru_w_gate"][l, d, gg, hh, 0, 0].offset if False else (((l * 2 + d) * 2 + gg) * 8 + hh) * 4096
                p.dma("sp", Wbd[hh * 64:(hh + 1) * 64, d, gg, :, hh * 64:(hh + 1) * 64],
                      dap(W["lru_w_gate"], off, [[64, 64], [2 * 4096, 4], [1, 64]]))
    lam = p.sb("lr_lam", [128, 2, 4])
    cv = p.sb("lr_cv", [128, 2, 4])
    bg = p.sb("lr_bg", [128, 2, 2, 4])
    cw = p.sb("lr_cw", [128, 4, 4])
    cb = p.sb("lr_cb", [128, 4])
    p.dma("sp", lam[:, :, :], dap(W["lru_lam"], l * 1024, [[1, 128], [512, 2], [128, 4]]), allow_slow_non_contiguous=True)
    for d in range(2):
        p.dma("sp", bg[:, d, :, :], dap(W["lru_b_gate"], (l * 2 + d) * 1024, [[1, 128], [512, 2], [128, 4]]),
              allow_slow_non_contiguous=True)
    p.dma("sp", cw[:, :, :], dap(W["lru_conv_w"], l * 2048, [[1, 128], [512, 4], [128, 4]]), allow_slow_non_contiguous=True)
    p.dma("sp", cb[:, :], dap(W["lru_conv_b"], l * 512, [[1, 128], [128, 4]]), allow_slow_non_contiguous=True)
    p.act(cv[:, :, :], lam[:, :, :], AF.Exp, scale=-1.0)
    p.act(cv[:, :, :], cv[:, :, :], AF.Ln, bias=1.0)
    p.ts(cv[:, :, :], cv[:, :, :], -8.0, None, op0=ALU.mult)
    TM = SEQ
    ax = p.sb("lr_ax", [128, TM])
    xc = p.sb("lr_xc", [128, TM])
    ay = p.sb("lr_ay", [128, TM])
    rt = p.sb("lr_r", [128, TM])
    it = p.sb("lr_i", [128, TM])
    at = p.sb("lr_a", [128, TM])
    a2 = p.sb("lr_a2", [128, TM])
    bt = p.sb("lr_b", [128, TM])
    hd = [p.sb("lr_h%d" % d, [128, TM]) for d in range(2)]
    yo = p.sb("lr_y", [128, TM], BF16)
    for b in range(NB):
        for s in ("ctx", "lat"):
            T = g.T[s]
            L = 64 if s == "lat" else T
            TT = min(T, 512)
            emit = not (last and s == "ctx")
            for ch in range(4):
                p.dma("sp", ax[:, 0:T], g.proj[s][b][OFF_AX + ch * 128:OFF_AX + (ch + 1) * 128, :])
                if emit:
                    p.dma("sp", ay[:, 0:T], g.proj[s][b][OFF_AY + ch * 128:OFF_AY + (ch + 1) * 128, :])
                a3 = ax[:, 0:T].rearrange("p (r l) -> p r l", l=L)
                x3 = xc[:, 0:T].rearrange("p (r l) -> p r l", l=L)
                p.ts(xc[:, 0:T], ax[:, 0:T], cw[:, 2, ch:ch + 1], cb[:, ch:ch + 1], op0=ALU.mult, op1=ALU.add)
                p.stt(x3[:, :, 2:L], a3[:, :, 0:L - 2], cw[:, 0, ch:ch + 1], x3[:, :, 2:L], ALU.mult, ALU.add)
                p.stt(x3[:, :, 1:L], a3[:, :, 0:L - 1], cw[:, 1, ch:ch + 1], x3[:, :, 1:L], ALU.mult, ALU.add)
                p.stt(x3[:, :, 0:L - 1], a3[:, :, 1:L], cw[:, 3, ch:ch + 1], x3[:, :, 0:L - 1], ALU.mult, ALU.add)
                for d in range(2):
                    for tt in range(T // TT):
                        sl = slice(tt * TT, (tt + 1) * TT)
                        pr = g.ps()
                        p.mm(pr[:, 0:TT], Wbd[:, d, 0, ch, :], xc[:, sl])
                        p.mm(pr[:, 512:512 + TT], Wbd[:, d, 1, ch, :], xc[:, sl])
                        p.act(rt[:, sl], pr[:, 0:TT], AF.Sigmoid, bias=bg[:, d, 0, ch:ch + 1])
                        p.act(it[:, sl], pr[:, 512:512 + TT], AF.Sigmoid, bias=bg[:, d, 1, ch:ch + 1])
                    p.act(at[:, 0:T], rt[:, 0:T], AF.Exp, scale=cv[:, d, ch:ch + 1])
                    p.tt(a2[:, 0:T], at[:, 0:T], at[:, 0:T], ALU.mult, eng="pool")
                    p.act(a2[:, 0:T], a2[:, 0:T], AF.Sqrt, scale=-1.0, bias=1.0)
                    p.tt(bt[:, 0:T], it[:, 0:T], xc[:, 0:T], ALU.mult)
                    p.tt(bt[:, 0:T], bt[:, 0:T], a2[:, 0:T], ALU.mult)
                    init = 0.0 if s == "ctx" else g.stl[:, b, ch, d:d + 1]
                    h = hd[d]
                    if d == 0:
                        p.scan(h[:, 0:T], at[:, 0:T], bt[:, 0:T], init)
                    else:
                        p.scan(rev(h[:, 0:T]), rev(at[:, 0:T]), rev(bt[:, 0:T]), init)
                    if s == "ctx":
                        col = T - 1 if d == 0 else 0
                        p.copy(g.stl[:, b, ch, d:d + 1], h[:, col:col + 1])
                if emit:
                    p.act(ay[:, 0:T], ay[:, 0:T], AF.Gelu_apprx_tanh)
                    p.tt(hd[0][:, 0:T], hd[0][:, 0:T], hd[1][:, 0:T], ALU.add)
                    p.tt(yo[:, 0:T], hd[0][:, 0:T], ay[:, 0:T], ALU.mult)
                    p.dma("sp", g.ya[s][b, ch * 128:(ch + 1) * 128, :], yo[:, 0:T])
    p.end_phase()


def ins(ap, axis, n):
    a = [list(x) for x in ap.ap]
    a.insert(axis, [0, n])
    return bass.AP(ap.tensor, ap.offset, a)


PI = math.pi


def phase_s5(g, l, last):
    p, NB, W = g.p, g.NB, g.W
    TA = CTX + SEQ
    seqs = (("ctx", 0, CTX), ("lat", CTX, SEQ))
    iota = g.cst[:, C_IOTA:C_IOTA + SEQ]
    M = p.sb("s5_M", [128, 4, 8])
    p.memset(M[:, :, :], 0.0)
    for rr in range(4):
        p.memset(M[0:64, rr, 2 * rr:2 * rr + 1], 1.0)
        p.memset(M[64:128, rr, 2 * rr + 1:2 * rr + 2], 1.0)
    prm = []
    for d in range(2):
        t = {}
        for nm in ("lre", "lim", "dt", "mag", "th", "cth", "sth", "ar", "ai", "fr", "fi", "t0", "t1", "t2"):
            t[nm] = p.sb("s5_%s%d" % (nm, d), [128, 16])
        base = (l * 2 + d) * 32 * 64
        for gg in range(2):
            p.dma("sp", t["lre"][gg * 64:(gg + 1) * 64, :], dap(W["s5_lam_re"], base + gg * 64, [[1, 64], [128, 16]]),
                  allow_slow_non_contiguous=True)
            p.dma("sp", t["lim"][gg * 64:(gg + 1) * 64, :], dap(W["s5_lam_im"], base + gg * 64, [[1, 64], [128, 16]]),
                  allow_slow_non_contiguous=True)
            p.dma("sp", t["dt"][gg * 64:(gg + 1) * 64, :], dap(W["s5_log_dt"], (l * 2 + d) * 32 + gg, [[0, 64], [2, 16]]),
                  allow_slow_non_contiguous=True)
        p.act(t["dt"][:, :], t["dt"][:, :], AF.Exp)
        p.tt(t["t0"][:, :], t["lre"][:, :], t["dt"][:, :], ALU.mult)
        p.act(t["mag"][:, :], t["t0"][:, :], AF.Exp)
        p.tt(t["th"][:, :], t["lim"][:, :], t["dt"][:, :], ALU.mult)
        ki = p.sb("s5_ki%d" % d, [128, 16], mybir.dt.int32)
        p.ts(t["t0"][:, :], t["th"][:, :], 1.0 / (2 * PI), None, op0=ALU.mult)
        p.copy(ki[:, :], t["t0"][:, :])
        p.copy(t["t1"][:, :], ki[:, :])
        p.stt(t["t0"][:, :], t["t1"][:, :], -2 * PI, t["th"][:, :], ALU.mult, ALU.add)
        p.ts(t["t1"][:, :], t["t0"][:, :], PI, None, op0=ALU.is_gt)
        p.stt(t["t0"][:, :], t["t1"][:, :], -2 * PI, t["t0"][:, :], ALU.mult, ALU.add)
        p.ts(t["t1"][:, :], t["t0"][:, :], -PI, None, op0=ALU.is_lt)
        p.stt(t["t0"][:, :], t["t1"][:, :], 2 * PI, t["t0"][:, :], ALU.mult, ALU.add)
        p.act(t["sth"][:, :], t["t0"][:, :], AF.Sin)
        p.ts(t["t0"][:, :], t["t0"][:, :], 0.5 * PI, None, op0=ALU.add)
        p.ts(t["t1"][:, :], t["t0"][:, :], PI, None, op0=ALU.is_gt)
        p.stt(t["t0"][:, :], t["t1"][:, :], -2 * PI, t["t0"][:, :], ALU.mult, ALU.add)
        p.act(t["cth"][:, :], t["t0"][:, :], AF.Sin)
        p.tt(t["ar"][:, :], t["mag"][:, :], t["cth"][:, :], ALU.mult)
        p.tt(t["ai"][:, :], t["mag"][:, :], t["sth"][:, :], ALU.mult)
        p.tt(t["t0"][:, :], t["lre"][:, :], t["lre"][:, :], ALU.mult)
        p.tt(t["t1"][:, :], t["lim"][:, :], t["lim"][:, :], ALU.mult)
        p.tt(t["t0"][:, :], t["t0"][:, :], t["t1"][:, :], ALU.add)
        p.recip(t["t0"][:, :], t["t0"][:, :])
        p.ts(t["t1"][:, :], t["ar"][:, :], -1.0, None, op0=ALU.add)
        p.tt(t["fr"][:, :], t["t1"][:, :], t["lre"][:, :], ALU.mult)
        p.tt(t["t2"][:, :], t["ai"][:, :], t["lim"][:, :], ALU.mult)
        p.tt(t["fr"][:, :], t["fr"][:, :], t["t2"][:, :], ALU.add)
        p.tt(t["fr"][:, :], t["fr"][:, :], t["t0"][:, :], ALU.mult)
        p.tt(t["fi"][:, :], t["ai"][:, :], t["lre"][:, :], ALU.mult)
        p.tt(t["t2"][:, :], t["t1"][:, :], t["lim"][:, :], ALU.mult)
        p.tt(t["fi"][:, :], t["fi"][:, :], t["t2"][:, :], ALU.subtract)
        p.tt(t["fi"][:, :], t["fi"][:, :], t["t0"][:, :], ALU.mult)
        prm.append(t)
    dsk = p.sb("s5_dsk", [128, 4])
    p.dma("sp", dsk[:, :], dap(W["s5_d"], l * 512, [[1, 128], [128, 4]]), allow_slow_non_contiguous=True)
    uc = p.sb("s5_u", [128, NB, TA])
    yacc = p.sb("s5_y", [128, NB, TA])
    cosT = p.sb("s5_cos", [128, SEQ])
    sinT = p.sb("s5_sin", [128, SEQ])
    tmp = p.sb("s5_tmp", [128, SEQ])
    xr = p.sb("s5_xr", [128, SEQ])
    xi = p.sb("s5_xi", [128, SEQ])
    wr = p.sb("s5_wr", [128, SEQ])
    wi = p.sb("s5_wi", [128, SEQ])
    t3 = p.sb("s5_t3", [128, SEQ])
    t4 = p.sb("s5_t4", [128, SEQ])
    braw = [p.sb("s5_braw%d" % i, [128, 4, 16]) for i in range(2)]
    craw = [p.sb("s5_craw%d" % i, [128, 4, 16]) for i in range(2)]
    bbs = [p.sb("s5_bbs%d" % i, [128, 4, 16]) for i in range(2)]
    tb = p.sb("s5_tb", [128, 4, 16])
    E = p.sb("s5_E", [128, 8, 16])
    BbT = p.sb("s5_BbT", [128, 2, 4, 2, 128])
    CT = p.sb("s5_CT", [128, 2, 4, 2, 8, 16])
    ini = p.sb("s5_ini", [128, 4])
    cs2 = p.sb("s5_cs2", [128, 8])
    for c in range(4):
        for d in range(2):
            t = prm[d]
            base = (l * 2 + d) * 32 * 1024
            for gg in range(2):
                for r4 in range(4):
                    for (dst, nm) in ((braw[0], "s5_b_re"), (braw[1], "s5_b_im")):
                        p.dma("sp", dst[gg * 64:(gg + 1) * 64, r4, :],
                              dap(W[nm], base + (8 * c + 2 * r4 + gg) * 1024, [[16, 64], [1, 16]]))
                    for (dst, nm) in ((craw[0], "s5_c_re"), (craw[1], "s5_c_im")):
                        p.dma("sp", dst[gg * 64:(gg + 1) * 64, r4, :],
                              dap(W[nm], base + (8 * c + 2 * r4 + gg) * 1024, [[1, 64], [64, 16]]),
                              allow_slow_non_contiguous=True)
            frb = ins(t["fr"][:, 4 * c:4 * c + 4], 2, 16)
            fib = ins(t["fi"][:, 4 * c:4 * c + 4], 2, 16)
            p.tt(bbs[0][:, :, :], braw[0][:, :, :], frb, ALU.mult)
            p.tt(tb[:, :, :], braw[1][:, :, :], fib, ALU.mult)
            p.tt(bbs[0][:, :, :], bbs[0][:, :, :], tb[:, :, :], ALU.subtract)
            p.tt(bbs[1][:, :, :], braw[1][:, :, :], frb, ALU.mult)
            p.tt(tb[:, :, :], braw[0][:, :, :], fib, ALU.mult)
            p.tt(bbs[1][:, :, :], bbs[1][:, :, :], tb[:, :, :], ALU.add)
            for r4 in range(4):
                mk = ins(M[:, r4, :], 2, 16)
                for ri in range(2):
                    p.tt(E[:, :, :], ins(bbs[ri][:, r4, :], 1, 8), mk, ALU.mult)
                    pt = g.ps()
                    p.tr(pt[:, 0:128], E[:, :, :].rearrange("p a b -> p (a b)"), g.ident)
                    p.copy(BbT[:, d, r4, ri, :], pt[:, 0:128])
                p.tt(CT[:, d, r4, 0, :, :], ins(craw[0][:, r4, :], 1, 8), mk, ALU.mult)
                p.stt(CT[:, d, r4, 1, :, :], ins(craw[1][:, r4, :], 1, 8), -1.0, mk, ALU.mult, ALU.mult)
        for b in range(NB):
            for (s, o, T) in seqs:
                p.dma("sp", uc[:, b, o:o + T], g.proj[s][b][OFF_U + c * 128:OFF_U + (c + 1) * 128, :])
        p.ts(yacc[:, :, :], uc[:, :, :], dsk[:, c:c + 1], None, op0=ALU.mult)
        for d in range(2):
            t = prm[d]
            for r4 in range(4):
                r = 4 * c + r4
                p.memset(cosT[:, 0:1], 1.0)
                p.memset(sinT[:, 0:1], 0.0)
                p.copy(cs2[:, 0:1], t["cth"][:, r:r + 1])
                p.copy(cs2[:, 1:2], t["sth"][:, r:r + 1])
                n_ = 1
                while n_ < SEQ:
                    p.ts(cs2[:, 2:3], cs2[:, 1:2], -1.0, None, op0=ALU.mult)
                    p.ts(cosT[:, n_:2 * n_], cosT[:, 0:n_], cs2[:, 0:1], None, op0=ALU.mult)
                    p.stt(cosT[:, n_:2 * n_], sinT[:, 0:n_], cs2[:, 2:3], cosT[:, n_:2 * n_], ALU.mult, ALU.add)
                    p.ts(sinT[:, n_:2 * n_], sinT[:, 0:n_], cs2[:, 0:1], None, op0=ALU.mult)
                    p.stt(sinT[:, n_:2 * n_], cosT[:, 0:n_], cs2[:, 1:2], sinT[:, n_:2 * n_], ALU.mult, ALU.add)
                    n_ *= 2
                    if n_ < SEQ:
                        p.tt(cs2[:, 3:4], cs2[:, 0:1], cs2[:, 1:2], ALU.mult)
                        p.tt(cs2[:, 4:5], cs2[:, 0:1], cs2[:, 0:1], ALU.mult)
                        p.tt(cs2[:, 5:6], cs2[:, 1:2], cs2[:, 1:2], ALU.mult)
                        p.tt(cs2[:, 0:1], cs2[:, 4:5], cs2[:, 5:6], ALU.subtract)
                        p.ts(cs2[:, 1:2], cs2[:, 3:4], 2.0, None, op0=ALU.mult)
                magb = ins(t["mag"][:, r:r + 1], 1, SEQ)
                for b in range(NB):
                    for (s, o, T) in seqs:
                        TT = min(T, 512)
                        fw = (d == 0)
                        cs = cosT[:, 0:T] if fw else rev(cosT[:, 0:T])
                        sn = sinT[:, 0:T] if fw else rev(sinT[:, 0:T])
                        for tt in range(T // TT):
                            sl = slice(tt * TT, (tt + 1) * TT)
                            pt = g.ps()
                            p.mm(pt[:, 0:TT], BbT[:, d, r4, 0, :], uc[:, b, o + tt * TT:o + (tt + 1) * TT])
                            p.mm(pt[:, 512:512 + TT], BbT[:, d, r4, 1, :], uc[:, b, o + tt * TT:o + (tt + 1) * TT])
                            p.act(xr[:, sl], pt[:, 0:TT], AF.Copy)
                            p.act(xi[:, sl], pt[:, 512:512 + TT], AF.Copy)
                        X, Y = xr[:, 0:T], xi[:, 0:T]
                        p.tt(wr[:, 0:T], X, cs, ALU.mult)
                        p.tt(t3[:, 0:T], Y, sn, ALU.mult, eng="pool")
                        p.tt(wr[:, 0:T], wr[:, 0:T], t3[:, 0:T], ALU.add)
                        p.tt(wi[:, 0:T], Y, cs, ALU.mult, eng="pool")
                        p.tt(t4[:, 0:T], X, sn, ALU.mult)
                        p.tt(wi[:, 0:T], wi[:, 0:T], t4[:, 0:T], ALU.subtract, eng="pool")
                        if s == "ctx":
                            ir, ii = 0.0, 0.0
                        else:
                            h0 = g.sts5[:, b, d, r, :]
                            p.tt(ini[:, 0:1], h0[:, 0:1], t["cth"][:, r:r + 1], ALU.mult)
                            p.tt(ini[:, 1:2], h0[:, 1:2], t["sth"][:, r:r + 1], ALU.mult)
                            p.tt(ini[:, 0:1], ini[:, 0:1], ini[:, 1:2], ALU.subtract)
                            p.tt(ini[:, 2:3], h0[:, 0:1], t["sth"][:, r:r + 1], ALU.mult)
                            p.tt(ini[:, 3:4], h0[:, 1:2], t["cth"][:, r:r + 1], ALU.mult)
                            p.tt(ini[:, 2:3], ini[:, 2:3], ini[:, 3:4], ALU.add)
                            ir, ii = ini[:, 0:1], ini[:, 2:3]
                        mb = ins(t["mag"][:, r:r + 1], 1, T)
                        mb = dap(t["mag"], t["mag"][:, r:r + 1].offset, [list(t["mag"][:, r:r + 1].ap[0]), [0, T]])
                        if fw:
                            p.scan(xr[:, 0:T], mb, wr[:, 0:T], ir)
                            p.scan(xi[:, 0:T], mb, wi[:, 0:T], ii)
                        else:
                            p.scan(rev(xr[:, 0:T]), mb, rev(wr[:, 0:T]), ir)
                            p.scan(rev(xi[:, 0:T]), mb, rev(wi[:, 0:T]), ii)
                        p.tt(wr[:, 0:T], X, cs, ALU.mult)
                        p.tt(t3[:, 0:T], Y, sn, ALU.mult, eng="pool")
                        p.tt(wr[:, 0:T], wr[:, 0:T], t3[:, 0:T], ALU.subtract)
                        p.tt(wi[:, 0:T], Y, cs, ALU.mult, eng="pool")
                        p.tt(t4[:, 0:T], X, sn, ALU.mult)
                        p.tt(wi[:, 0:T], wi[:, 0:T], t4[:, 0:T], ALU.add, eng="pool")
                        if s == "ctx":
                            col = T - 1 if fw else 0
                            p.copy(g.sts5[:, b, d, r, 0:1], wr[:, col:col + 1])
                            p.copy(g.sts5[:, b, d, r, 1:2], wi[:, col:col + 1])
                        if last and s == "ctx":
                            continue
                        for tt in range(T // TT):
                            sl = slice(tt * TT, (tt + 1) * TT)
                            pt = g.ps()
                            p.mm(pt[:, 0:TT], CT[:, d, r4, 0, :, :].rearrange("p a b -> p (a b)"), wr[:, sl],
                                 start=True, stop=False)
                            p.mm(pt[:, 0:TT], CT[:, d, r4, 1, :, :].rearrange("p a b -> p (a b)"), wi[:, sl],
                                 start=False, stop=True)
                            ya = yacc[:, b, o + tt * TT:o + (tt + 1) * TT]
                            p.tt(ya, ya, pt[:, 0:TT], ALU.add)
        p.act(yacc[:, :, :], yacc[:, :, :], AF.Gelu_apprx_tanh)
        for b in range(NB):
            for (s, o, T) in seqs:
                if last and s == "ctx":
                    continue
                p.dma("sp", g.ys5[s][b, c * 128:(c + 1) * 128, :], yacc[:, b, o:o + T])
    p.end_phase()
    wg = p.sb("s5_wg", [128, 4, 512], BF16)
    p.dma("pool", wg[:, :, :], dap(W["s5_w_glu"], l * 512 * 512, [[512, 128], [128 * 512, 4], [1, 512]]))
    bgl = p.sb("s5_bgl", [128, 4])
    p.dma("sp", bgl[:, :], dap(W["s5_b_glu"], l * 512, [[1, 128], [128, 4]]), allow_slow_non_contiguous=True)
    yg = [p.sb("s5_yg%d" % i, [128, 4, 512]) for i in range(2)]
    ygb = [p.sb("s5_ygb%d" % i, [128, 4, 512], BF16) for i in range(2)]
    sg = [p.sb("s5_sg%d" % i, [128, 512]) for i in range(2)]
    yo = [p.sb("s5_yo%d" % i, [128, 512], BF16) for i in range(2)]
    it = 0
    for b in range(NB):
        for (s, o, T) in seqs:
            if last and s == "ctx":
                continue
            TT = min(T, 512)
            for tt in range(T // TT):
                y_, yb_ = yg[it % 2], ygb[it % 2]
                it += 1
                src = g.ys5[s][b, :, tt * TT:(tt + 1) * TT]
                p.dma("sp", y_[:, :, 0:TT], dap(src, src.offset, [[T, 128], [128 * T, 4], [1, TT]]))
                p.copy(yb_[:, :, 0:TT], y_[:, :, 0:TT], eng="pool")
                for oc in range(4):
                    pt = g.ps()
                    for kc in range(4):
                        p.mm(pt[:, 0:TT], wg[:, kc, oc * 128:(oc + 1) * 128], yb_[:, kc, 0:TT], start=(kc == 0),
                             stop=(kc == 3))
                    s_, o_ = sg[oc % 2], yo[oc % 2]
                    p.act(s_[:, 0:TT], pt[:, 0:TT], AF.Sigmoid, bias=bgl[:, oc:oc + 1])
                    p.tt(o_[:, 0:TT], y_[:, oc, 0:TT], s_[:, 0:TT], ALU.mult)
                    p.dma("sp", g.yc[s][b, oc * 128:(oc + 1) * 128, tt * TT:(tt + 1) * TT], o_[:, 0:TT])
    p.end_phase()


def phase_dn(g, l, last):
    p, NB, W = g.p, g.NB, g.W
    seqs = (("ctx", CTX), ("lat", SEQ))
    ident64 = g.cst[0:64, C_ID:C_ID + 64]
    ones = g.ones
    cwd = p.sb("dn_cw", [128, 4, 24])
    p.dma("sp", cwd[:, :, :], dap(W["dn_conv_w"], l * 4 * 3072, [[1, 128], [3072, 4], [128, 24]]),
          allow_slow_non_contiguous=True)
    raw = [p.sb("dn_raw%d" % i, [128, SEQ]) for i in range(2)]
    xc = [p.sb("dn_xc%d" % i, [128, SEQ]) for i in range(2)]
    sq = p.sb("dn_sq", [128, SEQ])
    rn = p.sb("dn_rn", [128, SEQ])
    it = 0
    for b in range(NB):
        for (s, T) in seqs:
            L = 64 if s == "lat" else T
            TT = min(T, 512)
            for j in range(24):
                rw, x_ = raw[it % 2], xc[it % 2]
                it += 1
                rows = g.proj[s][b][OFF_Q + j * 128:OFF_Q + (j + 1) * 128, :]
                p.dma("sp", rw[:, 0:T], rows)
                a3 = rw[:, 0:T].rearrange("p (r l) -> p r l", l=L)
                x3 = x_[:, 0:T].rearrange("p (r l) -> p r l", l=L)
                p.ts(x_[:, 0:T], rw[:, 0:T], cwd[:, 2, j:j + 1], None, op0=ALU.mult)
                p.stt(x3[:, :, 2:L], a3[:, :, 0:L - 2], cwd[:, 0, j:j + 1], x3[:, :, 2:L], ALU.mult, ALU.add)
                p.stt(x3[:, :, 1:L], a3[:, :, 0:L - 1], cwd[:, 1, j:j + 1], x3[:, :, 1:L], ALU.mult, ALU.add)
                p.stt(x3[:, :, 0:L - 1], a3[:, :, 1:L], cwd[:, 3, j:j + 1], x3[:, :, 0:L - 1], ALU.mult, ALU.add)
                p.act(x_[:, 0:T], x_[:, 0:T], AF.Silu)
                if j < 16:
                    p.tt(sq[:, 0:T], x_[:, 0:T], x_[:, 0:T], ALU.mult, eng="pool")
                    for tt in range(T // TT):
                        sl = slice(tt * TT, (tt + 1) * TT)
                        pt = g.ps()
                        p.mm(pt[:, 0:TT], ones, sq[:, sl])
                        p.act(rn[:, sl], pt[:, 0:TT], AF.Sqrt, bias=g.epsc[:, 0:1])
                    p.recip(rn[:, 0:T], rn[:, 0:T])
                    sc = (128.0 ** -0.5) if j < 8 else 1.0
                    p.stt(x_[:, 0:T], x_[:, 0:T], sc, rn[:, 0:T], ALU.mult, ALU.mult)
                p.dma("sp", rows, x_[:, 0:T])
    p.end_phase()
    CB = 4
    NCH = SEQ // 64
    alg = p.sb("dn_alg", [64, 16])
    dtb = p.sb("dn_dtb", [64, 16])
    p.dma("sp", alg[:, :], dap(W["dn_a_log"], l * 16, [[0, 64], [1, 16]]))
    p.dma("sp", dtb[:, :], dap(W["dn_dt_bias"], l * 16, [[0, 64], [1, 16]]))
    p.act(alg[:, :], alg[:, :], AF.Exp)
    p.ts(alg[:, :], alg[:, :], -1.0, None, op0=ALU.mult)
    bt = p.sb("dn_bt", [64, NCH, 32])
    bet = p.sb("dn_bet", [64, NCH, 16])
    gt = p.sb("dn_gt", [64, NCH, 16])
    qkv = [[p.sb("dn_%s%d" % (nm, i), [128, 8, CB * 64]) for nm in "qkv"] for i in range(2)]
    S8 = p.sb("dn_S", [128, 8, 128])
    stdn = p.sb("dn_st", [128, 2, 8, 128])
    sm = p.sb("dn_sm", [64, 4, 8])
    gtot = p.sb("dn_gtot", [128, 8])
    gL = p.sb("dn_gL", [64, 8, 64])
    X = p.sb("dn_X", [64, 8, 64])
    D8 = p.sb("dn_D8", [64, 8, 64])
    DT8 = p.sb("dn_DT8", [64, 8, 64])
    P1 = p.sb("dn_P1", [64, 8, 64])
    P2 = p.sb("dn_P2", [64, 8, 64])
    bD = p.sb("dn_bD", [64, 8, 64])
    AtT = p.sb("dn_AtT", [64, 8, 64])
    Nk = [p.sb("dn_N%d" % i, [64, 8, 64], BF16) for i in range(2)]
    YR = [p.sb("dn_YR%d" % i, [64, 8, 2, 64], BF16) for i in range(2)]
    Vb8 = p.sb("dn_Vb", [64, 8, 128], BF16)
    Kbg8 = p.sb("dn_Kbg", [64, 8, 128], BF16)
    Kd8 = p.sb("dn_Kd", [64, 8, 128])
    U8 = p.sb("dn_U", [64, 8, 128])
    Vn8 = p.sb("dn_Vn", [64, 8, 128])
    WT8 = p.sb("dn_WT", [128, 8, 64])
    Qd8 = p.sb("dn_Qd", [128, 8, 64])
    oc = [p.sb("dn_oc%d" % i, [128, 8, 64]) for i in range(2)]
    f3 = lambda ap: ap.rearrange("p a b -> p (a b)")
    for b in range(NB):
        for (s, T) in seqs:
            nch = T // 64
            src = g.proj[s][b][OFF_BETA, :]
            for n_ in range(nch):
                p.dma("sp", bt[:, n_, :], dap(src, src.offset + n_ * 64, [[1, 64], [T, 32]]), allow_slow_non_contiguous=True)
            p.act(bet[:, 0:nch, :], bt[:, 0:nch, 0:16], AF.Sigmoid)
            p.tt(gt[:, 0:nch, :], bt[:, 0:nch, 16:32], ins(dtb[:, :], 1, nch), ALU.add)
            p.act(gt[:, 0:nch, :], gt[:, 0:nch, :], AF.Exp)
            p.act(gt[:, 0:nch, :], gt[:, 0:nch, :], AF.Ln, bias=1.0)
            p.tt(gt[:, 0:nch, :], gt[:, 0:nch, :], ins(alg[:, :], 1, nch), ALU.mult)
            for d in range(2):
                fw = (d == 0)
                LTc = g.cst[0:64, C_LTF:C_LTF + 64] if fw else g.cst[0:64, C_LTB:C_LTB + 64]
                Mi = g.cst[0:64, C_MGT:C_MGT + 64] if fw else g.cst[0:64, C_MLT:C_MLT + 64]
                MT = g.cst[0:64, C_MLT:C_MLT + 64] if fw else g.cst[0:64, C_MGT:C_MGT + 64]
                Si = g.cst[0:64, C_SLT:C_SLT + 64] if fw else g.cst[0:64, C_SGT:C_SGT + 64]
                ST = g.cst[0:64, C_SGT:C_SGT + 64] if fw else g.cst[0:64, C_SLT:C_SLT + 64]
                if s == "ctx":
                    p.memset(S8[:, :, :], 0.0)
                else:
                    p.copy(S8[:, :, :], stdn[:, d, :, :])
                order = list(range(nch)) if fw else list(range(nch - 1, -1, -1))
                cur_blk = None
                for ci, n in enumerate(order):
                    blk = n // CB
                    if blk != cur_blk:
                        cur_blk = blk
                        qb = qkv[(ci // CB) % 2]
                        nb_ = min(CB, nch - blk * CB)
                        for qi, off in enumerate((OFF_Q, OFF_K, OFF_V)):
                            sr = g.proj[s][b][off, blk * CB * 64]
                            p.dma("sp", qb[qi][:, :, 0:nb_ * 64],
                                  dap(sr, sr.offset, [[T, 128], [128 * T, 8], [1, nb_ * 64]]))
                    c0 = (n - blk * CB) * 64
                    qT, kT, vT = (qb[i][:, :, c0:c0 + 64] for i in range(3))
                    g8 = gt[:, n, d * 8:(d + 1) * 8]
                    be8 = bet[:, n, d * 8:(d + 1) * 8]
                    pt = g.ps()
                    p.mm(pt[0:64, 0:8], LTc, g8)
                    p.mm(pt[0:64, 8:16], ones[0:64, 0:64], g8)
                    p.mm(pt[:, 16:24], ones[0:64, :], g8)
                    Gc, Gam, Kdsc, bg = sm[:, 0, :], sm[:, 1, :], sm[:, 2, :], sm[:, 3, :]
                    p.copy(Gc, pt[0:64, 0:8])
                    p.act(Gam, pt[0:64, 0:8], AF.Exp)
                    p.tt(Kdsc, pt[0:64, 8:16], Gc, ALU.subtract)
                    p.act(Kdsc, Kdsc, AF.Exp)
                    p.act(gtot[:, :], pt[:, 16:24], AF.Exp)
                    p.tt(bg, be8, Gam, ALU.mult)
                    p.tt(gL[:, :, :], ins(g8, 2, 64), ins(LTc, 1, 8), ALU.mult)
                    pG = g.ps()
                    p.mm(pG[0:64, 0:512], ones[0:64, 0:64], f3(gL[:, :, :]))
                    p.tt(X[:, :, :], ins(Gc, 2, 64), pG[0:64, 0:512].rearrange("p (a b) -> p a b", b=64), ALU.subtract)
                    p.tt(D8[:, :, :], X[:, :, :], ins(Mi, 1, 8), ALU.subtract)
                    p.act(D8[:, :, :], D8[:, :, :], AF.Exp)
                    p.stt(DT8[:, :, :], X[:, :, :], -1.0, ins(MT, 1, 8), ALU.mult, ALU.subtract)
                    p.act(DT8[:, :, :], DT8[:, :, :], AF.Exp)
                    pK = g.ps()
                    for h in range(8):
                        p.mm(pK[0:64, h * 64:(h + 1) * 64], kT[:, h, :], kT[:, h, :])
                        p.mm(pK[0:64, 512 + h * 64:512 + (h + 1) * 64], kT[:, h, :], qT[:, h, :])
                    pKK = pK[0:64, 0:512].rearrange("p (a b) -> p a b", b=64)
                    pKQ = pK[0:64, 512:1024].rearrange("p (a b) -> p a b", b=64)
                    p.tt(P1[:, :, :], D8[:, :, :], ins(Si, 1, 8), ALU.mult)
                    p.tt(P1[:, :, :], P1[:, :, :], ins(be8, 2, 64), ALU.mult)
                    p.stt(Nk[0][:, :, :], pKK, -1.0, P1[:, :, :], ALU.mult, ALU.mult)
                    p.tt(bD[:, :, :], ins(be8, 2, 64), ins(ident64, 1, 8), ALU.mult)
                    pB = g.ps()
                    p.mm(pB[0:64, 0:512], ones[0:64, 0:64], f3(bD[:, :, :]))
                    p.tt(P2[:, :, :], DT8[:, :, :], ins(ST, 1, 8), ALU.mult)
                    p.tt(P2[:, :, :], P2[:, :, :], pB[0:64, 0:512].rearrange("p (a b) -> p a b", b=64), ALU.mult)
                    p.stt(YR[0][:, :, 0, :], pKK, -1.0, P2[:, :, :], ALU.mult, ALU.mult)
                    p.copy(YR[0][:, :, 1, :], ins(ident64, 1, 8))
                    p.tt(AtT[:, :, :], pKQ, DT8[:, :, :], ALU.mult)
                    for k in range(1, 7):
                        a_, b_ = (k - 1) % 2, k % 2
                        pA = g.ps()
                        for h in range(8):
                            p.mm(pA[0:64, h * 128:(h + 1) * 128], Nk[a_][:, h, :],
                                 YR[a_][:, h, :, :].rearrange("p a b -> p (a b)"))
                        pA4 = pA[0:64, :].rearrange("p (h t c) -> p h t c", t=2, c=64)
                        if k <= 5:
                            pN = g.ps()
                            for h in range(8):
                                p.mm(pN[0:64, h * 64:(h + 1) * 64], YR[a_][:, h, 0, :], Nk[a_][:, h, :])
                            p.act(YR[b_][:, :, 0, :], pA4[:, :, 0, :], AF.Copy)
                        p.tt(YR[b_][:, :, 1, :], pA4[:, :, 1, :], YR[a_][:, :, 1, :], ALU.add)
                        if k <= 5:
                            p.act(Nk[b_][:, :, :], pN[0:64, 0:512].rearrange("p (a b) -> p a b", b=64), AF.Copy)
                    R = YR[0][:, :, 1, :]
                    pTk = g.ps()
                    pTv = g.ps()
                    for h in range(8):
                        p.tr(pTk[0:64, h * 128:(h + 1) * 128], kT[:, h, :], g.ident)
                        p.tr(pTv[0:64, h * 128:(h + 1) * 128], vT[:, h, :], g.ident)
                    pTk3 = pTk[0:64, :].rearrange("p (a b) -> p a b", b=128)
                    pTv3 = pTv[0:64, :].rearrange("p (a b) -> p a b", b=128)
                    p.tt(Vb8[:, :, :], pTv3, ins(be8, 2, 128), ALU.mult)
                    p.tt(Kbg8[:, :, :], pTk3, ins(bg, 2, 128), ALU.mult)
                    p.tt(Kd8[:, :, :], pTk3, ins(Kdsc, 2, 128), ALU.mult)
                    pU = g.ps()
                    pW = g.ps()
                    for h in range(8):
                        p.mm(pU[0:64, h * 128:(h + 1) * 128], R[:, h, :], Vb8[:, h, :])
                        p.mm(pW[:, h * 64:(h + 1) * 64], Kbg8[:, h, :], R[:, h, :])
                    p.act(f3(U8[:, :, :]), pU[0:64, :], AF.Copy)
                    p.act(f3(WT8[:, :, :]), pW[:, 0:512], AF.Copy)
                    p.tt(bD[:, :, :], ins(Gam, 2, 64), ins(ident64, 1, 8), ALU.mult)
                    pGm = g.ps()
                    p.mm(pGm[:, 0:512], ones[0:64, :], f3(bD[:, :, :]))
                    p.tt(Qd8[:, :, :], qT, pGm[:, 0:512].rearrange("p (a b) -> p a b", b=64), ALU.mult)
                    pWS = g.ps()
                    for h in range(8):
                        p.mm(pWS[0:64, h * 128:(h + 1) * 128], WT8[:, h, :], S8[:, h, :])
                    p.tt(f3(Vn8[:, :, :]), f3(U8[:, :, :]), pWS[0:64, :], ALU.subtract)
                    pO = g.ps()
                    for h in range(8):
                        p.mm(pO[:, h * 64:(h + 1) * 64], S8[:, h, :], Qd8[:, h, :], start=True, stop=False)
                        p.mm(pO[:, h * 64:(h + 1) * 64], Vn8[:, h, :], AtT[:, h, :], start=False, stop=True)
                    if not (last and s == "ctx"):
                        o_ = oc[ci % 2]
                        p.act(f3(o_[:, :, :]), pO[:, 0:512], AF.Copy)
                        dst = g.odn[s][d, b, 0, n * 64]
                        p.dma("sp", dap(dst, dst.offset, [[T, 128], [128 * T, 8], [1, 64]]), o_[:, :, :])
                    pdS = g.ps()
                    for h in range(8):
                        p.mm(pdS[:, h * 128:(h + 1) * 128], Kd8[:, h, :], Vn8[:, h, :])
                    p.tt(S8[:, :, :], S8[:, :, :], ins(gtot[:, :], 2, 128), ALU.mult)
                    p.tt(f3(S8[:, :, :]), f3(S8[:, :, :]), pdS[:, :], ALU.add)
                if s == "ctx":
                    p.copy(stdn[:, d, :, :], S8[:, :, :])
    p.end_phase()
    ng = p.sb("dn_ng", [128, 1])
    p.dma("sp", ng[:, :], dap(W["dn_norm_g"], l * 128, [[1, 128], [1, 1]]))
    of = [p.sb("dn_of%d" % i, [128, SEQ]) for i in range(2)]
    ob = [p.sb("dn_ob%d" % i, [128, SEQ]) for i in range(2)]
    zt = [p.sb("dn_z%d" % i, [128, SEQ]) for i in range(2)]
    yo = [p.sb("dn_yo%d" % i, [128, SEQ], BF16) for i in range(2)]
    rs = p.sb("dn_rs", [128, SEQ])
    it = 0
    for b in range(NB):
        for (s, T) in seqs:
            if last and s == "ctx":
                continue
            TT = min(T, 512)
            for h in range(8):
                o1, o2, z_, y_ = of[it % 2], ob[it % 2], zt[it % 2], yo[it % 2]
                it += 1
                p.dma("sp", o1[:, 0:T], g.odn[s][0, b, h * 128:(h + 1) * 128, :])
                p.dma("sp", o2[:, 0:T], g.odn[s][1, b, h * 128:(h + 1) * 128, :])
                p.dma("sp", z_[:, 0:T], g.proj[s][b][OFF_Z + h * 128:OFF_Z + (h + 1) * 128, :])
                p.tt(o1[:, 0:T], o1[:, 0:T], o2[:, 0:T], ALU.add)
                p.tt(o2[:, 0:T], o1[:, 0:T], o1[:, 0:T], ALU.mult, eng="pool")
                for tt in range(T // TT):
                    sl = slice(tt * TT, (tt + 1) * TT)
                    pt = g.ps()
                    p.mm(pt[:, 0:TT], ones, o2[:, sl])
                    p.act(rs[:, sl], pt[:, 0:TT], AF.Sqrt, scale=1.0 / 128, bias=g.epsc[:, 0:1])
                p.recip(rs[:, 0:T], rs[:, 0:T])
                p.act(z_[:, 0:T], z_[:, 0:T], AF.Silu)
                p.stt(o1[:, 0:T], o1[:, 0:T], ng[:, 0:1], rs[:, 0:T], ALU.mult, ALU.mult)
                p.tt(y_[:, 0:T], o1[:, 0:T], z_[:, 0:T], ALU.mult)
                p.dma("sp", g.yb[s][b, h * 128:(h + 1) * 128, :], y_[:, 0:T])
    p.end_phase()


def phase_merge(g, l, last):
    p, NB, W = g.p, g.NB, g.W
    wa = p.sb("mg_wa", [128, 4, D], BF16)
    wb = p.sb("mg_wb", [128, 8, D], BF16)
    wc = p.sb("mg_wc", [128, 4, D], BF16)
    wo = p.sb("mg_wo", [128, 8, D], BF16)
    import os
    MGS = int(os.environ.get("MG_STOP", "9"))
    for hh in range(2):
        cs = slice(hh * 512, (hh + 1) * 512)
        p.dma("pool", wa[:, :, cs], dap(W["w_br_a"], l * 512 * D + hh * 512, [[D, 128], [128 * D, 4], [1, 512]]))
        p.dma("pool", wb[:, :, cs], dap(W["w_br_b"], l * D * D + hh * 512, [[D, 128], [128 * D, 8], [1, 512]]))
        p.dma("pool", wc[:, :, cs], dap(W["w_br_c"], l * 512 * D + hh * 512, [[D, 128], [128 * D, 4], [1, 512]]))
        p.dma("pool", wo[:, :, cs], dap(W["w_out"], l * D * D + hh * 512, [[D, 128], [128 * D, 8], [1, 512]]))
    if MGS == 1:
        p.end_phase()
        return
    yat = p.sb("mg_ya", [128, 4, 512], BF16)
    ybt = p.sb("mg_yb", [128, 8, 512], BF16)
    yct = p.sb("mg_yc", [128, 4, 512], BF16)
    g3 = [p.sb("mg_g%d" % i, [128, 3, 512]) for i in range(2)]
    m32 = p.sb("mg_m", [128, 512])
    t32 = p.sb("mg_t", [128, 512])
    mT = p.sb("mg_mT", [128, 8, 512], BF16)
    xt = [p.sb("mg_x%d" % i, [128, D]) for i in range(2)]
    tx = p.sb("mg_tx", [128, 512])
    xi = 0
    for b in range(NB):
        for s in ("ctx", "lat"):
            if last and s == "ctx":
                continue
            T = g.T[s]
            v = b if s == "lat" else NB
            GM = load_mod_bcast(g, l, v, 2, "mg_GM")
            TT = min(T, 512)
            for tt in range(T // TT):
                c0 = tt * TT
                for (dst, srcd, nk) in ((yat, g.ya, 4), (ybt, g.yb, 8), (yct, g.yc, 4)):
                    sr = srcd[s][b, 0, c0]
                    p.dma("sp", dst[:, :, 0:TT], dap(sr, sr.offset, [[T, 128], [128 * T, nk], [1, TT]]))
                for oc in range(8):
                    gt_ = g3[oc % 2]
                    for br in range(3):
                        r0 = OFF_GATE + br * D + oc * 128
                        p.dma("sp", gt_[:, br, 0:TT], g.proj[s][b][r0:r0 + 128, c0:c0 + TT])
                    p.act(gt_[:, :, 0:TT], gt_[:, :, 0:TT], AF.Sigmoid)
                    for br, (wt, yt, nk) in enumerate(((wa, yat, 4), (wb, ybt, 8), (wc, yct, 4))):
                        pt = g.ps()
                        for kc in range(nk):
                            p.mm(pt[:, 0:TT], wt[:, kc, oc * 128:(oc + 1) * 128], yt[:, kc, 0:TT], start=(kc == 0),
                                 stop=(kc == nk - 1))
                        if br == 0:
                            p.tt(m32[:, 0:TT], gt_[:, 0, 0:TT], pt[:, 0:TT], ALU.mult)
                        else:
                            p.tt(t32[:, 0:TT], gt_[:, br, 0:TT], pt[:, 0:TT], ALU.mult)
                            if br == 1:
                                p.tt(m32[:, 0:TT], m32[:, 0:TT], t32[:, 0:TT], ALU.add)
                            else:
                                p.tt(mT[:, oc, 0:TT], m32[:, 0:TT], t32[:, 0:TT], ALU.add)
                for st in range(TT // 128):
                    x_ = xt[xi % 2]
                    xi += 1
                    r0 = b * T + c0 + st * 128
                    p.dma("sp", x_[:, :], g.xs[s][r0:r0 + 128, :])
                    for half in range(2):
                        po = g.ps()
                        for kc in range(8):
                            p.mm(po[:, 0:512], mT[:, kc, st * 128:(st + 1) * 128], wo[:, kc, half * 512:(half + 1) * 512],
                                 start=(kc == 0), stop=(kc == 7))
                        p.tt(tx[:, :], po[:, 0:512], GM[:, half * 512:(half + 1) * 512], ALU.mult)
                        p.tt(x_[:, half * 512:(half + 1) * 512], x_[:, half * 512:(half + 1) * 512], tx[:, :], ALU.add)
                    p.dma("sp", g.xs[s][r0:r0 + 128, :], x_[:, :])
    p.end_phase()


MT_ = 512


def phase_moe(g, l, last):
    p, NB, W = g.p, g.NB, g.W
    macros = []
    for b in range(NB):
        if not last:
            macros.append(("ctx", b, 0, CTX))
        for t0 in range(0, SEQ, MT_):
            macros.append(("lat", b, t0, MT_))
    for (s, b, t0, MT) in macros:
        T = g.T[s]
        v = b if s == "lat" else NB
        r0 = b * T + t0
        G2 = load_mod_bcast(g, l, v, 4, "mo_G2")
        SH2 = load_mod_bcast(g, l, v, 3, "mo_SH2")
        gf = p.sb("mo_gf", [128, D])
        src = W["g_ffn"][l, :]
        p.dma("sp", gf[:, :], dap(src, src.offset, [[0, 128], [1, D]]))
        p.stt(G2[:, :], G2[:, :], 1.0, gf[:, :], ALU.add, ALU.mult)
        wr = p.sb("mo_wr", [128, 8, NE])
        p.dma("sp", wr[:, :, :], dap(W["w_router"], l * D * NE, [[NE, 128], [128 * NE, 8], [1, NE]]))
        brt = p.sb("mo_br", [128, NE])
        p.dma("sp", brt[:, :], dap(W["b_router"], l * NE, [[0, 128], [1, NE]]))
        lg = p.sb("mo_lg", [128, NE])
        ex = p.sb("mo_ex", [128, NE])
        mk = p.sb("mo_mk", [128, NE])
        mx = p.sb("mo_mx", [128, 8])
        sc = p.sb("mo_sc", [128, 4])

        def router(tt, h32):
            pr = g.ps()
            for kc in range(8):
                p.mm(pr[:, 0:NE], h32[:, kc, :], wr[:, kc, :], start=(kc == 0), stop=(kc == 7))
            p.tt(lg[:, :], pr[:, 0:NE], brt[:, :], ALU.add)
            p.op("dve", lambda e: e.max(out=mx[:, :], in_=lg[:, :]), [lg[:, :]], [mx[:, :]])
            p.ts(mk[:, :], lg[:, :], mx[:, 3:4], None, op0=ALU.is_ge)
            p.ts(sc[:, 0:1], mx[:, 0:1], -1.0, None, op0=ALU.mult)
            p.act(ex[:, :], lg[:, :], AF.Exp, bias=sc[:, 0:1])
            p.tt(ex[:, :], ex[:, :], mk[:, :], ALU.mult)
            p.op("dve", lambda e: e.reduce_sum(out=sc[:, 1:2], in_=ex[:, :], axis=AX.X), [ex[:, :]], [sc[:, 1:2]])
            p.recip(sc[:, 2:3], sc[:, 1:2])
            p.ts(g.Wg[:, tt, :], ex[:, :], sc[:, 2:3], None, op0=ALU.mult)
            pw = g.ps()
            p.tr(pw[0:NE, 0:128], g.Wg[:, tt, :], g.ident)
            p.copy(g.WgT[:, tt, :], pw[0:NE, 0:128])

        norm_tiles(g, g.xs[s][r0:r0 + MT, :], MT, G2, SH2, g.hT2, router)
        p.end_phase()
        import os
        MOS = int(os.environ.get("MOE_STOP", "9"))
        if MOS == 1:
            return
        nt = MT // 128
        yacc = p.sb("mo_y", [128, nt, D])
        b2all = p.sb("mo_b2all", [NE, D])
        p.dma("sp", b2all[:, :], dap(W["b_e2"], l * NE * D, [[D, NE], [1, D]]))
        for st in range(nt):
            for half in range(2):
                po = g.ps()
                p.mm(po[:, 0:512], g.WgT[:, st, :], b2all[:, half * 512:(half + 1) * 512])
                p.copy(yacc[:, st, half * 512:(half + 1) * 512], po[:, 0:512], eng="act")
        W1 = [p.sb("mo_W1_%d" % i, [128, 8, 2 * D], BF16) for i in range(2)]
        W2 = [p.sb("mo_W2_%d" % i, [128, 8, D], BF16) for i in range(2)]
        b1 = [p.sb("mo_b1_%d" % i, [128, 16]) for i in range(2)]
        actT = p.sb("mo_act", [128, 8, MT], BF16)
        gl = [p.sb("mo_gl%d" % i, [128, MT]) for i in range(2)]
        sg = [p.sb("mo_sg%d" % i, [128, MT]) for i in range(2)]
        l1 = [p.sb("mo_l1%d" % i, [128, MT]) for i in range(2)]
        tz = [p.sb("mo_tz%d" % i, [128, 512]) for i in range(2)]
        zi = 0
        for e in range(NE):
            w1, w2, b1t = W1[e % 2], W2[e % 2], b1[e % 2]
            for hh in range(2):
                sr = g.e1bf[l][e, 0, hh * D]
                p.dma("sp", w1[:, :, hh * D:(hh + 1) * D], dap(sr, sr.offset, [[2 * D, 128], [128 * 2 * D, 8], [1, D]]))
            sr = g.e2bf[l][e, 0, 0]
            p.dma("sp", w2[:, :, :], dap(sr, sr.offset, [[D, 128], [128 * D, 8], [1, D]]))
            p.dma("sp", b1t[:, :], dap(W["b_e1"], (l * NE + e) * 2 * D, [[1, 128], [128, 16]]), allow_slow_non_contiguous=True)
            for j in range(8):
                pg = g.ps()
                for kc in range(8):
                    p.mm(pg[:, 0:MT], w1[:, kc, j * 128:(j + 1) * 128], g.hT2[:, kc, 0:MT], start=(kc == 0), stop=(kc == 7))
                for kc in range(8):
                    p.mm(pg[:, 512:512 + MT], w1[:, kc, D + j * 128:D + (j + 1) * 128], g.hT2[:, kc, 0:MT], start=(kc == 0),
                         stop=(kc == 7))
                g_, s_, l_ = gl[j % 2], sg[j % 2], l1[j % 2]
                p.ts(g_[:, :], pg[:, 0:MT], b1t[:, j:j + 1], 7.0, op0=ALU.add, op1=ALU.min)
                p.act(l_[:, :], pg[:, 512:512 + MT], AF.Identity, bias=b1t[:, 8 + j:9 + j])
                p.act(s_[:, :], g_[:, :], AF.Sigmoid, scale=1.702)
                p.ts(l_[:, :], l_[:, :], -7.0, 7.0, op0=ALU.max, op1=ALU.min)
                p.tt(g_[:, :], g_[:, :], s_[:, :], ALU.mult)
                p.stt(actT[:, j, :], l_[:, :], 1.0, g_[:, :], ALU.add, ALU.mult)
            for st in range(nt):
                for half in range(2):
                    po = g.ps()
                    for kc in range(8):
                        p.mm(po[:, 0:512], actT[:, kc, st * 128:(st + 1) * 128], w2[:, kc, half * 512:(half + 1) * 512],
                             start=(kc == 0), stop=(kc == 7))
                    ya = yacc[:, st, half * 512:(half + 1) * 512]
                    p.stt(ya, po[:, 0:512], g.Wg[:, st, e:e + 1], ya, ALU.mult, ALU.add)
        GMLP = load_mod_bcast(g, l, v, 5, "mo_GMLP")
        xt = [p.sb("mo_x%d" % i, [128, D]) for i in range(2)]
        for st in range(nt):
            x_ = xt[st % 2]
            rr = r0 + st * 128
            p.dma("sp", x_[:, :], g.xs[s][rr:rr + 128, :])
            p.tt(yacc[:, st, :], yacc[:, st, :], GMLP[:, :], ALU.mult)
            p.tt(x_[:, :], x_[:, :], yacc[:, st, :], ALU.add)
            p.dma("sp", g.xs[s][rr:rr + 128, :], x_[:, :])
        p.end_phase()
        if MOS in (2, 3):
            return


def phase_final(g):
    p, NB, W = g.p, g.NB, g.W
    gfin = p.sb("fn_g", [128, D])
    p.dma("sp", gfin[:, :], dap(W["g_final"], 0, [[0, 128], [1, D]]))
    xts = [p.sb("fn_x%d" % i, [128, D]) for i in range(2)]
    sq = p.sb("fn_sq", [128, D])
    ss = p.sb("fn_ss", [128, 4])
    for tt in range(NB * SEQ // 128):
        xt = xts[tt % 2]
        p.dma("sp", xt[:, :], g.xs["lat"][tt * 128:(tt + 1) * 128, :])
        p.act(sq[:, :], xt[:, :], AF.Square)
        p.op("dve", lambda e, o=ss[:, 0:1], i=sq[:, :]: e.reduce_sum(out=o, in_=i, axis=AX.X), [sq[:, :]], [ss[:, 0:1]])
        p.ts(ss[:, 1:2], ss[:, 0:1], 1.0 / D, EPS, op0=ALU.mult, op1=ALU.add)
        p.act(ss[:, 2:3], ss[:, 1:2], AF.Sqrt)
        p.recip(ss[:, 3:4], ss[:, 2:3])
        p.stt(xt[:, :], xt[:, :], ss[:, 3:4], gfin[:, :], ALU.mult, ALU.mult)
        p.dma("sp", g.out[tt * 128:(tt + 1) * 128, :], xt[:, :])
    p.end_phase()


_NC_CACHE = {}


def kernel(**inputs):
    NCORES = 8
    NB = 32 // NCORES
    if "nc" not in _NC_CACHE:
        _NC_CACHE["nc"] = build_program(NB=NB)
    nc = _NC_CACHE["nc"]
    consts = make_consts()
    f32 = lambda a: np.ascontiguousarray(np.asarray(a, dtype=np.float32))
    wts = {n: f32(inputs[n]).reshape(WSHAPES[n]) for n in WNAMES}
    x = f32(inputs["x"])
    ctx = f32(inputs["ctx"])
    c = f32(inputs["c"])
    cctx = f32(inputs["c_ctx"]).reshape(1, D)
    in_maps = []
    for i in range(NCORES):
        m = {"x": x[i * NB:(i + 1) * NB].reshape(NB * SEQ, D), "ctx": ctx[i * NB:(i + 1) * NB].reshape(NB * CTX, D),
             "c": c[i * NB:(i + 1) * NB], "c_ctx": cctx, "consts": consts}
        m.update(wts)
        in_maps.append(m)
    res = run_bass_kernel_spmd(nc, in_maps, core_ids=list(range(NCORES)))
    out = np.concatenate([r["out"].reshape(NB, SEQ, D) for r in res.results], axis=0)
    return out.astype(np.float32)
```

```python
import contextlib
import math
import numpy as np
import concourse.bass as bass
import concourse.mybir as mybir
from concourse.bass_utils import run_bass_kernel_spmd

F32 = mybir.dt.float32
BF16 = mybir.dt.bfloat16
ALU = mybir.AluOpType
AF = mybir.ActivationFunctionType
AX = mybir.AxisListType

D = 1024
SEQ = 2048
CTX = 256
DEPTH = 2
D_IN = 8736
NE = 32
EPS = 1e-6
OFF_AX, OFF_AY, OFF_Q, OFF_K, OFF_V, OFF_Z, OFF_BETA, OFF_ALPHA, OFF_U, OFF_GATE = (
    0, 512, 1024, 2048, 3072, 4096, 5120, 5136, 5152, 5664)

SAME_ENGINE_SYNC = True
RELAX_SAME = True
NDSEM = 12


class Buf:
    __slots__ = ("w", "r")

    def __init__(self):
        self.w = {}
        self.r = {}


class Prog:
    ENG = ("pe", "act", "dve", "pool", "sp")

    def __init__(self, nc):
        self.nc = nc
        self.ops = {e: [] for e in self.ENG}
        self.cnt = {e: 0 for e in self.ENG}
        self.known = {e: {} for e in self.ENG}
        self.dq = {e: 0 for e in self.ENG}
        self.dval = {}
        self.bufs = {}
        self.st = contextlib.ExitStack()
        self.uid = 0
        self.raw_same = {}

    def sb(self, name, shape, dtype=F32):
        self.uid += 1
        t = self.st.enter_context(self.nc.sbuf_tensor("%s_%d" % (name, self.uid), list(shape), dtype))
        return t

    def psum(self, name, shape, dtype=F32):
        return self.st.enter_context(self.nc.psum_tensor(name, list(shape), dtype))

    def dram(self, name, shape, dtype=F32, kind="Internal"):
        return self.nc.dram_tensor(name, list(shape), dtype, kind=kind)

    def _buf(self, ap):
        n = ap.tensor.name
        b = self.bufs.get(n)
        if b is None:
            b = self.bufs[n] = Buf()
        return b

    def _sched(self, e, reads, writes, tok_fn):
        d = {}
        rb = [self._buf(a) for a in reads if a is not None and not isinstance(a, (int, float))]
        wb = [self._buf(a) for a in writes]
        rawv = 0
        for b in rb:
            rawv = max(rawv, b.w.get(e, 0))
        self.raw_same = {e: (self.cnt[e] if rawv < self.cnt[e] - 3 else rawv - 1) if rawv else self.cnt[e]}
        if rawv and rawv >= self.cnt[e] - 3:
            self.raw_same = {e: rawv - 1}
        else:
            self.raw_same = {e: self.cnt[e]}
        for a, b in zip([a for a in reads if a is not None and not isinstance(a, (int, float))], rb):
            for k, v in b.w.items():
                if d.get(k, 0) < v:
                    d[k] = v
            if a.tensor.name.startswith("ps"):
                for k, v in b.r.items():
                    if k != e and d.get(k, 0) < v:
                        d[k] = v
        for b in wb:
            for k, v in b.w.items():
                if d.get(k, 0) < v:
                    d[k] = v
            for k, v in b.r.items():
                if d.get(k, 0) < v:
                    d[k] = v
        return d, rb, wb

    def _waits(self, e, d):
        kn = self.known[e]
        for k, v in d.items():
            if k == e and ((not SAME_ENGINE_SYNC) or e in ("pe", "sp")):
                continue
            if k == e and RELAX_SAME and v <= self.raw_same.get(e, 0):
                continue
            if kn.get(k, 0) >= v:
                continue
            kn[k] = v
            self.ops[e].append(("wait", k, v))

    def op(self, e, fn, reads, writes):
        d, rb, wb = self._sched(e, reads, writes, None)
        self._waits(e, d)
        self.cnt[e] += 1
        k, v = e, self.cnt[e]
        self.ops[e].append(("op", fn))
        for b in rb:
            if b.r.get(k, 0) < v:
                b.r[k] = v
        for b in wb:
            b.w[k] = v
            b.r = {}

    def dma(self, q, out, in_, **kw):
        d, rb, wb = self._sched(q, [in_], [out], None)
        j = self.dq[q]
        self.dq[q] = (j + 1) % (4 if q == "pool" else NDSEM)
        key = ("d", q, j)
        prev = self.dval.get(key, 0)
        if prev:
            d[key] = max(d.get(key, 0), prev)
        self._waits(q, d)
        val = prev + 16
        self.dval[key] = val
        self.ops[q].append(("dma", out, in_, key, kw))
        for b in rb:
            if b.r.get(key, 0) < val:
                b.r[key] = val
        for b in wb:
            b.w[key] = val
            b.r = {}

    def mm(self, out, lhsT, rhs, start=True, stop=True):
        self.op("pe", lambda e: e.matmul(out, lhsT=lhsT, rhs=rhs, start=start, stop=stop), [lhsT, rhs], [out])

    def tr(self, out, in_, ident):
        self.op("pe", lambda e: e.transpose(out, in_, ident), [in_, ident], [out])

    def act(self, out, in_, func, bias=None, scale=None, accum_out=None, eng="act"):
        kw = {}
        rd = [in_]
        wr = [out]
        if bias is not None:
            kw["bias"] = bias
            rd.append(bias)
        if scale is not None:
            kw["scale"] = scale
            rd.append(scale)
        if accum_out is not None:
            kw["accum_out"] = accum_out
            wr.append(accum_out)
        self.op("act", lambda e: e.activation(out=out, in_=in_, func=func, **kw), rd, wr)

    def tt(self, out, in0, in1, op, eng="dve"):
        self.op(eng, lambda e: e.tensor_tensor(out=out, in0=in0, in1=in1, op=op), [in0, in1], [out])

    def ts(self, out, in0, s1, s2=None, op0=ALU.mult, op1=None, eng="dve"):
        kw = {}
        if op1 is not None:
            kw["op1"] = op1
        self.op(eng, lambda e: e.tensor_scalar(out=out, in0=in0, scalar1=s1, scalar2=s2, op0=op0, **kw),
                [in0, s1, s2], [out])

    def stt(self, out, in0, scalar, in1, op0, op1, eng="dve"):
        self.op(eng, lambda e: e.scalar_tensor_tensor(out=out, in0=in0, scalar=scalar, in1=in1, op0=op0, op1=op1),
                [in0, scalar, in1], [out])

    def copy(self, out, in_, eng="dve"):
        if eng == "act":
            self.act(out, in_, AF.Copy)
        else:
            self.op(eng, lambda e: e.tensor_copy(out=out, in_=in_), [in_], [out])

    def memset(self, out, val, eng="dve"):
        self.op(eng, lambda e: e.memset(out, val), [], [out])

    def recip(self, out, in_):
        self.op("dve", lambda e: e.reciprocal(out=out, in_=in_), [in_], [out])

    def scan(self, out, d0, d1, init, eng="dve"):
        self.op(eng, lambda e: e.tensor_tensor_scan(out=out, data0=d0, data1=d1, initial=init, op0=ALU.mult,
                                                    op1=ALU.add), [d0, d1, init], [out])

    def barrier(self):
        for e in self.ENG:
            d = {}
            for e2 in self.ENG:
                if e2 != e and self.cnt[e2]:
                    d[e2] = self.cnt[e2]
            for key, val in self.dval.items():
                d[key] = val
            self._waits(e, d)

    def setup(self):
        nc = self.nc
        self.gst = contextlib.ExitStack()
        self.sems = {}
        for e in self.ENG:
            self.sems[e] = self.gst.enter_context(nc.semaphore("s_" + e))
        for q in ("sp", "pool", "act"):
            for j in range(NDSEM):
                self.sems[("d", q, j)] = self.gst.enter_context(nc.semaphore("d_%s_%d" % (q, j)))
        self.st = contextlib.ExitStack()

    def gsb(self, name, shape, dtype=F32):
        return self.gst.enter_context(self.nc.sbuf_tensor(name, list(shape), dtype))

    def gpsum(self, name, shape, dtype=F32):
        return self.gst.enter_context(self.nc.psum_tensor(name, list(shape), dtype))

    def end_phase(self, final=False):
        nc = self.nc
        self.barrier()
        sems = self.sems
        ops = self.ops

        def run(e, eng):
            se = sems[e]
            for o in ops[e]:
                if o[0] == "wait":
                    eng.wait_ge(sems[o[1]], o[2])
                elif o[0] == "op":
                    o[1](eng).then_inc(se, 1)
                else:
                    _, out, in_, key, kw = o
                    eng.dma_start(out=out, in_=in_, **kw).then_inc(sems[key], 16)

        with nc.Block() as block:
            block.sync(lambda eng: run("sp", eng))
            block.tensor(lambda eng: run("pe", eng))
            block.scalar(lambda eng: run("act", eng))
            block.vector(lambda eng: run("dve", eng))
            block.gpsimd(lambda eng: run("pool", eng))
        self.ops = {e: [] for e in self.ENG}
        self.st.close()
        self.st = contextlib.ExitStack()
        if final:
            self.gst.close()


def rev(ap):
    a = [list(x) for x in ap.ap]
    step, n = a[-1]
    a[-1] = [-step, n]
    return bass.AP(ap.tensor, ap.offset + step * (n - 1), a)


def bcast_rows(ap, nparts):
    a = [list(x) for x in ap.ap]
    return bass.AP(ap.tensor, ap.offset, [[0, nparts]] + a[-1:])


WNAMES = ["w_ada", "b_ada", "g_mix", "g_ffn", "w_in", "lru_conv_w", "lru_conv_b", "lru_w_gate", "lru_b_gate",
          "lru_lam", "dn_conv_w", "dn_a_log", "dn_dt_bias", "dn_norm_g", "s5_lam_re", "s5_lam_im", "s5_log_dt",
          "s5_b_re", "s5_b_im", "s5_c_re", "s5_c_im", "s5_d", "s5_w_glu", "s5_b_glu", "w_br_a", "w_br_b", "w_br_c",
          "w_out", "w_router", "b_router", "w_e1", "b_e1", "w_e2", "b_e2", "g_final"]


WSHAPES = {
    "w_ada": [2, 1024, 6144], "b_ada": [2, 6144], "g_mix": [2, 1024], "g_ffn": [2, 1024], "w_in": [2, 1024, 8736],
    "lru_conv_w": [2, 4, 512], "lru_conv_b": [2, 512], "lru_w_gate": [2, 2, 2, 8, 64, 64], "lru_b_gate": [2, 2, 2, 512],
    "lru_lam": [2, 2, 512], "dn_conv_w": [2, 4, 3072], "dn_a_log": [2, 2, 8], "dn_dt_bias": [2, 2, 8],
    "dn_norm_g": [2, 128], "s5_lam_re": [2, 2, 32, 64], "s5_lam_im": [2, 2, 32, 64], "s5_log_dt": [2, 2, 32],
    "s5_b_re": [2, 2, 32, 64, 16], "s5_b_im": [2, 2, 32, 64, 16], "s5_c_re": [2, 2, 32, 16, 64],
    "s5_c_im": [2, 2, 32, 16, 64], "s5_d": [2, 512], "s5_w_glu": [2, 512, 512], "s5_b_glu": [2, 512],
    "w_br_a": [2, 512, 1024], "w_br_b": [2, 1024, 1024], "w_br_c": [2, 512, 1024], "w_out": [2, 1024, 1024],
    "w_router": [2, 1024, 32], "b_router": [2, 32], "w_e1": [2, 32, 1024, 2048], "b_e1": [2, 32, 2048],
    "w_e2": [2, 32, 1024, 1024], "b_e2": [2, 32, 1024], "g_final": [1, 1024],
}
BIG = 30000.0
C_ID, C_ONE, C_LTF, C_LTB, C_MGT, C_MLT, C_SGT, C_SLT, C_IOTA, C_END = 0, 128, 256, 320, 384, 448, 512, 576, 640, 640 + 2048


def make_consts():
    c = np.zeros((128, C_END), np.float32)
    c[:, C_ID:C_ID + 128] = np.eye(128, dtype=np.float32)
    c[:, C_ONE:C_ONE + 128] = 1.0
    p = np.arange(128)[:, None]
    f = np.arange(64)[None, :]
    c[:, C_LTF:C_LTF + 64] = (p <= f) & (p < 64)
    c[:, C_LTB:C_LTB + 64] = (p >= f) & (p < 64)
    c[:, C_MGT:C_MGT + 64] = BIG * (f > p)
    c[:, C_MLT:C_MLT + 64] = BIG * (f < p)
    c[:, C_SGT:C_SGT + 64] = (f > p)
    c[:, C_SLT:C_SLT + 64] = (f < p)
    c[:, C_IOTA:C_IOTA + 2048] = np.arange(2048, dtype=np.float32)[None, :]
    return c


def dap(t, offset, dims):
    return bass.AP(t.tensor if hasattr(t, "tensor") else t, offset, [list(d) for d in dims])


class Ctx:
    pass


def build_program(NB=4, layers=(0, 1), stop_after=None, dbg=(), skip=(), inject=()):
    nc = bass.Bass("TRN2", target_bir_lowering=False)
    p = Prog(nc)
    p.setup()
    NV = NB + 1
    g = Ctx()
    g.NB, g.NV, g.p, g.nc = NB, NV, p, nc
    g.skip = set(skip)
    x_in = nc.dram_tensor("x", [NB * SEQ, D], F32, kind="ExternalInput").ap()
    ctx_in = nc.dram_tensor("ctx", [NB * CTX, D], F32, kind="ExternalInput").ap()
    c_in = nc.dram_tensor("c", [NB, D], F32, kind="ExternalInput").ap()
    cctx_in = nc.dram_tensor("c_ctx", [1, D], F32, kind="ExternalInput").ap()
    consts_in = nc.dram_tensor("consts", [128, C_END], F32, kind="ExternalInput").ap()
    W = {}
    for name in WNAMES:
        W[name] = nc.dram_tensor(name, WSHAPES[name], F32, kind="ExternalInput").ap()
    out = nc.dram_tensor("out", [NB * SEQ, D], F32, kind="ExternalOutput").ap()
    g.W, g.out = W, out

    g.xs = {"lat": p.dram("xs_lat", [NB * SEQ, D]).ap(), "ctx": p.dram("xs_ctx", [NB * CTX, D]).ap()}
    g.T = {"lat": SEQ, "ctx": CTX}
    g.mod = p.dram("mod", [DEPTH, NV, 6 * D]).ap()
    g.proj = {"lat": [p.dram("proj_lat%d" % b, [D_IN, SEQ]).ap() for b in range(NB)],
              "ctx": [p.dram("proj_ctx%d" % b, [D_IN, CTX]).ap() for b in range(NB)]}
    g.ya = {s: p.dram("ya_" + s, [NB, 512, g.T[s]], BF16).ap() for s in ("lat", "ctx")}
    g.yb = {s: p.dram("yb_" + s, [NB, 1024, g.T[s]], BF16).ap() for s in ("lat", "ctx")}
    g.yc = {s: p.dram("yc_" + s, [NB, 512, g.T[s]], BF16).ap() for s in ("lat", "ctx")}
    g.ys5 = {s: p.dram("ys5_" + s, [NB, 512, g.T[s]]).ap() for s in ("lat", "ctx")}
    g.odn = {s: p.dram("odn_" + s, [2, NB, 1024, g.T[s]]).ap() for s in ("lat", "ctx")}
    g.e1bf = [p.dram("e1bf%d" % l_, [NE, D, 2 * D], BF16).ap() for l_ in range(DEPTH)]
    g.e2bf = [p.dram("e2bf%d" % l_, [NE, D, D], BF16).ap() for l_ in range(DEPTH)]

    g.cst = p.gsb("cst", [128, C_END])
    g.cstb = p.gsb("cstb", [128, 256], BF16)
    g.PS = [p.gpsum("ps%d" % i, [128, 1024]) for i in range(4)]
    g.psi = 0
    g.stl = p.gsb("st_lru", [128, NB, 4, 2])
    g.sts5 = p.gsb("st_s5", [128, NB, 2, 16, 2])
    g.hT2 = p.gsb("hT2", [128, 8, MT_], BF16)
    g.Wg = p.gsb("Wg", [128, MT_ // 128, NE])
    g.WgT = p.gsb("WgT", [NE, MT_ // 128, 128])

    g.dumps = set(x for x in dbg if x.startswith("@"))

    def dump(name, ap, dtype=F32):
        if "@" + name not in g.dumps:
            return
        shp = list(ap.shape)
        dst = nc.dram_tensor("dbg_" + name, [shp[0], int(np.prod(shp[1:]))], dtype, kind="ExternalOutput").ap()
        if len(shp) == 3:
            dst = dst.rearrange("p (a b) -> p a b", b=shp[2])
        p.dma("sp", dst, ap)
    g.dump = dump

    def ps():
        t = g.PS[g.psi % 4]
        g.psi += 1
        return t
    g.ps = ps
    g.epsc = p.gsb("epsc", [128, 1])
    p.memset(g.epsc[:, :], EPS)
    g.negpi = p.gsb("negpi", [128, 1])
    p.memset(g.negpi[:, :], -math.pi)
    g.ident = g.cst[:, C_ID:C_ID + 128]
    g.ones = g.cst[:, C_ONE:C_ONE + 128]

    p.dma("sp", g.cst[:, :], consts_in[:, :])
    p.copy(g.cstb[:, :], g.cst[:, 0:256])
    for b in range(NB):
        p.dma("sp", g.xs["lat"][b * SEQ:(b + 1) * SEQ, :], x_in[b * SEQ:(b + 1) * SEQ, :])
    p.dma("sp", g.xs["ctx"][:, :], ctx_in[:, :])
    if "moe" not in (stop_after or ()):
        pass
    g.cast_moe = lambda: None
    p.end_phase()

    def cast_moe(l):
        for e in range(NE):
            for cb in range(4):
                p.dma("pool", g.e1bf[l][e, :, cb * 512:(cb + 1) * 512], W["w_e1"][l, e, :, cb * 512:(cb + 1) * 512])
            for cb in range(2):
                p.dma("pool", g.e2bf[l][e, :, cb * 512:(cb + 1) * 512], W["w_e2"][l, e, :, cb * 512:(cb + 1) * 512])
    g.cast_moe = cast_moe

    for l in layers:
        last = (l == DEPTH - 1)
        phase_adaln(g, l, c_in, cctx_in)
        if stop_after == "adaln":
            break
        for b in range(NB):
            for s in ("ctx", "lat"):
                phase_norm_proj(g, l, b, s)
        if stop_after == "proj":
            break
        if "lru" not in g.skip:
            phase_lru(g, l, last)
        if stop_after == "lru":
            break
        if "s5" not in g.skip:
            phase_s5(g, l, last)
        if stop_after == "s5":
            break
        if "dn" not in g.skip:
            phase_dn(g, l, last)
        if stop_after == "dn":
            break
        phase_merge(g, l, last)
        if stop_after == "merge":
            break
        if "moe" not in g.skip:
            import os
            if not os.environ.get("MOE_NOCAST"):
                g.cast_moe(l)
            phase_moe(g, l, last)
        if stop_after == "moe":
            break
    if stop_after is None:
        phase_final(g)
    srcs = {"mod": g.mod, "proj_lat": g.proj["lat"][0], "proj_ctx": g.proj["ctx"][0], "xs_lat": g.xs["lat"],
            "xs_ctx": g.xs["ctx"]}
    for s_ in ("lat", "ctx"):
        for nm, dd in (("ya", g.ya), ("yb", g.yb), ("yc", g.yc), ("ys5", g.ys5), ("odn", g.odn)):
            srcs[nm + "_" + s_] = dd[s_]
    for name in dbg:
        if name.startswith("@"):
            continue
        src = srcs[name]
        n0 = src.shape[0]
        rest = int(np.prod(src.shape[1:]))
        dst = nc.dram_tensor("dbg_" + name, [n0, rest], src.dtype, kind="ExternalOutput").ap()
        for i in range(n0):
            p.dma("sp", dst[i:i + 1, :], dap(src, src.offset + i * rest, [[rest, 1], [1, rest]]))
    if stop_after is None:
        pass
    p.end_phase(final=True)
    return nc


def load_mod_bcast(g, l, v, idx, name):
    p = g.p
    t = p.sb(name, [128, D])
    src = g.mod[l, v, idx * D:(idx + 1) * D]
    p.dma("sp", t[:, :], dap(src, src.offset, [[0, 128], [1, D]]))
    return t


def phase_adaln(g, l, c_in, cctx_in):
    p, NB, NV, W = g.p, g.NB, g.NV, g.W
    sT = p.sb("ad_sT", [128, 8, NV])
    sTb = p.sb("ad_sTb", [128, 8, NV], BF16)
    for v in range(NB):
        p.dma("sp", sT[:, :, v], dap(c_in, v * D, [[1, 128], [128, 8]]), allow_slow_non_contiguous=True)
    p.dma("sp", sT[:, :, NB], dap(cctx_in, 0, [[1, 128], [128, 8]]), allow_slow_non_contiguous=True)
    p.act(sTb[:, :, :], sT[:, :, :], AF.Silu)
    bias = p.sb("ad_bias", [NV, 6 * D])
    src = W["b_ada"][l, :]
    p.dma("sp", bias[:, :], dap(src, src.offset, [[0, NV], [1, 6 * D]]))
    modsb = p.sb("ad_mod", [NV, 6 * D])
    wts = [p.sb("ad_w%d" % i, [128, 8, 512], BF16) for i in range(2)]
    for ct in range(12):
        wt = wts[ct % 2]
        src = W["w_ada"][l, :, ct * 512:(ct + 1) * 512]
        p.dma("pool", wt[:, :, :], dap(src, src.offset, [[6 * D, 128], [128 * 6 * D, 8], [1, 512]]))
        pt = g.ps()
        for kc in range(8):
            p.mm(pt[0:NV, 0:512], sTb[:, kc, :], wt[:, kc, :], start=(kc == 0), stop=(kc == 7))
        p.tt(modsb[:, ct * 512:(ct + 1) * 512], pt[0:NV, 0:512], bias[:, ct * 512:(ct + 1) * 512], ALU.add)
    p.dma("sp", g.mod[l, :, :], modsb[:, :])
    g.dump("sT", sT[:, :, :])
    g.dump("bias", bias[:, :])
    g.dump("modsb", modsb[:, :])
    p.end_phase()


def phase_norm_proj(g, l, b, s):
    p, NB, W = g.p, g.NB, g.W
    T = g.T[s]
    v = b if s == "lat" else NB
    xs = g.xs[s][b * T:(b + 1) * T, :]
    hT = p.sb("np_hT", [128, 8, T], BF16)
    G1 = load_mod_bcast(g, l, v, 1, "np_G1")
    SH = load_mod_bcast(g, l, v, 0, "np_SH")
    gm = p.sb("np_gm", [128, D])
    src = W["g_mix"][l, :]
    p.dma("sp", gm[:, :], dap(src, src.offset, [[0, 128], [1, D]]))
    p.stt(G1[:, :], G1[:, :], 1.0, gm[:, :], ALU.add, ALU.mult)
    norm_tiles(g, xs, T, G1, SH, hT, None)
    wts = [p.sb("np_w%d" % i, [128, 8, 512], BF16) for i in range(2)]
    stg = [p.sb("np_stg%d" % i, [128, 512]) for i in range(4)]
    si = 0
    ngrp = (D_IN + 511) // 512
    TT = min(T, 512)
    for og in range(ngrp):
        c0 = og * 512
        ncol = min(512, D_IN - c0)
        wt = wts[og % 2]
        src = W["w_in"][l, :, c0:c0 + ncol]
        p.dma("pool", wt[:, :, 0:ncol], dap(src, src.offset, [[D_IN, 128], [128 * D_IN, 8], [1, ncol]]))
        for oc in range((ncol + 127) // 128):
            m = min(128, ncol - oc * 128)
            for tt in range(T // TT):
                pt = g.ps()
                for kc in range(8):
                    p.mm(pt[0:m, 0:TT], wt[:, kc, oc * 128:oc * 128 + m], hT[:, kc, tt * TT:(tt + 1) * TT],
                         start=(kc == 0), stop=(kc == 7))
                sg = stg[si % 4]
                p.copy(sg[0:m, 0:TT], pt[0:m, 0:TT], eng=("act" if si % 2 else "dve"))
                si += 1
                r0 = c0 + oc * 128
                p.dma("sp", g.proj[s][b][r0:r0 + m, tt * TT:(tt + 1) * TT], sg[0:m, 0:TT])
    p.end_phase()


def norm_tiles(g, xs, T, G1, SH, hT, h32cb):
    p = g.p
    xts = [p.sb("nt_x%d" % i, [128, D]) for i in range(2)]
    sq = p.sb("nt_sq", [128, D])
    hN = [p.sb("nt_h%d" % i, [128, D]) for i in range(2)]
    ss = p.sb("nt_ss", [128, 4])
    h32 = [p.sb("nt_h32_%d" % i, [128, 8, 128]) for i in range(2)] if h32cb else None
    for tt in range(T // 128):
        xt = xts[tt % 2]
        hn = hN[tt % 2]
        p.dma("sp", xt[:, :], xs[tt * 128:(tt + 1) * 128, :])
        p.act(sq[:, :], xt[:, :], AF.Square)
        p.op("dve", lambda e, o=ss[:, 0:1], i=sq[:, :]: e.reduce_sum(out=o, in_=i, axis=AX.X), [sq[:, :]], [ss[:, 0:1]])
        p.ts(ss[:, 1:2], ss[:, 0:1], 1.0 / D, EPS, op0=ALU.mult, op1=ALU.add)
        p.act(ss[:, 2:3], ss[:, 1:2], AF.Sqrt)
        p.recip(ss[:, 3:4], ss[:, 2:3])
        p.stt(hn[:, :], xt[:, :], ss[:, 3:4], G1[:, :], ALU.mult, ALU.mult)
        p.tt(hn[:, :], hn[:, :], SH[:, :], ALU.add)
        for half in range(2):
            pt = g.ps()
            for k4 in range(4):
                kc = half * 4 + k4
                p.tr(pt[:, k4 * 128:(k4 + 1) * 128], hn[:, kc * 128:(kc + 1) * 128], g.ident)
            src = pt[:, 0:512].rearrange("p (a b) -> p a b", b=128)
            p.act(hT[:, half * 4:half * 4 + 4, tt * 128:(tt + 1) * 128], src, AF.Copy)
            if h32cb:
                p.copy(h32[tt % 2][:, half * 4:half * 4 + 4, :], src)
        if h32cb:
            h32cb(tt, h32[tt % 2])


def phase_lru(g, l, last):
    p, NB, W = g.p, g.NB, g.W
    Wbd = p.sb("lr_Wbd", [128, 2, 2, 4, 128])
    p.memset(Wbd[:, :, :, :, :], 0.0)
    for d in range(2):
        for gg in range(2):
            for hh in range(2):
                off = W["lru_w_gate"][l, d, gg, hh, 0, 0].offset if False else (((l * 2 + d) * 2 + gg) * 8 + hh) * 4096
                p.dma("sp", Wbd[hh * 64:(hh + 1) * 64, d, gg, :, hh * 64:(hh + 1) * 64],
                      dap(W["lru_w_gate"], off, [[64, 64], [2 * 4096, 4], [1, 64]]))
    lam = p.sb("lr_lam", [128, 2, 4])
    cv = p.sb("lr_cv", [128, 2, 4])
    bg = p.sb("lr_bg", [128, 2, 2, 4])
    cw = p.sb("lr_cw", [128, 4, 4])
    cb = p.sb("lr_cb", [128, 4])
    p.dma("sp", lam[:, :, :], dap(W["lru_lam"], l * 1024, [[1, 128], [512, 2], [128, 4]]), allow_slow_non_contiguous=True)
    for d in range(2):
        p.dma("sp", bg[:, d, :, :], dap(W["lru_b_gate"], (l * 2 + d) * 1024, [[1, 128], [512, 2], [128, 4]]),
              allow_slow_non_contiguous=True)
    p.dma("sp", cw[:, :, :], dap(W["lru_conv_w"], l * 2048, [[1, 128], [512, 4], [128, 4]]), allow_slow_non_contiguous=True)
    p.dma("sp", cb[:, :], dap(W["lru_conv_b"], l * 512, [[1, 128], [128, 4]]), allow_slow_non_contiguous=True)
    p.act(cv[:, :, :], lam[:, :, :], AF.Exp, scale=-1.0)
    p.act(cv[:, :, :], cv[:, :, :], AF.Ln, bias=1.0)
    p.ts(cv[:, :, :], cv[:, :, :], -8.0, None, op0=ALU.mult)
    TM = SEQ
    ax = p.sb("lr_ax", [128, TM])
    xc = p.sb("lr_xc", [128, TM])
    ay = p.sb("lr_ay", [128, TM])
    rt = p.sb("lr_r", [128, TM])
    it = p.sb("lr_i", [128, TM])
    at = p.sb("lr_a", [128, TM])
    a2 = p.sb("lr_a2", [128, TM])
    bt = p.sb("lr_b", [128, TM])
    hd = [p.sb("lr_h%d" % d, [128, TM]) for d in range(2)]
    yo = p.sb("lr_y", [128, TM], BF16)
    for b in range(NB):
        for s in ("ctx", "lat"):
            T = g.T[s]
            L = 64 if s == "lat" else T
            TT = min(T, 512)
            emit = not (last and s == "ctx")
            for ch in range(4):
                p.dma("sp", ax[:, 0:T], g.proj[s][b][OFF_AX + ch * 128:OFF_AX + (ch + 1) * 128, :])
                if emit:
                    p.dma("sp", ay[:, 0:T], g.proj[s][b][OFF_AY + ch * 128:OFF_AY + (ch + 1) * 128, :])
                a3 = ax[:, 0:T].rearrange("p (r l) -> p r l", l=L)
                x3 = xc[:, 0:T].rearrange("p (r l) -> p r l", l=L)
                p.ts(xc[:, 0:T], ax[:, 0:T], cw[:, 2, ch:ch + 1], cb[:, ch:ch + 1], op0=ALU.mult, op1=ALU.add)
                p.stt(x3[:, :, 2:L], a3[:, :, 0:L - 2], cw[:, 0, ch:ch + 1], x3[:, :, 2:L], ALU.mult, ALU.add)
                p.stt(x3[:, :, 1:L], a3[:, :, 0:L - 1], cw[:, 1, ch:ch + 1], x3[:, :, 1:L], ALU.mult, ALU.add)
                p.stt(x3[:, :, 0:L - 1], a3[:, :, 1:L], cw[:, 3, ch:ch + 1], x3[:, :, 0:L - 1], ALU.mult, ALU.add)
                for d in range(2):
                    for tt in range(T // TT):
                        sl = slice(tt * TT, (tt + 1) * TT)
                        pr = g.ps()
                        p.mm(pr[:, 0:TT], Wbd[:, d, 0, ch, :], xc[:, sl])
                        p.mm(pr[:, 512:512 + TT], Wbd[:, d, 1, ch, :], xc[:, sl])
                        p.act(rt[:, sl], pr[:, 0:TT], AF.Sigmoid, bias=bg[:, d, 0, ch:ch + 1])
                        p.act(it[:, sl], pr[:, 512:512 + TT], AF.Sigmoid, bias=bg[:, d, 1, ch:ch + 1])
                    p.act(at[:, 0:T], rt[:, 0:T], AF.Exp, scale=cv[:, d, ch:ch + 1])
                    p.tt(a2[:, 0:T], at[:, 0:T], at[:, 0:T], ALU.mult, eng="pool")
                    p.act(a2[:, 0:T], a2[:, 0:T], AF.Sqrt, scale=-1.0, bias=1.0)
                    p.tt(bt[:, 0:T], it[:, 0:T], xc[:, 0:T], ALU.mult)
                    p.tt(bt[:, 0:T], bt[:, 0:T], a2[:, 0:T], ALU.mult)
                    init = 0.0 if s == "ctx" else g.stl[:, b, ch, d:d + 1]
                    h = hd[d]
                    if d == 0:
                        p.scan(h[:, 0:T], at[:, 0:T], bt[:, 0:T], init)
                    else:
                        p.scan(rev(h[:, 0:T]), rev(at[:, 0:T]), rev(bt[:, 0:T]), init)
                    if s == "ctx":
                        col = T - 1 if d == 0 else 0
                        p.copy(g.stl[:, b, ch, d:d + 1], h[:, col:col + 1])
                if emit:
                    p.act(ay[:, 0:T], ay[:, 0:T], AF.Gelu_apprx_tanh)
                    p.tt(hd[0][:, 0:T], hd[0][:, 0:T], hd[1][:, 0:T], ALU.add)
                    p.tt(yo[:, 0:T], hd[0][:, 0:T], ay[:, 0:T], ALU.mult)
                    p.dma("sp", g.ya[s][b, ch * 128:(ch + 1) * 128, :], yo[:, 0:T])
    p.end_phase()


def ins(ap, axis, n):
    a = [list(x) for x in ap.ap]
    a.insert(axis, [0, n])
    return bass.AP(ap.tensor, ap.offset, a)


PI = math.pi


def phase_s5(g, l, last):
    p, NB, W = g.p, g.NB, g.W
    TA = CTX + SEQ
    seqs = (("ctx", 0, CTX), ("lat", CTX, SEQ))
    iota = g.cst[:, C_IOTA:C_IOTA + SEQ]
    M = p.sb("s5_M", [128, 4, 8])
    p.memset(M[:, :, :], 0.0)
    for rr in range(4):
        p.memset(M[0:64, rr, 2 * rr:2 * rr + 1], 1.0)
        p.memset(M[64:128, rr, 2 * rr + 1:2 * rr + 2], 1.0)
    prm = []
    for d in range(2):
        t = {}
        for nm in ("lre", "lim", "dt", "mag", "th", "cth", "sth", "ar", "ai", "fr", "fi", "t0", "t1", "t2"):
            t[nm] = p.sb("s5_%s%d" % (nm, d), [128, 16])
        base = (l * 2 + d) * 32 * 64
        for gg in range(2):
            p.dma("sp", t["lre"][gg * 64:(gg + 1) * 64, :], dap(W["s5_lam_re"], base + gg * 64, [[1, 64], [128, 16]]),
                  allow_slow_non_contiguous=True)
            p.dma("sp", t["lim"][gg * 64:(gg + 1) * 64, :], dap(W["s5_lam_im"], base + gg * 64, [[1, 64], [128, 16]]),
                  allow_slow_non_contiguous=True)
            p.dma("sp", t["dt"][gg * 64:(gg + 1) * 64, :], dap(W["s5_log_dt"], (l * 2 + d) * 32 + gg, [[0, 64], [2, 16]]),
                  allow_slow_non_contiguous=True)
        p.act(t["dt"][:, :], t["dt"][:, :], AF.Exp)
        p.tt(t["t0"][:, :], t["lre"][:, :], t["dt"][:, :], ALU.mult)
        p.act(t["mag"][:, :], t["t0"][:, :], AF.Exp)
        p.tt(t["th"][:, :], t["lim"][:, :], t["dt"][:, :], ALU.mult)
        ki = p.sb("s5_ki%d" % d, [128, 16], mybir.dt.int32)
        p.ts(t["t0"][:, :], t["th"][:, :], 1.0 / (2 * PI), None, op0=ALU.mult)
        p.copy(ki[:, :], t["t0"][:, :])
        p.copy(t["t1"][:, :], ki[:, :])
        p.stt(t["t0"][:, :], t["t1"][:, :], -2 * PI, t["th"][:, :], ALU.mult, ALU.add)
        p.ts(t["t1"][:, :], t["t0"][:, :], PI, None, op0=ALU.is_gt)
        p.stt(t["t0"][:, :], t["t1"][:, :], -2 * PI, t["t0"][:, :], ALU.mult, ALU.add)
        p.ts(t["t1"][:, :], t["t0"][:, :], -PI, None, op0=ALU.is_lt)
        p.stt(t["t0"][:, :], t["t1"][:, :], 2 * PI, t["t0"][:, :], ALU.mult, ALU.add)
        p.act(t["sth"][:, :], t["t0"][:, :], AF.Sin)
        p.ts(t["t0"][:, :], t["t0"][:, :], 0.5 * PI, None, op0=ALU.add)
        p.ts(t["t1"][:, :], t["t0"][:, :], PI, None, op0=ALU.is_gt)
        p.stt(t["t0"][:, :], t["t1"][:, :], -2 * PI, t["t0"][:, :], ALU.mult, ALU.add)
        p.act(t["cth"][:, :], t["t0"][:, :], AF.Sin)
        p.tt(t["ar"][:, :], t["mag"][:, :], t["cth"][:, :], ALU.mult)
        p.tt(t["ai"][:, :], t["mag"][:, :], t["sth"][:, :], ALU.mult)
        p.tt(t["t0"][:, :], t["lre"][:, :], t["lre"][:, :], ALU.mult)
        p.tt(t["t1"][:, :], t["lim"][:, :], t["lim"][:, :], ALU.mult)
        p.tt(t["t0"][:, :], t["t0"][:, :], t["t1"][:, :], ALU.add)
        p.recip(t["t0"][:, :], t["t0"][:, :])
        p.ts(t["t1"][:, :], t["ar"][:, :], -1.0, None, op0=ALU.add)
        p.tt(t["fr"][:, :], t["t1"][:, :], t["lre"][:, :], ALU.mult)
        p.tt(t["t2"][:, :], t["ai"][:, :], t["lim"][:, :], ALU.mult)
        p.tt(t["fr"][:, :], t["fr"][:, :], t["t2"][:, :], ALU.add)
        p.tt(t["fr"][:, :], t["fr"][:, :], t["t0"][:, :], ALU.mult)
        p.tt(t["fi"][:, :], t["ai"][:, :], t["lre"][:, :], ALU.mult)
        p.tt(t["t2"][:, :], t["t1"][:, :], t["lim"][:, :], ALU.mult)
        p.tt(t["fi"][:, :], t["fi"][:, :], t["t2"][:, :], ALU.subtract)
        p.tt(t["fi"][:, :], t["fi"][:, :], t["t0"][:, :], ALU.mult)
        prm.append(t)
    dsk = p.sb("s5_dsk", [128, 4])
    p.dma("sp", dsk[:, :], dap(W["s5_d"], l * 512, [[1, 128], [128, 4]]), allow_slow_non_contiguous=True)
    uc = p.sb("s5_u", [128, NB, TA])
    yacc = p.sb("s5_y", [128, NB, TA])
    cosT = p.sb("s5_cos", [128, SEQ])
    sinT = p.sb("s5_sin", [128, SEQ])
    tmp = p.sb("s5_tmp", [128, SEQ])
    xr = p.sb("s5_xr", [128, SEQ])
    xi = p.sb("s5_xi", [128, SEQ])
    wr = p.sb("s5_wr", [128, SEQ])
    wi = p.sb("s5_wi", [128, SEQ])
    t3 = p.sb("s5_t3", [128, SEQ])
    t4 = p.sb("s5_t4", [128, SEQ])
    braw = [p.sb("s5_braw%d" % i, [128, 4, 16]) for i in range(2)]
    craw = [p.sb("s5_craw%d" % i, [128, 4, 16]) for i in range(2)]
    bbs = [p.sb("s5_bbs%d" % i, [128, 4, 16]) for i in range(2)]
    tb = p.sb("s5_tb", [128, 4, 16])
    E = p.sb("s5_E", [128, 8, 16])
    BbT = p.sb("s5_BbT", [128, 2, 4, 2, 128])
    CT = p.sb("s5_CT", [128, 2, 4, 2, 8, 16])
    ini = p.sb("s5_ini", [128, 4])
    cs2 = p.sb("s5_cs2", [128, 8])
    for c in range(4):
        for d in range(2):
            t = prm[d]
            base = (l * 2 + d) * 32 * 1024
            for gg in range(2):
                for r4 in range(4):
                    for (dst, nm) in ((braw[0], "s5_b_re"), (braw[1], "s5_b_im")):
                        p.dma("sp", dst[gg * 64:(gg + 1) * 64, r4, :],
                              dap(W[nm], base + (8 * c + 2 * r4 + gg) * 1024, [[16, 64], [1, 16]]))
                    for (dst, nm) in ((craw[0], "s5_c_re"), (craw[1], "s5_c_im")):
                        p.dma("sp", dst[gg * 64:(gg + 1) * 64, r4, :],
                              dap(W[nm], base + (8 * c + 2 * r4 + gg) * 1024, [[1, 64], [64, 16]]),
                              allow_slow_non_contiguous=True)
            frb = ins(t["fr"][:, 4 * c:4 * c + 4], 2, 16)
            fib = ins(t["fi"][:, 4 * c:4 * c + 4], 2, 16)
            p.tt(bbs[0][:, :, :], braw[0][:, :, :], frb, ALU.mult)
            p.tt(tb[:, :, :], braw[1][:, :, :], fib, ALU.mult)
            p.tt(bbs[0][:, :, :], bbs[0][:, :, :], tb[:, :, :], ALU.subtract)
            p.tt(bbs[1][:, :, :], braw[1][:, :, :], frb, ALU.mult)
            p.tt(tb[:, :, :], braw[0][:, :, :], fib, ALU.mult)
            p.tt(bbs[1][:, :, :], bbs[1][:, :, :], tb[:, :, :], ALU.add)
            for r4 in range(4):
                mk = ins(M[:, r4, :], 2, 16)
                for ri in range(2):
                    p.tt(E[:, :, :], ins(bbs[ri][:, r4, :], 1, 8), mk, ALU.mult)
                    pt = g.ps()
                    p.tr(pt[:, 0:128], E[:, :, :].rearrange("p a b -> p (a b)"), g.ident)
                    p.copy(BbT[:, d, r4, ri, :], pt[:, 0:128])
                p.tt(CT[:, d, r4, 0, :, :], ins(craw[0][:, r4, :], 1, 8), mk, ALU.mult)
                p.stt(CT[:, d, r4, 1, :, :], ins(craw[1][:, r4, :], 1, 8), -1.0, mk, ALU.mult, ALU.mult)
        for b in range(NB):
            for (s, o, T) in seqs:
                p.dma("sp", uc[:, b, o:o + T], g.proj[s][b][OFF_U + c * 128:OFF_U + (c + 1) * 128, :])
        p.ts(yacc[:, :, :], uc[:, :, :], dsk[:, c:c + 1], None, op0=ALU.mult)
        for d in range(2):
            t = prm[d]
            for r4 in range(4):
                r = 4 * c + r4
                p.memset(cosT[:, 0:1], 1.0)
                p.memset(sinT[:, 0:1], 0.0)
                p.copy(cs2[:, 0:1], t["cth"][:, r:r + 1])
                p.copy(cs2[:, 1:2], t["sth"][:, r:r + 1])
                n_ = 1
                while n_ < SEQ:
                    p.ts(cs2[:, 2:3], cs2[:, 1:2], -1.0, None, op0=ALU.mult)
                    p.ts(cosT[:, n_:2 * n_], cosT[:, 0:n_], cs2[:, 0:1], None, op0=ALU.mult)
                    p.stt(cosT[:, n_:2 * n_], sinT[:, 0:n_], cs2[:, 2:3], cosT[:, n_:2 * n_], ALU.mult, ALU.add)
                    p.ts(sinT[:, n_:2 * n_], sinT[:, 0:n_], cs2[:, 0:1], None, op0=ALU.mult)
                    p.stt(sinT[:, n_:2 * n_], cosT[:, 0:n_], cs2[:, 1:2], sinT[:, n_:2 * n_], ALU.mult, ALU.add)
                    n_ *= 2
                    if n_ < SEQ:
                        p.tt(cs2[:, 3:4], cs2[:, 0:1], cs2[:, 1:2], ALU.mult)
                        p.tt(cs2[:, 4:5], cs2[:, 0:1], cs2[:, 0:1], ALU.mult)
                        p.tt(cs2[:, 5:6], cs2[:, 1:2], cs2[:, 1:2], ALU.mult)
                        p.tt(cs2[:, 0:1], cs2[:, 4:5], cs2[:, 5:6], ALU.subtract)
                        p.ts(cs2[:, 1:2], cs2[:, 3:4], 2.0, None, op0=ALU.mult)
                magb = ins(t["mag"][:, r:r + 1], 1, SEQ)
                for b in range(NB):
                    for (s, o, T) in seqs:
                        TT = min(T, 512)
                        fw = (d == 0)
                        cs = cosT[:, 0:T] if fw else rev(cosT[:, 0:T])
                        sn = sinT[:, 0:T] if fw else rev(sinT[:, 0:T])
                        for tt in range(T // TT):
                            sl = slice(tt * TT, (tt + 1) * TT)
                            pt = g.ps()
                            p.mm(pt[:, 0:TT], BbT[:, d, r4, 0, :], uc[:, b, o + tt * TT:o + (tt + 1) * TT])
                            p.mm(pt[:, 512:512 + TT], BbT[:, d, r4, 1, :], uc[:, b, o + tt * TT:o + (tt + 1) * TT])
                            p.act(xr[:, sl], pt[:, 0:TT], AF.Copy)
                            p.act(xi[:, sl], pt[:, 512:512 + TT], AF.Copy)
                        X, Y = xr[:, 0:T], xi[:, 0:T]
                        p.tt(wr[:, 0:T], X, cs, ALU.mult)
                        p.tt(t3[:, 0:T], Y, sn, ALU.mult, eng="pool")
                        p.tt(wr[:, 0:T], wr[:, 0:T], t3[:, 0:T], ALU.add)
                        p.tt(wi[:, 0:T], Y, cs, ALU.mult, eng="pool")
                        p.tt(t4[:, 0:T], X, sn, ALU.mult)
                        p.tt(wi[:, 0:T], wi[:, 0:T], t4[:, 0:T], ALU.subtract, eng="pool")
                        if s == "ctx":
                            ir, ii = 0.0, 0.0
                        else:
                            h0 = g.sts5[:, b, d, r, :]
                            p.tt(ini[:, 0:1], h0[:, 0:1], t["cth"][:, r:r + 1], ALU.mult)
                            p.tt(ini[:, 1:2], h0[:, 1:2], t["sth"][:, r:r + 1], ALU.mult)
                            p.tt(ini[:, 0:1], ini[:, 0:1], ini[:, 1:2], ALU.subtract)
                            p.tt(ini[:, 2:3], h0[:, 0:1], t["sth"][:, r:r + 1], ALU.mult)
                            p.tt(ini[:, 3:4], h0[:, 1:2], t["cth"][:, r:r + 1], ALU.mult)
                            p.tt(ini[:, 2:3], ini[:, 2:3], ini[:, 3:4], ALU.add)
                            ir, ii = ini[:, 0:1], ini[:, 2:3]
                        mb = ins(t["mag"][:, r:r + 1], 1, T)
                        mb = dap(t["mag"], t["mag"][:, r:r + 1].offset, [list(t["mag"][:, r:r + 1].ap[0]), [0, T]])
                        if fw:
                            p.scan(xr[:, 0:T], mb, wr[:, 0:T], ir)
                            p.scan(xi[:, 0:T], mb, wi[:, 0:T], ii)
                        else:
                            p.scan(rev(xr[:, 0:T]), mb, rev(wr[:, 0:T]), ir)
                            p.scan(rev(xi[:, 0:T]), mb, rev(wi[:, 0:T]), ii)
                        p.tt(wr[:, 0:T], X, cs, ALU.mult)
                        p.tt(t3[:, 0:T], Y, sn, ALU.mult, eng="pool")
                        p.tt(wr[:, 0:T], wr[:, 0:T], t3[:, 0:T], ALU.subtract)
                        p.tt(wi[:, 0:T], Y, cs, ALU.mult, eng="pool")
                        p.tt(t4[:, 0:T], X, sn, ALU.mult)
                        p.tt(wi[:, 0:T], wi[:, 0:T], t4[:, 0:T], ALU.add, eng="pool")
                        if s == "ctx":
                            col = T - 1 if fw else 0
                            p.copy(g.sts5[:, b, d, r, 0:1], wr[:, col:col + 1])
                            p.copy(g.sts5[:, b, d, r, 1:2], wi[:, col:col + 1])
                        if last and s == "ctx":
                            continue
                        for tt in range(T // TT):
                            sl = slice(tt * TT, (tt + 1) * TT)
                            pt = g.ps()
                            p.mm(pt[:, 0:TT], CT[:, d, r4, 0, :, :].rearrange("p a b -> p (a b)"), wr[:, sl],
                                 start=True, stop=False)
                            p.mm(pt[:, 0:TT], CT[:, d, r4, 1, :, :].rearrange("p a b -> p (a b)"), wi[:, sl],
                                 start=False, stop=True)
                            ya = yacc[:, b, o + tt * TT:o + (tt + 1) * TT]
                            p.tt(ya, ya, pt[:, 0:TT], ALU.add)
        p.act(yacc[:, :, :], yacc[:, :, :], AF.Gelu_apprx_tanh)
        for b in range(NB):
            for (s, o, T) in seqs:
                if last and s == "ctx":
                    continue
                p.dma("sp", g.ys5[s][b, c * 128:(c + 1) * 128, :], yacc[:, b, o:o + T])
    p.end_phase()
    wg = p.sb("s5_wg", [128, 4, 512], BF16)
    p.dma("pool", wg[:, :, :], dap(W["s5_w_glu"], l * 512 * 512, [[512, 128], [128 * 512, 4], [1, 512]]))
    bgl = p.sb("s5_bgl", [128, 4])
    p.dma("sp", bgl[:, :], dap(W["s5_b_glu"], l * 512, [[1, 128], [128, 4]]), allow_slow_non_contiguous=True)
    yg = [p.sb("s5_yg%d" % i, [128, 4, 512]) for i in range(2)]
    ygb = [p.sb("s5_ygb%d" % i, [128, 4, 512], BF16) for i in range(2)]
    sg = [p.sb("s5_sg%d" % i, [128, 512]) for i in range(2)]
    yo = [p.sb("s5_yo%d" % i, [128, 512], BF16) for i in range(2)]
    it = 0
    for b in range(NB):
        for (s, o, T) in seqs:
            if last and s == "ctx":
                continue
            TT = min(T, 512)
            for tt in range(T // TT):
                y_, yb_ = yg[it % 2], ygb[it % 2]
                it += 1
                src = g.ys5[s][b, :, tt * TT:(tt + 1) * TT]
                p.dma("sp", y_[:, :, 0:TT], dap(src, src.offset, [[T, 128], [128 * T, 4], [1, TT]]))
                p.copy(yb_[:, :, 0:TT], y_[:, :, 0:TT], eng="pool")
                for oc in range(4):
                    pt = g.ps()
                    for kc in range(4):
                        p.mm(pt[:, 0:TT], wg[:, kc, oc * 128:(oc + 1) * 128], yb_[:, kc, 0:TT], start=(kc == 0),
                             stop=(kc == 3))
                    s_, o_ = sg[oc % 2], yo[oc % 2]
                    p.act(s_[:, 0:TT], pt[:, 0:TT], AF.Sigmoid, bias=bgl[:, oc:oc + 1])
                    p.tt(o_[:, 0:TT], y_[:, oc, 0:TT], s_[:, 0:TT], ALU.mult)
                    p.dma("sp", g.yc[s][b, oc * 128:(oc + 1) * 128, tt * TT:(tt + 1) * TT], o_[:, 0:TT])
    p.end_phase()


def phase_dn(g, l, last):
    p, NB, W = g.p, g.NB, g.W
    seqs = (("ctx", CTX), ("lat", SEQ))
    ident64 = g.cst[0:64, C_ID:C_ID + 64]
    ones = g.ones
    cwd = p.sb("dn_cw", [128, 4, 24])
    p.dma("sp", cwd[:, :, :], dap(W["dn_conv_w"], l * 4 * 3072, [[1, 128], [3072, 4], [128, 24]]),
          allow_slow_non_contiguous=True)
    raw = [p.sb("dn_raw%d" % i, [128, SEQ]) for i in range(2)]
    xc = [p.sb("dn_xc%d" % i, [128, SEQ]) for i in range(2)]
    sq = p.sb("dn_sq", [128, SEQ])
    rn = p.sb("dn_rn", [128, SEQ])
    it = 0
    for b in range(NB):
        for (s, T) in seqs:
            L = 64 if s == "lat" else T
            TT = min(T, 512)
            for j in range(24):
                rw, x_ = raw[it % 2], xc[it % 2]
                it += 1
                rows = g.proj[s][b][OFF_Q + j * 128:OFF_Q + (j + 1) * 128, :]
                p.dma("sp", rw[:, 0:T], rows)
                a3 = rw[:, 0:T].rearrange("p (r l) -> p r l", l=L)
                x3 = x_[:, 0:T].rearrange("p (r l) -> p r l", l=L)
                p.ts(x_[:, 0:T], rw[:, 0:T], cwd[:, 2, j:j + 1], None, op0=ALU.mult)
                p.stt(x3[:, :, 2:L], a3[:, :, 0:L - 2], cwd[:, 0, j:j + 1], x3[:, :, 2:L], ALU.mult, ALU.add)
                p.stt(x3[:, :, 1:L], a3[:, :, 0:L - 1], cwd[:, 1, j:j + 1], x3[:, :, 1:L], ALU.mult, ALU.add)
                p.stt(x3[:, :, 0:L - 1], a3[:, :, 1:L], cwd[:, 3, j:j + 1], x3[:, :, 0:L - 1], ALU.mult, ALU.add)
                p.act(x_[:, 0:T], x_[:, 0:T], AF.Silu)
                if j < 16:
                    p.tt(sq[:, 0:T], x_[:, 0:T], x_[:, 0:T], ALU.mult, eng="pool")
                    for tt in range(T // TT):
                        sl = slice(tt * TT, (tt + 1) * TT)
                        pt = g.ps()
                        p.mm(pt[:, 0:TT], ones, sq[:, sl])
                        p.act(rn[:, sl], pt[:, 0:TT], AF.Sqrt, bias=g.epsc[:, 0:1])
                    p.recip(rn[:, 0:T], rn[:, 0:T])
                    sc = (128.0 ** -0.5) if j < 8 else 1.0
                    p.stt(x_[:, 0:T], x_[:, 0:T], sc, rn[:, 0:T], ALU.mult, ALU.mult)
                p.dma("sp", rows, x_[:, 0:T])
    p.end_phase()
    CB = 4
    NCH = SEQ // 64
    alg = p.sb("dn_alg", [64, 16])
    dtb = p.sb("dn_dtb", [64, 16])
    p.dma("sp", alg[:, :], dap(W["dn_a_log"], l * 16, [[0, 64], [1, 16]]))
    p.dma("sp", dtb[:, :], dap(W["dn_dt_bias"], l * 16, [[0, 64], [1, 16]]))
    p.act(alg[:, :], alg[:, :], AF.Exp)
    p.ts(alg[:, :], alg[:, :], -1.0, None, op0=ALU.mult)
    bt = p.sb("dn_bt", [64, NCH, 32])
    bet = p.sb("dn_bet", [64, NCH, 16])
    gt = p.sb("dn_gt", [64, NCH, 16])
    qkv = [[p.sb("dn_%s%d" % (nm, i), [128, 8, CB * 64]) for nm in "qkv"] for i in range(2)]
    S8 = p.sb("dn_S", [128, 8, 128])
    stdn = p.sb("dn_st", [128, 2, 8, 128])
    sm = p.sb("dn_sm", [64, 4, 8])
    gtot = p.sb("dn_gtot", [128, 8])
    gL = p.sb("dn_gL", [64, 8, 64])
    X = p.sb("dn_X", [64, 8, 64])
    D8 = p.sb("dn_D8", [64, 8, 64])
    DT8 = p.sb("dn_DT8", [64, 8, 64])
    P1 = p.sb("dn_P1", [64, 8, 64])
    P2 = p.sb("dn_P2", [64, 8, 64])
    bD = p.sb("dn_bD", [64, 8, 64])
    AtT = p.sb("dn_AtT", [64, 8, 64], BF16)
    Nk = [p.sb("dn_N%d" % i, [64, 8, 64], BF16) for i in range(2)]
    YR = [p.sb("dn_YR%d" % i, [64, 8, 2, 64], BF16) for i in range(2)]
    Vb8 = p.sb("dn_Vb", [64, 8, 128], BF16)
    Kbg8 = p.sb("dn_Kbg", [64, 8, 128], BF16)
    Kd8 = p.sb("dn_Kd", [64, 8, 128], BF16)
    U8 = p.sb("dn_U", [64, 8, 128])
    Vn8 = p.sb("dn_Vn", [64, 8, 128], BF16)
    WT8 = p.sb("dn_WT", [128, 8, 64], BF16)
    Qd8 = p.sb("dn_Qd", [128, 8, 64], BF16)
    S8b = p.sb("dn_Sb", [128, 8, 128], BF16)
    oc = [p.sb("dn_oc%d" % i, [128, 8, 64]) for i in range(2)]
    f3 = lambda ap: ap.rearrange("p a b -> p (a b)")
    for b in range(NB):
        for (s, T) in seqs:
            nch = T // 64
            src = g.proj[s][b][OFF_BETA, :]
            for n_ in range(nch):
                p.dma("sp", bt[:, n_, :], dap(src, src.offset + n_ * 64, [[1, 64], [T, 32]]), allow_slow_non_contiguous=True)
            p.act(bet[:, 0:nch, :], bt[:, 0:nch, 0:16], AF.Sigmoid)
            p.tt(gt[:, 0:nch, :], bt[:, 0:nch, 16:32], ins(dtb[:, :], 1, nch), ALU.add)
            p.act(gt[:, 0:nch, :], gt[:, 0:nch, :], AF.Exp)
            p.act(gt[:, 0:nch, :], gt[:, 0:nch, :], AF.Ln, bias=1.0)
            p.tt(gt[:, 0:nch, :], gt[:, 0:nch, :], ins(alg[:, :], 1, nch), ALU.mult)
            for d in range(2):
                fw = (d == 0)
                LTc = g.cst[0:64, C_LTF:C_LTF + 64] if fw else g.cst[0:64, C_LTB:C_LTB + 64]
                Mi = g.cst[0:64, C_MGT:C_MGT + 64] if fw else g.cst[0:64, C_MLT:C_MLT + 64]
                MT = g.cst[0:64, C_MLT:C_MLT + 64] if fw else g.cst[0:64, C_MGT:C_MGT + 64]
                Si = g.cst[0:64, C_SLT:C_SLT + 64] if fw else g.cst[0:64, C_SGT:C_SGT + 64]
                ST = g.cst[0:64, C_SGT:C_SGT + 64] if fw else g.cst[0:64, C_SLT:C_SLT + 64]
                if s == "ctx":
                    p.memset(S8[:, :, :], 0.0)
                else:
                    p.copy(S8[:, :, :], stdn[:, d, :, :])
                p.copy(S8b[:, :, :], S8[:, :, :], eng="act")
                order = list(range(nch)) if fw else list(range(nch - 1, -1, -1))
                cur_blk = None
                for ci, n in enumerate(order):
                    blk = n // CB
                    if blk != cur_blk:
                        cur_blk = blk
                        qb = qkv[(ci // CB) % 2]
                        nb_ = min(CB, nch - blk * CB)
                        for qi, off in enumerate((OFF_Q, OFF_K, OFF_V)):
                            sr = g.proj[s][b][off, blk * CB * 64]
                            p.dma("sp", qb[qi][:, :, 0:nb_ * 64],
                                  dap(sr, sr.offset, [[T, 128], [128 * T, 8], [1, nb_ * 64]]))
                    c0 = (n - blk * CB) * 64
                    qT, kT, vT = (qb[i][:, :, c0:c0 + 64] for i in range(3))
                    g8 = gt[:, n, d * 8:(d + 1) * 8]
                    be8 = bet[:, n, d * 8:(d + 1) * 8]
                    pt = g.ps()
                    p.mm(pt[0:64, 0:8], LTc, g8)
                    p.mm(pt[0:64, 8:16], ones[0:64, 0:64], g8)
                    p.mm(pt[:, 16:24], ones[0:64, :], g8)
                    Gc, Gam, Kdsc, bg = sm[:, 0, :], sm[:, 1, :], sm[:, 2, :], sm[:, 3, :]
                    p.copy(Gc, pt[0:64, 0:8])
                    p.act(Gam, pt[0:64, 0:8], AF.Exp)
                    p.tt(Kdsc, pt[0:64, 8:16], Gc, ALU.subtract)
                    p.act(Kdsc, Kdsc, AF.Exp)
                    p.act(gtot[:, :], pt[:, 16:24], AF.Exp)
                    p.tt(bg, be8, Gam, ALU.mult)
                    p.tt(gL[:, :, :], ins(g8, 2, 64), ins(LTc, 1, 8), ALU.mult)
                    pG = g.ps()
                    p.mm(pG[0:64, 0:512], ones[0:64, 0:64], f3(gL[:, :, :]))
                    p.tt(X[:, :, :], ins(Gc, 2, 64), pG[0:64, 0:512].rearrange("p (a b) -> p a b", b=64), ALU.subtract)
                    p.tt(D8[:, :, :], X[:, :, :], ins(Mi, 1, 8), ALU.subtract)
                    p.act(D8[:, :, :], D8[:, :, :], AF.Exp)
                    p.stt(DT8[:, :, :], X[:, :, :], -1.0, ins(MT, 1, 8), ALU.mult, ALU.subtract)
                    p.act(DT8[:, :, :], DT8[:, :, :], AF.Exp)
                    pK = g.ps()
                    for h in range(8):
                        p.mm(pK[0:64, h * 64:(h + 1) * 64], kT[:, h, :], kT[:, h, :])
                        p.mm(pK[0:64, 512 + h * 64:512 + (h + 1) * 64], kT[:, h, :], qT[:, h, :])
                    pKK = pK[0:64, 0:512].rearrange("p (a b) -> p a b", b=64)
                    pKQ = pK[0:64, 512:1024].rearrange("p (a b) -> p a b", b=64)
                    p.tt(P1[:, :, :], D8[:, :, :], ins(Si, 1, 8), ALU.mult)
                    p.tt(P1[:, :, :], P1[:, :, :], ins(be8, 2, 64), ALU.mult)
                    p.stt(Nk[0][:, :, :], pKK, -1.0, P1[:, :, :], ALU.mult, ALU.mult)
                    p.tt(bD[:, :, :], ins(be8, 2, 64), ins(ident64, 1, 8), ALU.mult)
                    pB = g.ps()
                    p.mm(pB[0:64, 0:512], ones[0:64, 0:64], f3(bD[:, :, :]))
                    p.tt(P2[:, :, :], DT8[:, :, :], ins(ST, 1, 8), ALU.mult)
                    p.tt(P2[:, :, :], P2[:, :, :], pB[0:64, 0:512].rearrange("p (a b) -> p a b", b=64), ALU.mult)
                    p.stt(YR[0][:, :, 0, :], pKK, -1.0, P2[:, :, :], ALU.mult, ALU.mult)
                    p.copy(YR[0][:, :, 1, :], ins(ident64, 1, 8))
                    p.tt(AtT[:, :, :], pKQ, DT8[:, :, :], ALU.mult)
                    for k in range(1, 7):
                        a_, b_ = (k - 1) % 2, k % 2
                        pA = g.ps()
                        for h in range(8):
                            p.mm(pA[0:64, h * 128:(h + 1) * 128], Nk[a_][:, h, :],
                                 YR[a_][:, h, :, :].rearrange("p a b -> p (a b)"))
                        pA4 = pA[0:64, :].rearrange("p (h t c) -> p h t c", t=2, c=64)
                        if k <= 5:
                            pN = g.ps()
                            for h in range(8):
                                p.mm(pN[0:64, h * 64:(h + 1) * 64], YR[a_][:, h, 0, :], Nk[a_][:, h, :])
                            p.act(YR[b_][:, :, 0, :], pA4[:, :, 0, :], AF.Copy)
                        p.tt(YR[b_][:, :, 1, :], pA4[:, :, 1, :], YR[a_][:, :, 1, :], ALU.add)
                        if k <= 5:
                            p.act(Nk[b_][:, :, :], pN[0:64, 0:512].rearrange("p (a b) -> p a b", b=64), AF.Copy)
                    R = YR[0][:, :, 1, :]
                    pTk = g.ps()
                    pTv = g.ps()
                    for h in range(8):
                        p.tr(pTk[0:64, h * 128:(h + 1) * 128], kT[:, h, :], g.ident)
                        p.tr(pTv[0:64, h * 128:(h + 1) * 128], vT[:, h, :], g.ident)
                    pTk3 = pTk[0:64, :].rearrange("p (a b) -> p a b", b=128)
                    pTv3 = pTv[0:64, :].rearrange("p (a b) -> p a b", b=128)
                    p.tt(Vb8[:, :, :], pTv3, ins(be8, 2, 128), ALU.mult)
                    p.tt(Kbg8[:, :, :], pTk3, ins(bg, 2, 128), ALU.mult)
                    p.tt(Kd8[:, :, :], pTk3, ins(Kdsc, 2, 128), ALU.mult)
                    pU = g.ps()
                    pW = g.ps()
                    for h in range(8):
                        p.mm(pU[0:64, h * 128:(h + 1) * 128], R[:, h, :], Vb8[:, h, :])
                        p.mm(pW[:, h * 64:(h + 1) * 64], Kbg8[:, h, :], R[:, h, :])
                    p.act(f3(U8[:, :, :]), pU[0:64, :], AF.Copy)
                    p.act(f3(WT8[:, :, :]), pW[:, 0:512], AF.Copy)
                    p.tt(bD[:, :, :], ins(Gam, 2, 64), ins(ident64, 1, 8), ALU.mult)
                    pGm = g.ps()
                    p.mm(pGm[:, 0:512], ones[0:64, :], f3(bD[:, :, :]))
                    p.tt(Qd8[:, :, :], qT, pGm[:, 0:512].rearrange("p (a b) -> p a b", b=64), ALU.mult)
                    pWS = g.ps()
                    for h in range(8):
                        p.mm(pWS[0:64, h * 128:(h + 1) * 128], WT8[:, h, :], S8b[:, h, :])
                    p.tt(f3(Vn8[:, :, :]), f3(U8[:, :, :]), pWS[0:64, :], ALU.subtract)
                    pO = g.ps()
                    for h in range(8):
                        p.mm(pO[:, h * 64:(h + 1) * 64], S8b[:, h, :], Qd8[:, h, :], start=True, stop=False)
                        p.mm(pO[:, h * 64:(h + 1) * 64], Vn8[:, h, :], AtT[:, h, :], start=False, stop=True)
                    if not (last and s == "ctx"):
                        o_ = oc[ci % 2]
                        p.act(f3(o_[:, :, :]), pO[:, 0:512], AF.Copy)
                        dst = g.odn[s][d, b, 0, n * 64]
                        p.dma("sp", dap(dst, dst.offset, [[T, 128], [128 * T, 8], [1, 64]]), o_[:, :, :])
                    pdS = g.ps()
                    for h in range(8):
                        p.mm(pdS[:, h * 128:(h + 1) * 128], Kd8[:, h, :], Vn8[:, h, :])
                    p.tt(S8[:, :, :], S8[:, :, :], ins(gtot[:, :], 2, 128), ALU.mult)
                    p.tt(f3(S8[:, :, :]), f3(S8[:, :, :]), pdS[:, :], ALU.add)
                    p.copy(S8b[:, :, :], S8[:, :, :], eng="act")
                if s == "ctx":
                    p.copy(stdn[:, d, :, :], S8[:, :, :])
    p.end_phase()
    ng = p.sb("dn_ng", [128, 1])
    p.dma("sp", ng[:, :], dap(W["dn_norm_g"], l * 128, [[1, 128], [1, 1]]))
    of = [p.sb("dn_of%d" % i, [128, SEQ]) for i in range(2)]
    ob = [p.sb("dn_ob%d" % i, [128, SEQ]) for i in range(2)]
    zt = [p.sb("dn_z%d" % i, [128, SEQ]) for i in range(2)]
    yo = [p.sb("dn_yo%d" % i, [128, SEQ], BF16) for i in range(2)]
    rs = p.sb("dn_rs", [128, SEQ])
    it = 0
    for b in range(NB):
        for (s, T) in seqs:
            if last and s == "ctx":
                continue
            TT = min(T, 512)
            for h in range(8):
                o1, o2, z_, y_ = of[it % 2], ob[it % 2], zt[it % 2], yo[it % 2]
                it += 1
                p.dma("sp", o1[:, 0:T], g.odn[s][0, b, h * 128:(h + 1) * 128, :])
                p.dma("sp", o2[:, 0:T], g.odn[s][1, b, h * 128:(h + 1) * 128, :])
                p.dma("sp", z_[:, 0:T], g.proj[s][b][OFF_Z + h * 128:OFF_Z + (h + 1) * 128, :])
                p.tt(o1[:, 0:T], o1[:, 0:T], o2[:, 0:T], ALU.add)
                p.tt(o2[:, 0:T], o1[:, 0:T], o1[:, 0:T], ALU.mult, eng="pool")
                for tt in range(T // TT):
                    sl = slice(tt * TT, (tt + 1) * TT)
                    pt = g.ps()
                    p.mm(pt[:, 0:TT], ones, o2[:, sl])
                    p.act(rs[:, sl], pt[:, 0:TT], AF.Sqrt, scale=1.0 / 128, bias=g.epsc[:, 0:1])
                p.recip(rs[:, 0:T], rs[:, 0:T])
                p.act(z_[:, 0:T], z_[:, 0:T], AF.Silu)
                p.stt(o1[:, 0:T], o1[:, 0:T], ng[:, 0:1], rs[:, 0:T], ALU.mult, ALU.mult)
                p.tt(y_[:, 0:T], o1[:, 0:T], z_[:, 0:T], ALU.mult)
                p.dma("sp", g.yb[s][b, h * 128:(h + 1) * 128, :], y_[:, 0:T])
    p.end_phase()


def phase_merge(g, l, last):
    p, NB, W = g.p, g.NB, g.W
    wa = p.sb("mg_wa", [128, 4, D], BF16)
    wb = p.sb("mg_wb", [128, 8, D], BF16)
    wc = p.sb("mg_wc", [128, 4, D], BF16)
    wo = p.sb("mg_wo", [128, 8, D], BF16)
    import os
    MGS = int(os.environ.get("MG_STOP", "9"))
    for hh in range(2):
        cs = slice(hh * 512, (hh + 1) * 512)
        p.dma("pool", wa[:, :, cs], dap(W["w_br_a"], l * 512 * D + hh * 512, [[D, 128], [128 * D, 4], [1, 512]]))
        p.dma("pool", wb[:, :, cs], dap(W["w_br_b"], l * D * D + hh * 512, [[D, 128], [128 * D, 8], [1, 512]]))
        p.dma("pool", wc[:, :, cs], dap(W["w_br_c"], l * 512 * D + hh * 512, [[D, 128], [128 * D, 4], [1, 512]]))
        p.dma("pool", wo[:, :, cs], dap(W["w_out"], l * D * D + hh * 512, [[D, 128], [128 * D, 8], [1, 512]]))
    if MGS == 1:
        p.end_phase()
        return
    yat = p.sb("mg_ya", [128, 4, 512], BF16)
    ybt = p.sb("mg_yb", [128, 8, 512], BF16)
    yct = p.sb("mg_yc", [128, 4, 512], BF16)
    g3 = [p.sb("mg_g%d" % i, [128, 3, 512]) for i in range(2)]
    m32 = p.sb("mg_m", [128, 512])
    t32 = p.sb("mg_t", [128, 512])
    mT = p.sb("mg_mT", [128, 8, 512], BF16)
    xt = [p.sb("mg_x%d" % i, [128, D]) for i in range(2)]
    tx = p.sb("mg_tx", [128, 512])
    xi = 0
    for b in range(NB):
        for s in ("ctx", "lat"):
            if last and s == "ctx":
                continue
            T = g.T[s]
            v = b if s == "lat" else NB
            GM = load_mod_bcast(g, l, v, 2, "mg_GM")
            TT = min(T, 512)
            for tt in range(T // TT):
                c0 = tt * TT
                for (dst, srcd, nk) in ((yat, g.ya, 4), (ybt, g.yb, 8), (yct, g.yc, 4)):
                    sr = srcd[s][b, 0, c0]
                    p.dma("sp", dst[:, :, 0:TT], dap(sr, sr.offset, [[T, 128], [128 * T, nk], [1, TT]]))
                for oc in range(8):
                    gt_ = g3[oc % 2]
                    for br in range(3):
                        r0 = OFF_GATE + br * D + oc * 128
                        p.dma("sp", gt_[:, br, 0:TT], g.proj[s][b][r0:r0 + 128, c0:c0 + TT])
                    p.act(gt_[:, :, 0:TT], gt_[:, :, 0:TT], AF.Sigmoid)
                    for br, (wt, yt, nk) in enumerate(((wa, yat, 4), (wb, ybt, 8), (wc, yct, 4))):
                        pt = g.ps()
                        for kc in range(nk):
                            p.mm(pt[:, 0:TT], wt[:, kc, oc * 128:(oc + 1) * 128], yt[:, kc, 0:TT], start=(kc == 0),
                                 stop=(kc == nk - 1))
                        if br == 0:
                            p.tt(m32[:, 0:TT], gt_[:, 0, 0:TT], pt[:, 0:TT], ALU.mult)
                        else:
                            p.tt(t32[:, 0:TT], gt_[:, br, 0:TT], pt[:, 0:TT], ALU.mult)
                            if br == 1:
                                p.tt(m32[:, 0:TT], m32[:, 0:TT], t32[:, 0:TT], ALU.add)
                            else:
                                p.tt(mT[:, oc, 0:TT], m32[:, 0:TT], t32[:, 0:TT], ALU.add)
                for st in range(TT // 128):
                    x_ = xt[xi % 2]
                    xi += 1
                    r0 = b * T + c0 + st * 128
                    p.dma("sp", x_[:, :], g.xs[s][r0:r0 + 128, :])
                    for half in range(2):
                        po = g.ps()
                        for kc in range(8):
                            p.mm(po[:, 0:512], mT[:, kc, st * 128:(st + 1) * 128], wo[:, kc, half * 512:(half + 1) * 512],
                                 start=(kc == 0), stop=(kc == 7))
                        p.tt(tx[:, :], po[:, 0:512], GM[:, half * 512:(half + 1) * 512], ALU.mult)
                        p.tt(x_[:, half * 512:(half + 1) * 512], x_[:, half * 512:(half + 1) * 512], tx[:, :], ALU.add)
                    p.dma("sp", g.xs[s][r0:r0 + 128, :], x_[:, :])
    p.end_phase()


MT_ = 512


def phase_moe(g, l, last):
    p, NB, W = g.p, g.NB, g.W
    macros = []
    for b in range(NB):
        if not last:
            macros.append(("ctx", b, 0, CTX))
        for t0 in range(0, SEQ, MT_):
            macros.append(("lat", b, t0, MT_))
    for (s, b, t0, MT) in macros:
        T = g.T[s]
        v = b if s == "lat" else NB
        r0 = b * T + t0
        G2 = load_mod_bcast(g, l, v, 4, "mo_G2")
        SH2 = load_mod_bcast(g, l, v, 3, "mo_SH2")
        gf = p.sb("mo_gf", [128, D])
        src = W["g_ffn"][l, :]
        p.dma("sp", gf[:, :], dap(src, src.offset, [[0, 128], [1, D]]))
        p.stt(G2[:, :], G2[:, :], 1.0, gf[:, :], ALU.add, ALU.mult)
        wr = p.sb("mo_wr", [128, 8, NE])
        p.dma("sp", wr[:, :, :], dap(W["w_router"], l * D * NE, [[NE, 128], [128 * NE, 8], [1, NE]]))
        brt = p.sb("mo_br", [128, NE])
        p.dma("sp", brt[:, :], dap(W["b_router"], l * NE, [[0, 128], [1, NE]]))
        lg = p.sb("mo_lg", [128, NE])
        ex = p.sb("mo_ex", [128, NE])
        mk = p.sb("mo_mk", [128, NE])
        mx = p.sb("mo_mx", [128, 8])
        sc = p.sb("mo_sc", [128, 4])

        def router(tt, h32):
            pr = g.ps()
            for kc in range(8):
                p.mm(pr[:, 0:NE], h32[:, kc, :], wr[:, kc, :], start=(kc == 0), stop=(kc == 7))
            p.tt(lg[:, :], pr[:, 0:NE], brt[:, :], ALU.add)
            p.op("dve", lambda e: e.max(out=mx[:, :], in_=lg[:, :]), [lg[:, :]], [mx[:, :]])
            p.ts(mk[:, :], lg[:, :], mx[:, 3:4], None, op0=ALU.is_ge)
            p.ts(sc[:, 0:1], mx[:, 0:1], -1.0, None, op0=ALU.mult)
            p.act(ex[:, :], lg[:, :], AF.Exp, bias=sc[:, 0:1])
            p.tt(ex[:, :], ex[:, :], mk[:, :], ALU.mult)
            p.op("dve", lambda e: e.reduce_sum(out=sc[:, 1:2], in_=ex[:, :], axis=AX.X), [ex[:, :]], [sc[:, 1:2]])
            p.recip(sc[:, 2:3], sc[:, 1:2])
            p.ts(g.Wg[:, tt, :], ex[:, :], sc[:, 2:3], None, op0=ALU.mult)
            pw = g.ps()
            p.tr(pw[0:NE, 0:128], g.Wg[:, tt, :], g.ident)
            p.copy(g.WgT[:, tt, :], pw[0:NE, 0:128])

        norm_tiles(g, g.xs[s][r0:r0 + MT, :], MT, G2, SH2, g.hT2, router)
        p.end_phase()
        import os
        MOS = int(os.environ.get("MOE_STOP", "9"))
        if MOS == 1:
            return
        nt = MT // 128
        yacc = p.sb("mo_y", [128, nt, D])
        b2all = p.sb("mo_b2all", [NE, D])
        p.dma("sp", b2all[:, :], dap(W["b_e2"], l * NE * D, [[D, NE], [1, D]]))
        for st in range(nt):
            for half in range(2):
                po = g.ps()
                p.mm(po[:, 0:512], g.WgT[:, st, :], b2all[:, half * 512:(half + 1) * 512])
                p.copy(yacc[:, st, half * 512:(half + 1) * 512], po[:, 0:512], eng="act")
        W1 = [p.sb("mo_W1_%d" % i, [128, 8, 2 * D], BF16) for i in range(2)]
        W2 = [p.sb("mo_W2_%d" % i, [128, 8, D], BF16) for i in range(2)]
        b1 = [p.sb("mo_b1_%d" % i, [128, 16]) for i in range(2)]
        actT = p.sb("mo_act", [128, 8, MT], BF16)
        gl = [p.sb("mo_gl%d" % i, [128, MT]) for i in range(2)]
        sg = [p.sb("mo_sg%d" % i, [128, MT]) for i in range(2)]
        l1 = [p.sb("mo_l1%d" % i, [128, MT]) for i in range(2)]
        tz = [p.sb("mo_tz%d" % i, [128, 512]) for i in range(2)]
        zi = 0
        for e in range(NE):
            w1, w2, b1t = W1[e % 2], W2[e % 2], b1[e % 2]
            for hh in range(2):
                sr = g.e1bf[l][e, 0, hh * D]
                p.dma("sp", w1[:, :, hh * D:(hh + 1) * D], dap(sr, sr.offset, [[2 * D, 128], [128 * 2 * D, 8], [1, D]]))
            sr = g.e2bf[l][e, 0, 0]
            p.dma("sp", w2[:, :, :], dap(sr, sr.offset, [[D, 128], [128 * D, 8], [1, D]]))
            p.dma("sp", b1t[:, :], dap(W["b_e1"], (l * NE + e) * 2 * D, [[1, 128], [128, 16]]), allow_slow_non_contiguous=True)
            for j in range(8):
                pg = g.ps()
                for kc in range(8):
                    p.mm(pg[:, 0:MT], w1[:, kc, j * 128:(j + 1) * 128], g.hT2[:, kc, 0:MT], start=(kc == 0), stop=(kc == 7))
                for kc in range(8):
                    p.mm(pg[:, 512:512 + MT], w1[:, kc, D + j * 128:D + (j + 1) * 128], g.hT2[:, kc, 0:MT], start=(kc == 0),
                         stop=(kc == 7))
                g_, s_, l_ = gl[j % 2], sg[j % 2], l1[j % 2]
                p.ts(g_[:, :], pg[:, 0:MT], b1t[:, j:j + 1], 7.0, op0=ALU.add, op1=ALU.min)
                p.act(l_[:, :], pg[:, 512:512 + MT], AF.Identity, bias=b1t[:, 8 + j:9 + j])
                p.act(s_[:, :], g_[:, :], AF.Sigmoid, scale=1.702)
                p.ts(l_[:, :], l_[:, :], -7.0, 7.0, op0=ALU.max, op1=ALU.min)
                p.tt(g_[:, :], g_[:, :], s_[:, :], ALU.mult)
                p.stt(actT[:, j, :], l_[:, :], 1.0, g_[:, :], ALU.add, ALU.mult)
            for st in range(nt):
                for half in range(2):
                    po = g.ps()
                    for kc in range(8):
                        p.mm(po[:, 0:512], actT[:, kc, st * 128:(st + 1) * 128], w2[:, kc, half * 512:(half + 1) * 512],
                             start=(kc == 0), stop=(kc == 7))
                    ya = yacc[:, st, half * 512:(half + 1) * 512]
                    p.stt(ya, po[:, 0:512], g.Wg[:, st, e:e + 1], ya, ALU.mult, ALU.add)
        GMLP = load_mod_bcast(g, l, v, 5, "mo_GMLP")
        xt = [p.sb("mo_x%d" % i, [128, D]) for i in range(2)]
        for st in range(nt):
            x_ = xt[st % 2]
            rr = r0 + st * 128
            p.dma("sp", x_[:, :], g.xs[s][rr:rr + 128, :])
            p.tt(yacc[:, st, :], yacc[:, st, :], GMLP[:, :], ALU.mult)
            p.tt(x_[:, :], x_[:, :], yacc[:, st, :], ALU.add)
            p.dma("sp", g.xs[s][rr:rr + 128, :], x_[:, :])
        p.end_phase()
        if MOS in (2, 3):
            return


def phase_final(g):
    p, NB, W = g.p, g.NB, g.W
    gfin = p.sb("fn_g", [128, D])
    p.dma("sp", gfin[:, :], dap(W["g_final"], 0, [[0, 128], [1, D]]))
    xts = [p.sb("fn_x%d" % i, [128, D]) for i in range(2)]
    sq = p.sb("fn_sq", [128, D])
    ss = p.sb("fn_ss", [128, 4])
    for tt in range(NB * SEQ // 128):
        xt = xts[tt % 2]
        p.dma("sp", xt[:, :], g.xs["lat"][tt * 128:(tt + 1) * 128, :])
        p.act(sq[:, :], xt[:, :], AF.Square)
        p.op("dve", lambda e, o=ss[:, 0:1], i=sq[:, :]: e.reduce_sum(out=o, in_=i, axis=AX.X), [sq[:, :]], [ss[:, 0:1]])
        p.ts(ss[:, 1:2], ss[:, 0:1], 1.0 / D, EPS, op0=ALU.mult, op1=ALU.add)
        p.act(ss[:, 2:3], ss[:, 1:2], AF.Sqrt)
        p.recip(ss[:, 3:4], ss[:, 2:3])
        p.stt(xt[:, :], xt[:, :], ss[:, 3:4], gfin[:, :], ALU.mult, ALU.mult)
        p.dma("sp", g.out[tt * 128:(tt + 1) * 128, :], xt[:, :])
    p.end_phase()


_NC_CACHE = {}


def kernel(**inputs):
    NCORES = 8
    NB = 32 // NCORES
    if "nc" not in _NC_CACHE:
        _NC_CACHE["nc"] = build_program(NB=NB)
    nc = _NC_CACHE["nc"]
    consts = make_consts()
    f32 = lambda a: np.ascontiguousarray(np.asarray(a, dtype=np.float32))
    wts = {n: f32(inputs[n]).reshape(WSHAPES[n]) for n in WNAMES}
    x = f32(inputs["x"])
    ctx = f32(inputs["ctx"])
    c = f32(inputs["c"])
    cctx = f32(inputs["c_ctx"]).reshape(1, D)
    in_maps = []
    for i in range(NCORES):
        m = {"x": x[i * NB:(i + 1) * NB].reshape(NB * SEQ, D), "ctx": ctx[i * NB:(i + 1) * NB].reshape(NB * CTX, D),
             "c": c[i * NB:(i + 1) * NB], "c_ctx": cctx, "consts": consts}
        m.update(wts)
        in_maps.append(m)
    res = run_bass_kernel_spmd(nc, in_maps, core_ids=list(range(NCORES)))
    out = np.concatenate([r["out"].reshape(NB, SEQ, D) for r in res.results], axis=0)
    return out.astype(np.float32)
```
